# Optimizing a Trainium2 kernel written in Bass

```python
import jax
import jax.numpy as jnp
from jax import lax
import numpy as np

D_MODEL = 1024
BATCH = 4
SEQ = 4096
DEPTH = 2

GRID_W = 64
CTX_LEN = 256
HEAD_DIM = 64
RET_HEADS = 8
NA_HEADS = 8
RET_W = RET_HEADS * HEAD_DIM
NA_W = NA_HEADS * HEAD_DIM
MIX_W = RET_W + NA_W
AB_KV_W = 2 * RET_W + 2 * NA_W
AB_IN_W = AB_KV_W + 2 * RET_W + NA_W
RET_CHUNK = 128
NA_ROWS = 8
NA_COLS = 16
ROPE_BASE = 10000.0
SGU_CHUNK = 128
SGU_GROUPS = 8
SGU_W = 3 * D_MODEL
N_EXPERTS = 16
N_GROUPS = 4
EXPERTS_PER_GROUP = N_EXPERTS // N_GROUPS
GROUP_SCORE_K = 2
TOP_K = 2
EXPERT_W = 512
N_EVEN = (DEPTH + 1) // 2
N_ODD = DEPTH // 2
EPS = 1e-6

kernel_name = 'hybrid_retention_natten_sgu_moe_dit'


def rms_norm(x, g):
    xf = x.astype(jnp.float32)
    y = xf * lax.rsqrt(jnp.mean(xf * xf, axis=-1, keepdims=True) + EPS)
    return (y * g.astype(jnp.float32)).astype(x.dtype)


def modulate(h, shift, scale):
    return h * (1 + scale[:, None, :]) + shift[:, None, :]


def split_heads(t, n_heads):
    b, l, _ = t.shape
    return t.reshape(b, l, n_heads, HEAD_DIM).transpose(0, 2, 1, 3)


def merge_heads(t):
    b, h, l, d = t.shape
    return t.transpose(0, 2, 1, 3).reshape(b, l, h * d)


def rope_2d(t, rows, cols):
    half = HEAD_DIM // 2
    quarter = half // 2
    inv_freq = ROPE_BASE ** (-jnp.arange(quarter, dtype=jnp.float32) / quarter)

    def rotate(u, pos):
        ang = pos.astype(jnp.float32)[:, None] * inv_freq
        cos, sin = jnp.cos(ang).astype(u.dtype), jnp.sin(ang).astype(u.dtype)
        u1, u2 = u[..., :quarter], u[..., quarter:]
        return jnp.concatenate([u1 * cos - u2 * sin, u1 * sin + u2 * cos], axis=-1)

    return jnp.concatenate([rotate(t[..., :half], rows), rotate(t[..., half:], cols)], axis=-1)


def retention_chunkwise(q, k, v, log_gamma, s0):
    b, h, l, d = q.shape
    n = l // RET_CHUNK
    lg = log_gamma.astype(jnp.float32)
    pos = jnp.arange(RET_CHUNK, dtype=jnp.float32)
    diff = pos[:, None] - pos[None, :]
    intra = jnp.where(diff >= 0, jnp.exp(lg[:, None, None] * jnp.maximum(diff, 0.0)), 0.0)
    q_decay = jnp.exp(lg[:, None] * (pos + 1.0))[..., None]
    k_decay = jnp.exp(lg[:, None] * (RET_CHUNK - 1.0 - pos))[..., None]
    chunk_decay = jnp.exp(lg * RET_CHUNK)[:, None, None]

    def to_chunks(t):
        return t.astype(jnp.float32).reshape(b, h, n, RET_CHUNK, d).transpose(2, 0, 1, 3, 4)

    def step(s, qkv):
        qi, ki, vi = qkv
        scores = jnp.einsum('bhid,bhjd->bhij', qi, ki) * intra
        o = jnp.einsum('bhij,bhje->bhie', scores, vi) + jnp.einsum('bhid,bhde->bhie', qi, s) * q_decay
        s = s * chunk_decay + jnp.einsum('bhjd,bhje->bhde', ki * k_decay, vi)
        return s, o

    _, o = lax.scan(step, s0.astype(jnp.float32), (to_chunks(q), to_chunks(k), to_chunks(v)))
    return o.transpose(1, 2, 0, 3, 4).reshape(b, h, l, d).astype(q.dtype)


def retention_context_states(k, v, log_gamma):
    kf, vf = k.astype(jnp.float32), v.astype(jnp.float32)
    n = k.shape[2]
    m = jnp.arange(n, dtype=jnp.float32)
    w_fwd = jnp.exp(log_gamma[0][:, None] * (n - 1.0 - m))
    w_bwd = jnp.exp(log_gamma[1][:, None] * m)
    s_fwd = jnp.einsum('bhnd,hn,bhne->bhde', kf, w_fwd, vf)
    s_bwd = jnp.einsum('bhnd,hn,bhne->bhde', kf, w_bwd, vf)
    return s_fwd, s_bwd


def bidir_retention(q, k, v, log_gamma, s_fwd, s_bwd):
    fwd = retention_chunkwise(q, k, v, log_gamma[0], s_fwd)
    bwd = retention_chunkwise(q[:, :, ::-1], k[:, :, ::-1], v[:, :, ::-1], log_gamma[1], s_bwd)[:, :, ::-1]
    return fwd + bwd


def neighbourhood_attention(q, k, v, k_ctx, v_ctx, rpb):
    b, h, l, d = q.shape
    rows = l // GRID_W
    kr = min(NA_ROWS, rows)
    qg = q.reshape(b, h, rows, GRID_W, d)
    kg = k.reshape(b, h, rows, GRID_W, d)
    vg = v.reshape(b, h, rows, GRID_W, d)
    col_pos = jnp.arange(GRID_W)
    col_idx = jnp.clip(col_pos - NA_COLS // 2, 0, GRID_W - NA_COLS)[:, None] + jnp.arange(NA_COLS)
    col_bias_idx = col_idx - col_pos[:, None] + (NA_COLS - 1)
    n_loc = kr * NA_COLS

    def row_block(r):
        r0 = jnp.clip(r - kr // 2, 0, rows - kr)
        k_rows = lax.dynamic_slice_in_dim(kg, r0, kr, axis=2)
        v_rows = lax.dynamic_slice_in_dim(vg, r0, kr, axis=2)
        q_r = lax.dynamic_index_in_dim(qg, r, axis=2, keepdims=False)
        k_win = k_rows[:, :, :, col_idx]
        v_win = v_rows[:, :, :, col_idx]
        row_bias_idx = r0 + jnp.arange(kr) - r + (NA_ROWS - 1)
        bias = rpb[:, row_bias_idx][:, :, col_bias_idx].transpose(0, 2, 1, 3)
        s_loc = jnp.einsum('bhwd,bhrwcd->bhwrc', q_r, k_win) + bias
        s_ctx = jnp.einsum('bhwd,bhnd->bhwn', q_r, k_ctx)
        s = jnp.concatenate([s_loc.reshape(b, h, GRID_W, n_loc), s_ctx], axis=-1).astype(jnp.float32)
        p = jax.nn.softmax(s, axis=-1).astype(q.dtype)
        p_loc = p[..., :n_loc].reshape(b, h, GRID_W, kr, NA_COLS)
        p_ctx = p[..., n_loc:]
        return jnp.einsum('bhwrc,bhrwcd->bhwd', p_loc, v_win) + jnp.einsum('bhwn,bhnd->bhwd', p_ctx, v_ctx)

    out = lax.map(row_block, jnp.arange(rows))
    return out.transpose(1, 2, 0, 3, 4).reshape(b, h, l, d)


def retention_na_mixer(h, h_ctx, rows, cols, w_in, w_out, ret_theta, ret_g, q_g, k_g, rpb, ctx_out):
    scale = HEAD_DIM ** -0.5
    kv, rest = jnp.split(h @ w_in, [AB_KV_W], axis=-1)
    r_k, r_v, n_k, n_v = [split_heads(t, RET_HEADS) for t in jnp.split(kv, 4, axis=-1)]
    r_q, r_gate, n_q = jnp.split(rest, 3, axis=-1)
    proj_c = h_ctx @ (w_in if ctx_out else w_in[:, :AB_KV_W])
    rc_k, rc_v, nc_k, nc_v = [split_heads(t, RET_HEADS) for t in jnp.split(proj_c[..., :AB_KV_W], 4, axis=-1)]

    log_gamma = -jax.nn.softplus(ret_theta.astype(jnp.float32))
    rc_k = rc_k * scale
    s_fwd, s_bwd = retention_context_states(rc_k, rc_v, log_gamma)
    q = rope_2d(split_heads(r_q, RET_HEADS), rows, cols)
    k = rope_2d(r_k, rows, cols) * scale
    o = bidir_retention(q, k, r_v, log_gamma, s_fwd, s_bwd)
    ret = merge_heads(rms_norm(o, ret_g)) * jax.nn.silu(r_gate)

    kc_n = rms_norm(nc_k, k_g)
    qn = rms_norm(split_heads(n_q, NA_HEADS), q_g) * scale
    na = merge_heads(neighbourhood_attention(qn, rms_norm(n_k, k_g), n_v, kc_n, nc_v, rpb))
    y = jnp.concatenate([ret, na], axis=-1) @ w_out
    if not ctx_out:
        return y, None

    rc_q, rc_gate, nc_q = jnp.split(proj_c[..., AB_KV_W:], 3, axis=-1)
    zeros = jnp.zeros(s_fwd.shape, jnp.float32)
    o_c = bidir_retention(split_heads(rc_q, RET_HEADS), rc_k, rc_v, log_gamma, zeros, zeros)
    ret_c = merge_heads(rms_norm(o_c, ret_g)) * jax.nn.silu(rc_gate)
    qn_c = rms_norm(split_heads(nc_q, NA_HEADS), q_g) * scale
    p = jax.nn.softmax(jnp.einsum('bhqd,bhkd->bhqk', qn_c, kc_n).astype(jnp.float32), axis=-1).astype(h.dtype)
    na_c = merge_heads(jnp.einsum('bhqk,bhkd->bhqd', p, nc_v))
    y_ctx = jnp.concatenate([ret_c, na_c], axis=-1) @ w_out
    return y, y_ctx


def spatial_gating_unit(h, w_in, b_in, norm_g, w_s, b_s, w_out):
    b, l, _ = h.shape
    z = jax.nn.gelu(h @ w_in + b_in)
    u, v = jnp.split(z, 2, axis=-1)
    v = rms_norm(v, norm_g)
    vg = v.reshape(b, l // SGU_CHUNK, SGU_CHUNK, SGU_GROUPS, SGU_W // SGU_GROUPS)
    mixed = jnp.einsum('gij,bnjgc->bnigc', w_s, vg) + b_s.T[:, :, None]
    return (u * mixed.reshape(b, l, SGU_W)) @ w_out


def moe(h, router_w, router_bias, w_gate, w_up, w_down):
    affinity = jax.nn.sigmoid((h @ router_w).astype(jnp.float32))
    sel = affinity + router_bias.astype(jnp.float32)
    grouped = sel.reshape(*sel.shape[:-1], N_GROUPS, EXPERTS_PER_GROUP)
    group_score = lax.top_k(grouped, GROUP_SCORE_K)[0].sum(axis=-1)
    best = jnp.argmax(group_score, axis=-1)
    in_group = (jnp.arange(N_EXPERTS) // EXPERTS_PER_GROUP) == best[..., None]
    _, top_idx = lax.top_k(jnp.where(in_group, sel, -jnp.inf), TOP_K)
    w = jnp.take_along_axis(affinity, top_idx, axis=-1)
    w = w / jnp.sum(w, axis=-1, keepdims=True)
    combine = jnp.sum(jax.nn.one_hot(top_idx, N_EXPERTS, dtype=jnp.float32) * w[..., None], axis=-2).astype(h.dtype)
    out = jnp.zeros_like(h)
    for e in range(N_EXPERTS):
        y = (jax.nn.silu(h @ w_gate[e]) * (h @ w_up[e])) @ w_down[e]
        out = out + combine[..., e:e + 1] * y
    return out


def setup_inputs(seed: int = 0) -> dict:
    key = jax.random.key(seed)
    ks = jax.random.split(key, 26)
    f32 = jnp.float32
    D = D_MODEL

    def nrm(k, shape, scale):
        return jax.random.normal(k, shape, f32) * scale

    theta0 = np.log(np.expm1(-np.log(1.0 - 2.0 ** (-5.0 - np.arange(RET_HEADS))))).astype(np.float32)
    return {
        'x': nrm(ks[0], (BATCH, SEQ, D), 1.0),
        'c': nrm(ks[1], (BATCH, D), 1.0),
        'ctx': nrm(ks[2], (BATCH, CTX_LEN, D), 1.0),
        'c_ctx': nrm(ks[3], (D,), 1.0),
        'ada_w': nrm(ks[4], (DEPTH, D, 6 * D), 0.5 * D ** -0.5),
        'ada_b': nrm(ks[5], (DEPTH, 6 * D), 0.02),
        'norm_mix_g': 1.0 + nrm(ks[6], (DEPTH, D), 0.05),
        'norm_ffn_g': 1.0 + nrm(ks[7], (DEPTH, D), 0.05),
        'router_w': nrm(ks[8], (D, N_EXPERTS), D ** -0.5),
        'router_bias': nrm(ks[9], (N_EXPERTS,), 0.01),
        'moe_w_gate': nrm(ks[10], (DEPTH, N_EXPERTS, D, EXPERT_W), D ** -0.5),
        'moe_w_up': nrm(ks[11], (DEPTH, N_EXPERTS, D, EXPERT_W), D ** -0.5),
        'moe_w_down': nrm(ks[12], (DEPTH, N_EXPERTS, EXPERT_W, D), EXPERT_W ** -0.5),
        'ab_w_in': nrm(ks[13], (N_EVEN, D, AB_IN_W), D ** -0.5),
        'ab_w_out': nrm(ks[14], (N_EVEN, MIX_W, D), MIX_W ** -0.5),
        'ret_decay': jnp.asarray(theta0) + nrm(ks[15], (N_EVEN, 2, RET_HEADS), 0.05),
        'ret_norm_g': 1.0 + nrm(ks[16], (N_EVEN, HEAD_DIM), 0.05),
        'na_q_g': 1.0 + nrm(ks[17], (N_EVEN, HEAD_DIM), 0.05),
        'na_k_g': 1.0 + nrm(ks[18], (N_EVEN, HEAD_DIM), 0.05),
        'na_rpb': nrm(ks[19], (N_EVEN, NA_HEADS, 2 * NA_ROWS - 1, 2 * NA_COLS - 1), 0.02),
        'sgu_w_in': nrm(ks[20], (N_ODD, D, 2 * SGU_W), D ** -0.5),
        'sgu_b_in': nrm(ks[21], (N_ODD, 2 * SGU_W), 0.02),
        'sgu_norm_g': 1.0 + nrm(ks[22], (N_ODD, SGU_W), 0.05),
        'sgu_w_s': nrm(ks[23], (N_ODD, SGU_GROUPS, SGU_CHUNK, SGU_CHUNK), SGU_CHUNK ** -0.5),
        'sgu_b_s': 1.0 + nrm(ks[24], (N_ODD, SGU_GROUPS, SGU_CHUNK), 0.02),
        'sgu_w_out': nrm(ks[25], (N_ODD, SGU_W, D), SGU_W ** -0.5),
    }


def reference(x, c, ctx, c_ctx, ada_w, ada_b, norm_mix_g, norm_ffn_g, router_w, router_bias,
              moe_w_gate, moe_w_up, moe_w_down, ab_w_in, ab_w_out, ret_decay, ret_norm_g,
              na_q_g, na_k_g, na_rpb, sgu_w_in, sgu_b_in, sgu_norm_g, sgu_w_s, sgu_b_s, sgu_w_out):
    seq = x.shape[1]
    t = jnp.arange(seq)
    rows, cols = t // GRID_W, t % GRID_W
    silu_c = jax.nn.silu(c)
    silu_cc = jax.nn.silu(c_ctx)[None]
    for l in range(DEPTH):
        even = l % 2 == 0
        i = l // 2
        ctx_out = any(j % 2 == 0 for j in range(l + 1, DEPTH))
        sh_m, sc_m, g_m, sh_f, sc_f, g_f = jnp.split(silu_c @ ada_w[l] + ada_b[l], 6, axis=-1)
        h = modulate(rms_norm(x, norm_mix_g[l]), sh_m, sc_m)
        if even or ctx_out:
            n_mod = 6 if ctx_out else 2
            mod_c = jnp.split(silu_cc @ ada_w[l][:, :n_mod * D_MODEL] + ada_b[l][:n_mod * D_MODEL], n_mod, axis=-1)
            h_ctx = modulate(rms_norm(ctx, norm_mix_g[l]), mod_c[0], mod_c[1])
        if even:
            y, y_ctx = retention_na_mixer(h, h_ctx, rows, cols, ab_w_in[i], ab_w_out[i], ret_decay[i],
                                          ret_norm_g[i], na_q_g[i], na_k_g[i], na_rpb[i], ctx_out)
        else:
            sgu_args = (sgu_w_in[i], sgu_b_in[i], sgu_norm_g[i], sgu_w_s[i], sgu_b_s[i], sgu_w_out[i])
            y = spatial_gating_unit(h, *sgu_args)
            y_ctx = spatial_gating_unit(h_ctx, *sgu_args) if ctx_out else None
        x = x + g_m[:, None, :] * y
        h_f = modulate(rms_norm(x, norm_ffn_g[l]), sh_f, sc_f)
        x = x + g_f[:, None, :] * moe(h_f, router_w, router_bias, moe_w_gate[l], moe_w_up[l], moe_w_down[l])
        if ctx_out:
            ctx = ctx + mod_c[2][:, None, :] * y_ctx
            hc_f = modulate(rms_norm(ctx, norm_ffn_g[l]), mod_c[3], mod_c[4])
            ctx = ctx + mod_c[5][:, None, :] * moe(hc_f, router_w, router_bias, moe_w_gate[l], moe_w_up[l], moe_w_down[l])
    return x
```

```python
import numpy as np
import ml_dtypes
from contextlib import ExitStack
import concourse.bass as bass
import concourse.mybir as mybir
from concourse.bass_utils import run_bass_kernel_spmd

F32 = mybir.dt.float32
BF16 = mybir.dt.bfloat16
AF = mybir.ActivationFunctionType
ALU = mybir.AluOpType
AX = mybir.AxisListType

D = 1024
NT = 16
TOK = 2048
NEG = -30000.0
EPS = 1e-6
BIG = 1.0e9


class Buf:
    __slots__ = ("name", "w", "r", "excl")

    def __init__(self, name="", excl=False):
        self.name = name
        self.w = None
        self.r = []
        self.excl = excl


class Eng:
    def __init__(self, name, h, sem):
        self.name = name
        self.h = h
        self.sem = sem
        self.count = 0
        self.waited = {}
        self.dsems = []
        self.dcnt = []
        self.di = 0


class KB:
    def __init__(self):
        self.nc = bass.Bass("TRN2", target_bir_lowering=False)
        self.es = ExitStack()
        nc = self.nc
        self.E = {}
        for name, h in (("pe", nc.tensor), ("act", nc.scalar), ("dve", nc.vector),
                        ("pool", nc.gpsimd), ("sp", nc.sync)):
            sem = self.es.enter_context(nc.semaphore("s_" + name))
            self.E[name] = Eng(name, h, sem)
        for qn in ("sp", "pool", "act"):
            q = self.E[qn]
            for i in range(8):
                q.dsems.append(self.es.enter_context(nc.semaphore("d_%s%d" % (qn, i))))
                q.dcnt.append(0)
        self.dma_events = []

    def _wait(self, e, ev):
        key, sem, val = ev
        if e.waited.get(key, 0) >= val:
            return
        e.h.wait_ge(sem, val)
        e.waited[key] = val

    def _deps(self, e, R, W):
        for b in R:
            if b.w is not None:
                if b.w[0] == e.name and e.name == "pe":
                    continue
                self._wait(e, b.w)
        for b in W:
            if b.w is not None and b.w[0] != e.name:
                self._wait(e, b.w)
            for ev in b.r:
                if ev[0] != e.name:
                    self._wait(e, ev)

    def op(self, en, fn, R=(), W=(), inc=True):
        e = self.E[en]
        if any(b.excl for b in R):
            W = list(W) + [b for b in R if b.excl and b not in W]
            R = [b for b in R if not b.excl]
        self._deps(e, R, W)
        ins = fn()
        val = e.count + 1
        if inc:
            ins.then_inc(e.sem, 1)
            e.count = val
        ev = (en, e.sem, val)
        for b in W:
            b.w = ev
            b.r = []
        for b in R:
            b.r.append(ev)
        return ins

    def dma(self, qn, out, in_, R=(), W=()):
        q = self.E[qn]
        self._deps(q, R, W)
        slot = q.di % len(q.dsems)
        q.di += 1
        sem = q.dsems[slot]
        key = "d_%s%d" % (qn, slot)
        if q.dcnt[slot] > 0:
            self._wait(q, (key, sem, 16 * q.dcnt[slot]))
        q.dcnt[slot] += 1
        q.h.dma_start(out=out, in_=in_).then_inc(sem, 16)
        ev = (key, sem, 16 * q.dcnt[slot])
        for b in W:
            b.w = ev
            b.r = []
        for b in R:
            b.r.append(ev)
        self.dma_events.append(ev)

    def barrier(self):
        evs = [(n, e.sem, e.count) for n, e in self.E.items() if e.count > 0]
        last = {}
        for ev in self.dma_events:
            last[ev[0]] = ev
        evs += list(last.values())
        self.dma_events = list(last.values())
        for n, e in self.E.items():
            for ev in evs:
                if ev[0] != n:
                    self._wait(e, ev)

    def sb(self, es, name, shape, dt):
        self.uid = getattr(self, "uid", 0) + 1
        return es.enter_context(self.nc.sbuf_tensor("sb%d_%s" % (self.uid, name), shape, dt))


class Ring:
    def __init__(self, kb, es, name, n, shape, dt):
        self.t = [kb.sb(es, "%s%d" % (name, i), shape, dt) for i in range(n)]
        self.b = [Buf("%s%d" % (name, i)) for i in range(n)]
        self.i = 0

    def next(self):
        k = self.i % len(self.t)
        self.i += 1
        return self.t[k], self.b[k]


def build(mode='full'):
    kb = KB()
    nc = kb.nc
    es = kb.es
    op = kb.op
    dma = kb.dma
    V = nc.vector
    A = nc.scalar
    P = nc.tensor
    G = nc.gpsimd

    def din(name, shape, dt=F32):
        return nc.dram_tensor(name, list(shape), dt, kind="ExternalInput").ap()

    SHAPES = {
        "x_loc": [TOK, D], "x_oth": [TOK, D], "x_halo": [512, D], "x_ctx": [256, D], "c_row": [1, D], "cc_row": [1, D],
        "ada_w": [2, D, 6 * D], "ada_b": [2, 6 * D], "norm_mix_g": [2, D], "norm_ffn_g": [2, D],
        "router_w": [D, 16], "router_bias": [1, 16],
        "moe_w_gate": [2, 16, D, 512], "moe_w_up": [2, 16, D, 512], "moe_w_down": [2, 16, 512, D],
        "ab_w_in": [D, 3584], "ab_w_out": [D, D], "ret_decay": [1, 16], "head_g": [1, 192],
        "bias_tab": [8, 128, 896], "rowmask": [2, 192], "rope_loc": [TOK, 256], "rope_oth": [TOK + 256, 128],
        "exp_init": [128, 36], "cst": [128, 260], "ident": [128, 128], "a2": [2, 128],
        "sgu_w_in": [D, 6144], "sgu_b_in": [1, 6144], "sgu_norm_g": [1, 3072], "sgu_w_s": [8, 128, 128],
        "sgu_bsT": [128, 8], "sgu_w_out": [3072, D], "dbg_x": [TOK, D],
    }
    declared = {}

    class Lazy:
        def __init__(self, name):
            self.name = name

        def ap(self):
            if self.name not in declared:
                declared[self.name] = nc.dram_tensor(self.name, list(SHAPES[self.name]), F32, kind="ExternalInput").ap()
            return declared[self.name]

        def __getitem__(self, k):
            return self.ap()[k]

        def rearrange(self, *a, **kw):
            return self.ap().rearrange(*a, **kw)

    kb.declared = declared
    x_loc, x_oth, x_halo, x_ctx, c_row, cc_row = (Lazy(n) for n in ("x_loc", "x_oth", "x_halo", "x_ctx", "c_row", "cc_row"))
    ada_w, ada_b, nmix_g, nffn_g = (Lazy(n) for n in ("ada_w", "ada_b", "norm_mix_g", "norm_ffn_g"))
    router_w, router_b = Lazy("router_w"), Lazy("router_bias")
    w_gate, w_up, w_down = Lazy("moe_w_gate"), Lazy("moe_w_up"), Lazy("moe_w_down")
    ab_w_in, ab_w_out, ret_decay, hg = Lazy("ab_w_in"), Lazy("ab_w_out"), Lazy("ret_decay"), Lazy("head_g")
    bias_tab, rowmask, rope_loc, rope_oth = Lazy("bias_tab"), Lazy("rowmask"), Lazy("rope_loc"), Lazy("rope_oth")
    exp_init, cst, ident_d, a2_d = Lazy("exp_init"), Lazy("cst"), Lazy("ident"), Lazy("a2")
    sgu_w_in, sgu_b_in, sgu_ng, sgu_ws = Lazy("sgu_w_in"), Lazy("sgu_b_in"), Lazy("sgu_norm_g"), Lazy("sgu_w_s")
    sgu_bsT, sgu_w_out, dbg_x = Lazy("sgu_bsT"), Lazy("sgu_w_out"), Lazy("dbg_x")
    y_out = nc.dram_tensor("y_out", [TOK, D], F32, kind="ExternalOutput").ap()
    xa = nc.dram_tensor("xa_scr", [TOK, D], F32, kind="Internal").ap()
    xb = nc.dram_tensor("xb_scr", [TOK, D], F32, kind="Internal").ap()
    xc = nc.dram_tensor("xc_scr", [TOK, D], F32, kind="Internal").ap()

    ps = es.enter_context(nc.psum_tensor("ps", [128, 4096], F32))
    ps_bf = ps.bitcast(BF16)
    psb = [Buf("ps%d" % i, excl=True) for i in range(8)]

    class PsRing:
        def __init__(self, banks):
            self.banks = banks
            self.i = 0

        def next(self):
            k = self.banks[self.i % len(self.banks)]
            self.i += 1
            return k

    def bank(k, n=512, off=0):
        return ps[:, k * 512 + off:k * 512 + off + n]

    def bank_bf(k, n=1024, off=0):
        return ps_bf[:, k * 1024 + off:k * 1024 + off + n]

    g = ExitStack()
    es.enter_context(g)
    ident = kb.sb(g, "ident", [128, 128], BF16)
    identf = kb.sb(g, "identf", [128, 128], F32)
    ones1 = kb.sb(g, "ones1", [1, 128], BF16)
    silc = kb.sb(g, "silc", [128, 8, 128], BF16)
    silcc = kb.sb(g, "silcc", [128, 8, 128], BF16)
    b_const = Buf("const")
    dma("pool", ident[:], ident_d[:, :], W=[b_const])
    dma("sp", identf[:], ident_d[:, :], W=[b_const])
    op("dve", lambda: V.memset(ones1[:], 1.0), W=[b_const])

    with ExitStack() as t:
        crow = kb.sb(t, "crow", [128, 2, D], F32)
        csil = kb.sb(t, "csil", [128, 2, D], BF16)
        bt = Buf("crow")
        dma("sp", crow[:, 0, :], c_row[0:1, :].partition_broadcast(128), W=[bt])
        dma("sp", crow[:, 1, :], cc_row[0:1, :].partition_broadcast(128), W=[bt])
        op("act", lambda: A.activation(out=csil[:], in_=crow[:], func=AF.Silu), R=[bt], W=[bt])
        for j, dst in enumerate((silc, silcc)):
            for dk in range(8):
                op("pe", lambda: P.transpose(bank_bf(0, 128, dk * 128), csil[:, j, dk * 128:(dk + 1) * 128], ident[:]),
                   R=[bt, b_const], W=[psb[0]], inc=(dk == 7))
            op("dve", lambda: V.tensor_copy(dst[:].rearrange("p a b -> p (a b)"), bank_bf(0)), R=[psb[0]], W=[b_const])
        kb.barrier()

    def ada_chunk(l, j, lhs, out_ap, out_buf, wring, bring, post=None):
        wt, wb = wring.next()
        dma("pool", wt[:], ada_w[l, :, j * 1024:(j + 1) * 1024].rearrange("(k p) n -> p k n", p=128), W=[wb])
        bt_, bb = bring.next()
        dma("pool", bt_[:], ada_b[l:l + 1, j * 1024:(j + 1) * 1024], W=[bb])
        for hf in range(2):
            k = 6 + hf
            for dk in range(8):
                op("pe", lambda: P.matmul(bank(k), lhs[:, dk, :], wt[:, dk, hf * 512:(hf + 1) * 512],
                                          start=(dk == 0), stop=False), R=[wb, b_const], W=[psb[k]], inc=False)
            op("pe", lambda: P.matmul(bank(k), ones1[:], bt_[:, hf * 512:(hf + 1) * 512], start=False, stop=True),
               R=[bb, b_const], W=[psb[k]])
        if post is None:
            op("act", lambda: A.copy(out=out_ap, in_=ps[:, 6 * 512:8 * 512]), R=[psb[6], psb[7]], W=[out_buf])
        else:
            post(ps[:, 6 * 512:8 * 512], [psb[6], psb[7]])

    def front_a(xt, xbuf, G1, SH, gbuf, fr, want_f32T=False):
        junk, jb = fr["junk"].next()
        st, sbf = fr["st"].next()
        op("act", lambda: A.activation(out=junk[:], in_=xt, func=AF.Square, accum_out=st[:, 0:1]),
           R=[xbuf], W=[jb, sbf])
        op("act", lambda: A.activation(out=st[:, 1:2], in_=st[:, 0:1], func=AF.Sqrt, scale=1.0 / D, bias=fr["eps"][:, 0:1]),
           R=[sbf, b_const], W=[sbf])
        op("dve", lambda: V.reciprocal(st[:, 2:3], st[:, 1:2]), R=[sbf], W=[sbf])
        tmp, tb = fr["tmp"].next()
        op("dve", lambda: V.scalar_tensor_tensor(out=tmp[:], in0=xt, scalar=st[:, 2:3], in1=G1, op0=ALU.mult, op1=ALU.mult),
           R=[xbuf, sbf, gbuf], W=[tb])
        if want_f32T:
            h, hb = fr["h32"].next()
        else:
            h, hb = fr["h"].next()
        op("pool", lambda: G.tensor_tensor(out=h[:], in0=tmp[:], in1=SH, op=ALU.add), R=[tb, gbuf], W=[hb])
        return h, hb, want_f32T

    def front_b(fa, fr, bf_dst=None):
        h, hb, want_f32T = fa
        if not want_f32T:
            hT, hTb = fr["hT"].next()
            k = fr["ps"].next()
            for dk in range(8):
                op("pe", lambda: P.transpose(bank_bf(k, 128, dk * 128), h[:, dk * 128:(dk + 1) * 128], ident[:]),
                   R=[hb, b_const], W=[psb[k]], inc=(dk == 7))
            op("act", lambda: A.copy(out=hT[:].rearrange("p a b -> p (a b)"), in_=bank_bf(k)), R=[psb[k]], W=[hTb])
            return hT, hTb, None, None
        else:
            hT32, hT32b = fr["hT32"].next()
            dst, dstb = bf_dst
            for half in range(2):
                k = fr["ps"].next()
                for q in range(4):
                    dk = half * 4 + q
                    op("pe", lambda: P.transpose(bank(k, 128, q * 128), h[:, dk * 128:(dk + 1) * 128], identf[:]),
                       R=[hb, b_const], W=[psb[k]], inc=(q == 3))
                op("act", lambda: A.copy(out=dst[:, half * 4:(half + 1) * 4, :], in_=bank(k).rearrange("p (a b) -> p a b", b=128)),
                   R=[psb[k]], W=[dstb])
                op("dve", lambda: V.tensor_copy(hT32[:, half * 4:(half + 1) * 4, :].rearrange("p a b -> p (a b)"), bank(k)),
                   R=[psb[k]], W=[hT32b])
            return None, None, hT32, hT32b

    def front(xt, xbuf, G1, SH, gbuf, fr, want_f32T=False):
        return front_b(front_a(xt, xbuf, G1, SH, gbuf, fr, want_f32T), fr)

    def pipelined(n, head, body):
        nxt = head(0)
        for i in range(n):
            cur = nxt
            nxt = head(i + 1) if i + 1 < n else None
            body(i, cur)

    def make_front(t, ps_banks, f32T=False):
        fr = {}
        fr["junk"] = Ring(kb, t, "fjunk", 1, [128, D], BF16)
        fr["st"] = Ring(kb, t, "fst", 3, [128, 4], F32)
        fr["tmp"] = Ring(kb, t, "ftmp", 1, [128, D], F32)
        if f32T:
            fr["h32"] = Ring(kb, t, "fh32", 2, [128, D], F32)
            fr["hT32"] = Ring(kb, t, "fhT32", 2, [128, 8, 128], F32)
        else:
            fr["h"] = Ring(kb, t, "fh", 2, [128, D], BF16)
        if not f32T:
            fr["hT"] = Ring(kb, t, "fhT", 2, [128, 8, 128], BF16)
        fr["ps"] = PsRing(ps_banks)
        eps = kb.sb(t, "feps", [128, 1], F32)
        op("dve", lambda: V.memset(eps[:], EPS), W=[b_const])
        fr["eps"] = eps
        return fr

    def make_mod(t, l, jshift, jscale, gsrc, lhs, name, wring, bring):
        G1 = kb.sb(t, name + "G1", [128, D], F32)
        SH = kb.sb(t, name + "SH", [128, D], F32)
        gb = kb.sb(t, name + "gb", [128, D], F32)
        mb = Buf(name)
        dma("sp", gb[:], gsrc.partition_broadcast(128), W=[mb])
        ada_chunk(l, jshift, lhs, SH[:], mb, wring, bring)

        def post(psap, pbufs):
            op("dve", lambda: V.scalar_tensor_tensor(out=G1[:], in0=psap, scalar=1.0, in1=gb[:], op0=ALU.add, op1=ALU.mult),
               R=pbufs + [mb], W=[mb])
        ada_chunk(l, jscale, lhs, None, mb, wring, bring, post=post)
        return G1, SH, mb

    def moe_phase(l, x_src, x_dst):
        with ExitStack() as t:
            xres = kb.sb(t, "xres", [128, NT, D], F32)
            xrb = [Buf("xres%d" % i) for i in range(NT)]
            hfT = kb.sb(t, "hfT", [128, 8, TOK], BF16)
            hfTb = [Buf("hfT%d" % i) for i in range(NT)]
            comb = kb.sb(t, "comb", [128, NT, 16], F32)
            combb = [Buf("comb%d" % i) for i in range(NT)]
            rw = kb.sb(t, "rw", [128, 8, 16], F32)
            rbias = kb.sb(t, "rbias", [128, 16], F32)
            gfb = kb.sb(t, "gfb", [128, D], F32)
            gfbuf = Buf("gfb")
            cb = Buf("rconst")
            dma("sp", rw[:], router_w.rearrange("(k p) e -> p k e", p=128), W=[cb])
            dma("sp", rbias[:], router_b[0:1, :].partition_broadcast(128), W=[cb])
            for i in range(NT):
                dma("sp", xres[:, i, :], x_src[i * 128:(i + 1) * 128, :], W=[xrb[i]])
            with ExitStack() as t2:
                wring = Ring(kb, t2, "adaw", 2, [128, 8, 1024], BF16)
                bring = Ring(kb, t2, "adab", 2, [1, 1024], BF16)
                G1, SH, mb = make_mod(t2, l, 3, 4, nffn_g[l:l + 1, :], silc, "mf", wring, bring)
                ada_chunk(l, 5, silc, gfb[:], gfbuf, wring, bring)
                fr = make_front(t2, [0, 1], f32T=True)
                RB = kb.sb(t2, "RB", [128, 4, NT, 16], F32)
                RS = kb.sb(t2, "RS", [128, 6, NT, 4], F32)
                rb = Buf("RB")
                fas = {}
                t32 = {}
                for j in range(NT + 2):
                    if j < NT:
                        fas[j] = front_a(xres[:, j, :], xrb[j], G1[:], SH[:], mb, fr, want_f32T=True)
                    i = j - 1
                    if 0 <= i < NT:
                        _, _, hT32, hT32b = front_b(fas.pop(i), fr, bf_dst=(hfT[:, :, i * 128:(i + 1) * 128], hfTb[i]))
                        t32[i] = (hT32, hT32b)
                    i = j - 2
                    if 0 <= i < NT:
                        hT32, hT32b = t32.pop(i)
                        k = 2 + (i % 2)
                        for dk in range(8):
                            op("pe", lambda: P.matmul(bank(k, 16), hT32[:, dk, :], rw[:, dk, :], start=(dk == 0), stop=(dk == 7)),
                               R=[hT32b, cb], W=[psb[k]], inc=(dk == 7))
                        op("act", lambda: A.activation(out=RB[:, 0, i, :], in_=bank(k, 16), func=AF.Sigmoid), R=[psb[k]], W=[rb])
                aff = RB[:, 0]
                sel = RB[:, 1]
                tmp = RB[:, 2]
                msk = RB[:, 3]
                m1 = RS[:, 0]
                m2 = RS[:, 1]
                gs = RS[:, 2]
                goh = RS[:, 3]
                gm = RS[:, 4, :, 0]
                den = RS[:, 5, :, 0]
                rden = RS[:, 5, :, 1]
                g4 = lambda ap: ap.rearrange("p t (g e) -> p (t g) e", e=4)
                f4 = lambda ap: ap.rearrange("p t g -> p (t g)")
                b4 = lambda ap: f4(ap).unsqueeze(2).to_broadcast([128, NT * 4, 4])
                op("dve", lambda: V.tensor_tensor(out=sel, in0=aff, in1=rbias[:].unsqueeze(1).to_broadcast([128, NT, 16]), op=ALU.add),
                   R=[rb, cb], W=[rb])
                op("dve", lambda: V.tensor_reduce(out=f4(m1), in_=g4(sel), op=ALU.max, axis=AX.X), R=[rb], W=[rb])
                op("dve", lambda: V.tensor_tensor(out=g4(tmp), in0=g4(sel), in1=b4(m1), op=ALU.is_ge), R=[rb], W=[rb])
                op("dve", lambda: V.scalar_tensor_tensor(out=tmp, in0=tmp, scalar=-1.0e4, in1=sel, op0=ALU.mult, op1=ALU.add),
                   R=[rb], W=[rb])
                op("dve", lambda: V.tensor_reduce(out=f4(m2), in_=g4(tmp), op=ALU.max, axis=AX.X), R=[rb], W=[rb])
                op("dve", lambda: V.tensor_tensor(out=gs, in0=m1, in1=m2, op=ALU.add), R=[rb], W=[rb])
                op("dve", lambda: V.tensor_reduce(out=gm, in_=gs, op=ALU.max, axis=AX.X), R=[rb], W=[rb])
                op("dve", lambda: V.tensor_tensor(out=goh, in0=gs, in1=gm.unsqueeze(2).to_broadcast([128, NT, 4]), op=ALU.is_ge),
                   R=[rb], W=[rb])
                op("dve", lambda: V.tensor_tensor(out=g4(msk), in0=g4(sel), in1=b4(m2), op=ALU.is_ge), R=[rb], W=[rb])
                op("dve", lambda: V.tensor_tensor(out=g4(msk), in0=g4(msk), in1=b4(goh), op=ALU.mult), R=[rb], W=[rb])
                op("dve", lambda: V.tensor_tensor(out=msk, in0=msk, in1=aff, op=ALU.mult), R=[rb], W=[rb])
                op("dve", lambda: V.tensor_reduce(out=den, in_=msk, op=ALU.add, axis=AX.X), R=[rb], W=[rb])
                op("dve", lambda: V.reciprocal(rden, den), R=[rb], W=[rb])
                op("dve", lambda: V.tensor_tensor(out=comb[:], in0=msk, in1=rden.unsqueeze(2).to_broadcast([128, NT, 16]), op=ALU.mult),
                   R=[rb], W=combb)
                kb.barrier()
            with ExitStack() as t3:
                wg = Ring(kb, t3, "wg", 2, [128, 8, 512], BF16)
                wu = Ring(kb, t3, "wu", 2, [128, 8, 512], BF16)
                wd = Ring(kb, t3, "wd", 2, [128, 4, D], BF16)
                wd2 = Ring(kb, t3, "wd2", 2, [128, 4, D], BF16)
                sg = Ring(kb, t3, "sg", 2, [128, 512], BF16)
                at = Ring(kb, t3, "at", 2, [128, 4, 512], BF16)
                gu = PsRing([0, 1, 2, 3])
                yr = PsRing([4, 5, 6, 7])
                for e in range(16):
                    wgt, wgb = wg.next()
                    wut, wub = wu.next()
                    wdt, wdb = wd.next()
                    wd2t, wd2b = wd2.next()
                    dma("pool", wgt[:], w_gate[l, e].rearrange("(k p) n -> p k n", p=128), W=[wgb])
                    dma("pool", wut[:], w_up[l, e].rearrange("(k p) n -> p k n", p=128), W=[wub])
                    dma("pool", wdt[:], w_down[l, e].rearrange("(k p) n -> p k n", p=128), W=[wdb])
                    for fc in range(4):
                        op("pool", lambda: G.tensor_tensor(out=wd2t[:, fc, :], in0=wdt[:, fc, :], in1=gfb[:], op=ALU.mult),
                           R=[wdb, gfbuf], W=[wd2b])
                    for tg in range(4):
                        att, atb = at.next()
                        toks = slice(tg * 512, (tg + 1) * 512)
                        hb4 = hfTb[tg * 4:(tg + 1) * 4]
                        for fc in range(4):
                            kg = gu.next()
                            ku = gu.next()
                            for dk in range(8):
                                op("pe", lambda: P.matmul(bank(kg), wgt[:, dk, fc * 128:(fc + 1) * 128], hfT[:, dk, toks],
                                                          start=(dk == 0), stop=(dk == 7)), R=[wgb] + hb4, W=[psb[kg]], inc=(dk == 7))
                            for dk in range(8):
                                op("pe", lambda: P.matmul(bank(ku), wut[:, dk, fc * 128:(fc + 1) * 128], hfT[:, dk, toks],
                                                          start=(dk == 0), stop=(dk == 7)), R=[wub] + hb4, W=[psb[ku]], inc=(dk == 7))
                            sgt, sgb = sg.next()
                            op("act", lambda: A.activation(out=sgt[:], in_=bank(kg), func=AF.Silu), R=[psb[kg]], W=[sgb])
                            op("dve", lambda: V.tensor_tensor(out=att[:, fc, :], in0=bank(ku), in1=sgt[:], op=ALU.mult),
                               R=[psb[ku], sgb], W=[atb])
                        for ti in range(4):
                            i = tg * 4 + ti
                            for hf in range(2):
                                ky = yr.next()
                                for fc in range(4):
                                    op("pe", lambda: P.matmul(bank(ky), att[:, fc, ti * 128:(ti + 1) * 128],
                                                              wd2t[:, fc, hf * 512:(hf + 1) * 512], start=(fc == 0), stop=(fc == 3)),
                                       R=[atb, wd2b], W=[psb[ky]], inc=(fc == 3))
                                xs = xres[:, i, hf * 512:(hf + 1) * 512]
                                op("dve", lambda: V.scalar_tensor_tensor(out=xs, in0=bank(ky), scalar=comb[:, i, e:e + 1], in1=xs,
                                                                         op0=ALU.mult, op1=ALU.add),
                                   R=[psb[ky], combb[i], xrb[i]], W=[xrb[i]])
                for i in range(NT):
                    dma("sp", x_dst[i * 128:(i + 1) * 128, :], xres[:, i, :], R=[xrb[i]])
                kb.barrier()

    def sgu_phase(x_src, x_dst):
        l = 1
        with ExitStack() as t:
            wout = kb.sb(t, "swout", [128, 24, D], BF16)
            wsT = kb.sb(t, "swsT", [128, 8, 128], BF16)
            bsT = kb.sb(t, "sbsT", [128, 8], F32)
            ngb = kb.sb(t, "sngb", [128, 3072], BF16)
            gmb = kb.sb(t, "sgmb", [128, D], F32)
            gmbuf = Buf("gmb")
            cb = Buf("sconst")
            for q in range(4):
                dma("pool", wout[:, q * 6:(q + 1) * 6, :],
                    sgu_w_out[q * 768:(q + 1) * 768, :].rearrange("(k p) n -> p k n", p=128), W=[cb])
            dma("sp", bsT[:], sgu_bsT[:, :], W=[cb])
            dma("pool", ngb[:], sgu_ng[0:1, :].partition_broadcast(128), W=[cb])
            G1 = kb.sb(t, "smG1", [128, D], F32)
            SH = kb.sb(t, "smSH", [128, D], F32)
            with ExitStack() as t0:
                wring = Ring(kb, t0, "adaw", 2, [128, 8, 1024], BF16)
                bring = Ring(kb, t0, "adab", 2, [1, 1024], BF16)
                G1_, SH_, mb = make_mod(t0, l, 0, 1, nmix_g[l:l + 1, :], silc, "sm", wring, bring)
                op("pool", lambda: G.tensor_copy(G1[:], G1_[:]), R=[mb], W=[cb])
                op("pool", lambda: G.tensor_copy(SH[:], SH_[:]), R=[mb], W=[cb])
                ada_chunk(l, 2, silc, gmb[:], gmbuf, wring, bring)
                wsf = kb.sb(t0, "wsf", [128, 8, 128], F32)
                wsb = Buf("wsf")
                dma("sp", wsf[:], sgu_ws.rearrange("g i j -> i g j"), W=[wsb])
                for gi in range(8):
                    op("pe", lambda: P.transpose(bank(gi // 4, 128, (gi % 4) * 128), wsf[:, gi, :], identf[:]),
                       R=[wsb, b_const], W=[psb[gi // 4]])
                op("dve", lambda: V.tensor_copy(wsT[:].rearrange("p a b -> p (a b)"), ps[:, 0:1024]), R=[psb[0], psb[1]], W=[cb])
                kb.barrier()
            fr = make_front(t, [0])
            hTblk = kb.sb(t, "shTblk", [128, 8, 512], BF16)
            hTblkb = Buf("hTblk")
            uu = kb.sb(t, "suu", [128, 4, 3072], BF16)
            vv = kb.sb(t, "svv", [128, 4, 3072], BF16)
            uvb = [Buf("uv%d" % i) for i in range(4)]
            wblk = Ring(kb, t, "swblk", 2, [128, 8, 512], BF16)
            bblk = Ring(kb, t, "sbblk", 2, [1, 512], BF16)
            vn = Ring(kb, t, "svn", 2, [128, 3072], BF16)
            gt = Ring(kb, t, "sgt", 1, [128, 3072], BF16)
            gT = Ring(kb, t, "sgT", 1, [128, 24, 128], BF16)
            st = Ring(kb, t, "sst", 2, [128, 4], F32)
            yt = Ring(kb, t, "syt", 1, [128, 512], F32)
            xo = Ring(kb, t, "sxo", 1, [128, D], F32)
            eps = fr["eps"]
            zr = PsRing([1, 2, 3, 4])
            xin = Ring(kb, t, "sxin", 2, [128, D], F32)
            xres_r = Ring(kb, t, "sxres", 2, [128, D], F32)

            def stA(tb, ti):
                i = tb * 4 + ti
                xt, xbuf = xin.next()
                dma("sp", xt[:], x_src[i * 128:(i + 1) * 128, :], W=[xbuf])
                hT, hTb, _, _ = front(xt[:], xbuf, G1[:], SH[:], cb, fr)
                op("pool", lambda: G.tensor_copy(hTblk[:, :, ti * 128:(ti + 1) * 128], hT[:]), R=[hTb], W=[hTblkb])

            def stB(tb):
                for cbk in range(12):
                    wt, wb = wblk.next()
                    bt_, bb = bblk.next()
                    dma("pool", wt[:], sgu_w_in[:, cbk * 512:(cbk + 1) * 512].rearrange("(k p) n -> p k n", p=128), W=[wb])
                    dma("pool", bt_[:], sgu_b_in[0:1, cbk * 512:(cbk + 1) * 512], W=[bb])
                    for ti in range(4):
                        k = zr.next()
                        for dk in range(8):
                            op("pe", lambda: P.matmul(bank(k), hTblk[:, dk, ti * 128:(ti + 1) * 128], wt[:, dk, :],
                                                      start=(dk == 0), stop=False), R=[hTblkb, wb], W=[psb[k]], inc=False)
                        op("pe", lambda: P.matmul(bank(k), ones1[:], bt_[:], start=False, stop=True), R=[bb, b_const], W=[psb[k]])
                        dst = uu[:, ti, cbk * 512:(cbk + 1) * 512] if cbk < 6 else vv[:, ti, (cbk - 6) * 512:(cbk - 5) * 512]
                        op("act", lambda: A.activation(out=dst, in_=bank(k), func=AF.Gelu), R=[psb[k]], W=[uvb[ti]])

            def stC(tb, ti):
                i = tb * 4 + ti
                xr, xrb_ = xres_r.next()
                dma("sp", xr[:], x_src[i * 128:(i + 1) * 128, :], W=[xrb_])
                s_, sbf = st.next()
                vnt, vnb = vn.next()
                op("act", lambda: A.activation(out=vnt[:], in_=vv[:, ti, :], func=AF.Square, accum_out=s_[:, 0:1]),
                   R=[uvb[ti]], W=[vnb, sbf])
                op("act", lambda: A.activation(out=s_[:, 1:2], in_=s_[:, 0:1], func=AF.Sqrt, scale=1.0 / 3072, bias=eps[:, 0:1]),
                   R=[sbf, b_const], W=[sbf])
                op("dve", lambda: V.reciprocal(s_[:, 2:3], s_[:, 1:2]), R=[sbf], W=[sbf])
                op("dve", lambda: V.scalar_tensor_tensor(out=vnt[:], in0=vv[:, ti, :], scalar=s_[:, 2:3], in1=ngb[:],
                                                         op0=ALU.mult, op1=ALU.mult), R=[uvb[ti], sbf, cb], W=[vnb])
                gtt, gtb = gt.next()
                for gi in range(8):
                    k = zr.next()
                    op("pe", lambda: P.matmul(bank(k, 384), wsT[:, gi, :], vnt[:, gi * 384:(gi + 1) * 384], start=True, stop=True),
                       R=[vnb, cb], W=[psb[k]])
                    op("dve", lambda: V.scalar_tensor_tensor(out=gtt[:, gi * 384:(gi + 1) * 384], in0=bank(k, 384),
                                                             scalar=bsT[:, gi:gi + 1], in1=uu[:, ti, gi * 384:(gi + 1) * 384],
                                                             op0=ALU.add, op1=ALU.mult), R=[psb[k], cb, uvb[ti]], W=[gtb])
                gTt, gTb = gT.next()
                for q in range(3):
                    k = zr.next()
                    for c8 in range(8):
                        kc = q * 8 + c8
                        op("pe", lambda: P.transpose(bank_bf(k, 128, c8 * 128), gtt[:, kc * 128:(kc + 1) * 128], ident[:]),
                           R=[gtb, b_const], W=[psb[k]], inc=(c8 == 7))
                    op("act", lambda: A.copy(out=gTt[:, q * 8:(q + 1) * 8, :].rearrange("p a b -> p (a b)"), in_=bank_bf(k)),
                       R=[psb[k]], W=[gTb])
                xot, xob = xo.next()
                for hf in range(2):
                    k = 5 + hf
                    for kc in range(24):
                        op("pe", lambda: P.matmul(bank(k), gTt[:, kc, :], wout[:, kc, hf * 512:(hf + 1) * 512],
                                                  start=(kc == 0), stop=(kc == 23)), R=[gTb, cb], W=[psb[k]], inc=(kc == 23))
                    ytt, ytb = yt.next()
                    op("dve", lambda: V.tensor_tensor(out=ytt[:], in0=bank(k), in1=gmb[:, hf * 512:(hf + 1) * 512], op=ALU.mult),
                       R=[psb[k], gmbuf], W=[ytb])
                    op("pool", lambda: G.tensor_tensor(out=xot[:, hf * 512:(hf + 1) * 512], in0=ytt[:],
                                                       in1=xr[:, hf * 512:(hf + 1) * 512], op=ALU.add),
                       R=[ytb, xrb_], W=[xob])
                dma("sp", x_dst[i * 128:(i + 1) * 128, :], xot[:], R=[xob])

            for ti in range(4):
                stA(0, ti)
            for tb in range(4):
                stB(tb)
                for ti in range(4):
                    if tb + 1 < 4:
                        stA(tb + 1, ti)
                    stC(tb, ti)
            kb.barrier()


    def mixer0_phase(x_dst):
        l = 0
        import os
        MS = os.environ.get("MIX_STOP", "")
        b3 = lambda ap, n: ap.unsqueeze(2).to_broadcast([128, n, 64])
        h3 = lambda ap: ap.rearrange("p (h e) -> p h e", e=64)
        with ExitStack() as t:
            G1m = kb.sb(t, "G1m", [128, D], F32)
            SHm = kb.sb(t, "SHm", [128, D], F32)
            gmb = kb.sb(t, "gmb", [128, D], F32)
            mbuf = Buf("modm")
            lgb = kb.sb(t, "lgb", [128, 16], F32)
            hgb = kb.sb(t, "hgb", [128, 192], F32)
            eps = kb.sb(t, "eps0", [128, 1], F32)
            one = kb.sb(t, "one0", [128, 1], F32)
            ret_out = kb.sb(t, "ret_out", [128, NT, 512], BF16)
            retb = [Buf("ret%d" % i) for i in range(NT)]
            KcT = kb.sb(t, "KcT", [128, 4, 256], BF16)
            Vc = kb.sb(t, "Vc", [128, 2, 512], BF16)
            ctxb = Buf("ctxkv")
            tb_ = Buf("tables")
            retg = hgb[:, 0:64]
            qg = hgb[:, 64:128]
            kg = hgb[:, 128:192]
            op("dve", lambda: V.memset(eps[:], EPS), W=[tb_])
            op("dve", lambda: V.memset(one[:], 1.0), W=[tb_])
            dma("sp", hgb[:], hg[0:1, :].partition_broadcast(128), W=[tb_])
            dma("sp", lgb[:], ret_decay[0:1, :].partition_broadcast(128), W=[tb_])
            op("dve", lambda: V.tensor_scalar(out=qg, in0=qg, scalar1=0.125, scalar2=None, op0=ALU.mult), R=[tb_], W=[tb_])
            op("act", lambda: A.activation(out=lgb[:], in_=lgb[:], func=AF.Exp), R=[tb_], W=[tb_])
            op("act", lambda: A.activation(out=lgb[:], in_=lgb[:], func=AF.Ln, bias=one[:, 0:1]), R=[tb_], W=[tb_])
            op("dve", lambda: V.tensor_scalar(out=lgb[:], in0=lgb[:], scalar1=-1.0, scalar2=None, op0=ALU.mult), R=[tb_], W=[tb_])
            with ExitStack() as t0:
                wring = Ring(kb, t0, "adaw", 2, [128, 8, 1024], BF16)
                bring = Ring(kb, t0, "adab", 2, [1, 1024], BF16)
                G1_, SH_, mb_ = make_mod(t0, l, 0, 1, nmix_g[l:l + 1, :], silc, "mm", wring, bring)
                op("pool", lambda: G.tensor_copy(G1m[:], G1_[:]), R=[mb_], W=[mbuf])
                op("pool", lambda: G.tensor_copy(SHm[:], SH_[:]), R=[mb_], W=[mbuf])
                ada_chunk(l, 2, silc, gmb[:], mbuf, wring, bring)
                kb.barrier()

            def norm_heads_T(src_bank, gain, tmps, dstK, dstKb):
                sqt, sqb = tmps["sq"].next()
                s8, s8b = tmps["s8"].next()
                op("act", lambda: A.activation(out=sqt[:], in_=bank(src_bank), func=AF.Square), R=[psb[src_bank]], W=[sqb])
                op("dve", lambda: V.tensor_reduce(out=s8[:, 0:8], in_=h3(sqt[:]), op=ALU.add, axis=AX.X), R=[sqb], W=[s8b])
                op("act", lambda: A.activation(out=s8[:, 8:16], in_=s8[:, 0:8], func=AF.Sqrt, scale=1.0 / 64, bias=eps[:, 0:1]),
                   R=[s8b, tb_], W=[s8b])
                op("dve", lambda: V.reciprocal(s8[:, 16:24], s8[:, 8:16]), R=[s8b], W=[s8b])
                op("dve", lambda: V.tensor_tensor(out=h3(sqt[:]), in0=h3(bank(src_bank)), in1=b3(s8[:, 16:24], 8), op=ALU.mult),
                   R=[psb[src_bank], s8b], W=[sqb])
                knb, knbb = tmps["knb"].next()
                op("pool", lambda: G.tensor_tensor(out=h3(knb[:]), in0=h3(sqt[:]), in1=gain.unsqueeze(1).to_broadcast([128, 8, 64]),
                                                   op=ALU.mult), R=[sqb, tb_], W=[knbb])
                k = tmps["ps"].next()
                for hp in range(4):
                    op("pe", lambda: P.transpose(bank_bf(k, 128, hp * 128), knb[:, hp * 128:(hp + 1) * 128], ident[:]),
                       R=[knbb, b_const], W=[psb[k]], inc=(hp == 3))
                op("dve", lambda: V.tensor_copy(dstK, bank_bf(k, 512).rearrange("p (a b) -> p a b", b=128)), R=[psb[k]], W=[dstKb])

            def make_tmps(tt, ps_banks):
                return {"sq": Ring(kb, tt, "nsq", 2, [128, 512], F32), "s8": Ring(kb, tt, "ns8", 2, [128, 24], F32),
                        "knb": Ring(kb, tt, "nknb", 2, [128, 512], BF16), "ps": PsRing(ps_banks)}

            def proj(hT, hTb, w, wb, c0, k):
                for dk in range(8):
                    op("pe", lambda: P.matmul(bank(k), hT[:, dk, :], w[:, dk, c0:c0 + 512], start=(dk == 0), stop=(dk == 7)),
                       R=[hTb, wb], W=[psb[k]], inc=(dk == 7))

            def rope(src, srcbufs, Ct, St, tabb, t1, t1b, t2, t2b):
                s5 = lambda ap: ap.rearrange("p (h b f q) -> p h b f q", h=8, b=2, f=2, q=16)
                S4 = St.rearrange("p (b f q) -> p b f q", b=2, f=2, q=16)
                op("dve", lambda: V.tensor_tensor(out=h3(t1), in0=h3(src), in1=Ct.unsqueeze(1).to_broadcast([128, 8, 64]), op=ALU.mult),
                   R=srcbufs + [tabb], W=[t1b])
                for f in range(2):
                    op("dve", lambda: V.tensor_tensor(out=s5(t2)[:, :, :, f, :], in0=s5(src)[:, :, :, 1 - f, :],
                                                      in1=S4[:, :, f, :].unsqueeze(1).to_broadcast([128, 8, 2, 16]), op=ALU.mult),
                       R=srcbufs + [tabb], W=[t2b])

            if MS == "tables":
                return
            with ExitStack() as tR:
                cstt = kb.sb(tR, "cstt", [128, 260], F32)
                MT = kb.sb(tR, "MT", [128, 8, 128], F32)
                dec = kb.sb(tR, "dec", [128, 4, 8], F32)
                CD = kb.sb(tR, "CD", [128, 2, 4], F32)
                ei = kb.sb(tR, "ei", [128, 36], F32)
                Sinit = kb.sb(tR, "Sinit", [128, 2, 256], F32)
                sib = Buf("Sinit")
                G1c = kb.sb(tR, "G1c", [128, D], F32)
                SHc = kb.sb(tR, "SHc", [128, D], F32)
                cbuf = Buf("modc")
                wq = kb.sb(tR, "w_qkvg", [128, 8, 2048], BF16)
                wqb = Buf("wq")
                wn = kb.sb(tR, "w_nkv", [128, 8, 1024], BF16)
                wnb = Buf("wn")
                DBt = kb.sb(tR, "DBt", [128, NT, 256], BF16)
                SBst = kb.sb(tR, "SBst", [128, NT, 256], BF16)
                dbb = [Buf("db%d" % i) for i in range(NT)]
                sbb = [Buf("sb%d" % i) for i in range(NT)]
                for q in range(4):
                    dma("pool", wq[:, :, q * 512:(q + 1) * 512], ab_w_in[:, q * 512:(q + 1) * 512].rearrange("(k p) n -> p k n", p=128), W=[wqb])
                for q in range(2):
                    dma("pool", wn[:, :, q * 512:(q + 1) * 512],
                        ab_w_in[:, 2560 + q * 512:2560 + (q + 1) * 512].rearrange("(k p) n -> p k n", p=128), W=[wnb])
                dma("sp", cstt[:], cst[:, :], W=[tb_])
                dma("sp", ei[:], exp_init[:, :], W=[tb_])
                DFt = cstt[:, 0:128]
                DBe = cstt[:, 128:256]
                with ExitStack() as t0:
                    wring = Ring(kb, t0, "adaw", 2, [128, 8, 1024], BF16)
                    bring = Ring(kb, t0, "adab", 2, [1, 1024], BF16)
                    G1_, SH_, mb_ = make_mod(t0, l, 0, 1, nmix_g[l:l + 1, :], silcc, "mc", wring, bring)
                    op("pool", lambda: G.tensor_copy(G1c[:], G1_[:]), R=[mb_], W=[cbuf])
                    op("pool", lambda: G.tensor_copy(SHc[:], SH_[:]), R=[mb_], W=[cbuf])
                    tmpM = kb.sb(t0, "tmpM", [128, 128], F32)
                    tmb = Buf("tmpM")
                    for h in range(8):
                        op("act", lambda: A.activation(out=MT[:, h, :], in_=DFt, func=AF.Exp, scale=lgb[:, h:h + 1]), R=[tb_], W=[tb_])
                        op("act", lambda: A.activation(out=tmpM[:], in_=DBe, func=AF.Exp, scale=lgb[:, 8 + h:9 + h]), R=[tb_], W=[tmb])
                        op("dve", lambda: V.tensor_tensor(out=MT[:, h, :], in0=MT[:, h, :], in1=tmpM[:], op=ALU.add), R=[tb_, tmb], W=[tb_])
                    for j, (c0, pc) in enumerate(((0, 256), (8, 257), (0, 258), (8, 259))):
                        op("act", lambda: A.activation(out=dec[:, j, :], in_=lgb[:, c0:c0 + 8], func=AF.Exp, scale=cstt[:, pc:pc + 1]),
                           R=[tb_], W=[tb_])
                    lg4 = lgb[:].rearrange("p (d q h) -> p d q h", d=2, q=4, h=2)
                    for hh in range(2):
                        ps_ = slice(hh * 64, (hh + 1) * 64)
                        op("act", lambda: A.activation(out=CD[ps_, :, :], in_=lg4[ps_, :, :, hh], func=AF.Exp, scale=128.0), R=[tb_], W=[tb_])
                    kb.barrier()
                if MS == "tables2":
                    dma("sp", x_dst[0:128, 0:1024], MT[:].rearrange("p a b -> p (a b)"), R=[tb_])
                    dma("sp", x_dst[128:256, 0:32], dec[:].rearrange("p a b -> p (a b)"), R=[tb_])
                    dma("sp", x_dst[128:256, 32:40], CD[:].rearrange("p a b -> p (a b)"), R=[tb_])
                    dma("sp", x_dst[128:256, 64:80], lgb[:], R=[tb_])
                    kb.barrier()
                    return
                fr = make_front(tR, [0])
                xin = Ring(kb, tR, "rxin", 2, [128, D], F32)
                ropt = Ring(kb, tR, "ropt", 2, [128, 256], F32)
                t1r = Ring(kb, tR, "t1r", 1, [128, 1024], F32)
                t2r = Ring(kb, tR, "t2r", 1, [128, 1024], F32)
                krr = Ring(kb, tR, "krr", 1, [128, 512], F32)
                wtr = Ring(kb, tR, "wtr", 2, [128, 2, 8], F32)
                kfr = Ring(kb, tR, "kfr", 2, [128, 2, 512], BF16)
                vbr = Ring(kb, tR, "vbr", 2, [128, 512], BF16)
                tmps = make_tmps(tR, [5])

                NO = 18
                def headO(i):
                    isctx = i >= 16
                    xt, xbuf = xin.next()
                    src = x_ctx[(i - 16) * 128:(i - 15) * 128, :] if isctx else x_oth[i * 128:(i + 1) * 128, :]
                    dma("sp", xt[:], src, W=[xbuf])
                    rt, rtb = ropt.next()
                    dma("sp", rt[:, 0:128], rope_oth[i * 128:(i + 1) * 128, :], W=[rtb])
                    fa = front_a(xt[:], xbuf, (G1c if isctx else G1m)[:], (SHc if isctx else SHm)[:], cbuf if isctx else mbuf, fr)
                    return xt, xbuf, rt, rtb, fa

                def bodyO(i, hd):
                    isctx = i >= 16
                    xt, xbuf, rt, rtb, fa = hd
                    hT, hTb, _, _ = front_b(fa, fr)
                    proj(hT, hTb, wq, wqb, 512, 1)
                    proj(hT, hTb, wq, wqb, 1024, 2)
                    t1, t1b = t1r.next()
                    t2, t2b = t2r.next()
                    rope(bank(1), [psb[1]], rt[:, 0:64], rt[:, 64:128], rtb, t1[:, 0:512], t1b, t2[:, 0:512], t2b)
                    kr, krb = krr.next()
                    op("pool", lambda: G.tensor_tensor(out=kr[:], in0=t1[:, 0:512], in1=t2[:, 0:512], op=ALU.add), R=[t1b, t2b], W=[krb])
                    wt, wtb = wtr.next()
                    for d_ in range(2):
                        op("act", lambda: A.activation(out=wt[:, d_, :], in_=lgb[:, d_ * 8:(d_ + 1) * 8], func=AF.Exp,
                                                       scale=ei[:, d_ * 18 + i:d_ * 18 + i + 1]), R=[tb_], W=[wtb])
                    kf, kfb = kfr.next()
                    for d_ in range(2):
                        op("pool", lambda: G.tensor_tensor(out=h3(kf[:, d_, :]), in0=h3(kr[:]), in1=b3(wt[:, d_, :], 8), op=ALU.mult),
                           R=[krb, wtb], W=[kfb])
                    vb, vbb = vbr.next()
                    op("act", lambda: A.copy(out=vb[:], in_=bank(2)), R=[psb[2]], W=[vbb])
                    for d_ in range(2):
                        kk = 6 + d_
                        for h in range(8):
                            hp, hh = h // 2, h % 2
                            op("pe", lambda: P.matmul(ps[hh * 64:(hh + 1) * 64, kk * 512 + hp * 64:kk * 512 + (hp + 1) * 64],
                                                      kf[:, d_, h * 64:(h + 1) * 64], vb[:, h * 64:(h + 1) * 64],
                                                      start=(i == 0 and h < 2), stop=(i == NO - 1), skip_group_check=True),
                               R=[kfb, vbb], W=[psb[kk]], inc=(h == 7))
                    if isctx:
                        proj(hT, hTb, wn, wnb, 0, 3)
                        proj(hT, hTb, wn, wnb, 512, 4)
                        norm_heads_T(3, kg, tmps, KcT[:, :, (i - 16) * 128:(i - 15) * 128], ctxb)
                        op("act", lambda: A.copy(out=Vc[:, i - 16, :], in_=bank(4)), R=[psb[4]], W=[ctxb])
                pipelined(NO, headO, bodyO)
                for d_ in range(2):
                    op("act", lambda: A.copy(out=Sinit[:, d_, :], in_=bank(6 + d_, 256)), R=[psb[6 + d_]], W=[sib])
                if MS == "sweepO":
                    dma("sp", x_dst[0:128, 0:512], Sinit[:].rearrange("p a b -> p (a b)"), R=[sib])
                    dma("sp", x_dst[128:256, 0:512].bitcast(BF16), KcT[:].rearrange("p a b -> p (a b)"), R=[ctxb])
                    dma("sp", x_dst[256:384, 0:512].bitcast(BF16), Vc[:].rearrange("p a b -> p (a b)"), R=[ctxb])
                    kb.barrier()
                    return

                def headL(c):
                    xt, xbuf = xin.next()
                    dma("sp", xt[:], x_loc[c * 128:(c + 1) * 128, :], W=[xbuf])
                    rt, rtb = ropt.next()
                    dma("sp", rt[:], rope_loc[c * 128:(c + 1) * 128, :], W=[rtb])
                    fa = front_a(xt[:], xbuf, G1m[:], SHm[:], mbuf, fr)
                    return xt, xbuf, rt, rtb, fa

                def bodyE1(c, hd):
                    xt, xbuf, rt, rtb, fa = hd
                    hT, hTb, _, _ = front_b(fa, fr)
                    proj(hT, hTb, wq, wqb, 512, 1)
                    proj(hT, hTb, wq, wqb, 1024, 2)
                    t1, t1b = t1r.next()
                    t2, t2b = t2r.next()
                    rope(bank(1), [psb[1]], rt[:, 64:128], rt[:, 192:256], rtb, t1[:, 0:512], t1b, t2[:, 0:512], t2b)
                    kr, krb = krr.next()
                    op("pool", lambda: G.tensor_tensor(out=kr[:], in0=t1[:, 0:512], in1=t2[:, 0:512], op=ALU.add), R=[t1b, t2b], W=[krb])
                    kf, kfb = kfr.next()
                    op("pool", lambda: G.tensor_tensor(out=h3(kf[:, 0, :]), in0=h3(kr[:]), in1=b3(dec[:, 3, :], 8), op=ALU.mult),
                       R=[krb, tb_], W=[kfb])
                    vb, vbb = vbr.next()
                    op("act", lambda: A.copy(out=vb[:], in_=bank(2)), R=[psb[2]], W=[vbb])
                    kk = 3 + (c % 2)
                    for h in range(8):
                        hp, hh = h // 2, h % 2
                        op("pe", lambda: P.matmul(ps[hh * 64:(hh + 1) * 64, kk * 512 + hp * 64:kk * 512 + (hp + 1) * 64],
                                                  kf[:, 0, h * 64:(h + 1) * 64], vb[:, h * 64:(h + 1) * 64], start=True, stop=True),
                           R=[kfb, vbb], W=[psb[kk]], inc=(h == 7))
                    op("act", lambda: A.copy(out=DBt[:, c, :], in_=bank(kk, 256)), R=[psb[kk]], W=[dbb[c]])
                pipelined(NT, headL, bodyE1)
                srun = Ring(kb, tR, "srun", 2, [128, 256], F32)
                stmp = Ring(kb, tR, "stmp", 2, [128, 256], F32)
                q4 = lambda ap: ap.rearrange("p (q e) -> p q e", e=64)
                cdb = lambda d_: CD[:, d_, :].unsqueeze(2).to_broadcast([128, 4, 64])
                cur, curb = srun.next()
                op("dve", lambda: V.tensor_copy(cur[:], Sinit[:, 1, :]), R=[sib], W=[curb])
                for c in range(NT - 1, -1, -1):
                    op("act", lambda: A.copy(out=SBst[:, c, :], in_=cur[:]), R=[curb], W=[sbb[c]])
                    if c == 0:
                        break
                    tm, tmb_ = stmp.next()
                    op("pool", lambda: G.tensor_tensor(out=q4(tm[:]), in0=q4(cur[:]), in1=cdb(1), op=ALU.mult), R=[curb, tb_], W=[tmb_])
                    nxt, nxtb = srun.next()
                    op("dve", lambda: V.tensor_tensor(out=nxt[:], in0=tm[:], in1=DBt[:, c, :], op=ALU.add), R=[tmb_, dbb[c]], W=[nxtb])
                    cur, curb = nxt, nxtb
                if MS == "sweepE1":
                    for c in range(NT):
                        dma("sp", x_dst[c * 128:(c + 1) * 128, 0:128].bitcast(BF16), SBst[:, c, :], R=[sbb[c]])
                    kb.barrier()
                    return

                qkr_r = Ring(kb, tR, "qkr", 2, [128, 1024], BF16)
                qkT_r = Ring(kb, tR, "qkT", 2, [128, 8, 128], BF16)
                qz_r = Ring(kb, tR, "qz", 2, [128, 2, 4, 128], BF16)
                for qi_ in range(2):
                    op("pool", lambda: G.memset(qz_r.t[qi_][:], 0.0), W=[qz_r.b[qi_]])
                gs_r = Ring(kb, tR, "gsr", 2, [128, 512], BF16)
                gs2_r = Ring(kb, tR, "gs2r", 2, [128, 512], BF16)
                Pm_r = Ring(kb, tR, "Pmr", 1, [128, 8, 128], BF16)
                o_r = Ring(kb, tR, "or", 2, [128, 512], F32)
                s8r = Ring(kb, tR, "rs8", 2, [128, 24], F32)
                SFr = Ring(kb, tR, "SFr", 2, [128, 256], F32)
                SFbr = Ring(kb, tR, "SFbr", 2, [128, 256], BF16)
                SF, SFb_ = SFr.next()
                op("dve", lambda: V.tensor_copy(SF[:], Sinit[:, 0, :]), R=[sib], W=[SFb_])
                SFh, SFhb = SFbr.next()
                op("act", lambda: A.copy(out=SFh[:], in_=SF[:]), R=[SFb_], W=[SFhb])
                RS_ = {"SF": SF, "SFb": SFb_, "SFh": SFh, "SFhb": SFhb}

                def bodyR(c, hd):
                    xt, xbuf, rt, rtb, fa = hd
                    SF, SFb_, SFh, SFhb = RS_["SF"], RS_["SFb"], RS_["SFh"], RS_["SFhb"]
                    hT, hTb, _, _ = front_b(fa, fr)
                    for j in range(4):
                        proj(hT, hTb, wq, wqb, j * 512, 1 + j)
                    t1, t1b = t1r.next()
                    t2, t2b = t2r.next()
                    for j in range(2):
                        rope(bank(1 + j), [psb[1 + j]], rt[:, j * 64:(j + 1) * 64], rt[:, 128 + j * 64:128 + (j + 1) * 64], rtb,
                             t1[:, j * 512:(j + 1) * 512], t1b, t2[:, j * 512:(j + 1) * 512], t2b)
                    qkr, qkrb = qkr_r.next()
                    op("pool", lambda: G.tensor_tensor(out=qkr[:], in0=t1[:], in1=t2[:], op=ALU.add), R=[t1b, t2b], W=[qkrb])
                    vb, vbb = vbr.next()
                    op("act", lambda: A.copy(out=vb[:], in_=bank(3)), R=[psb[3]], W=[vbb])
                    gs, gsb = gs_r.next()
                    op("act", lambda: A.activation(out=gs[:], in_=bank(4), func=AF.Silu), R=[psb[4]], W=[gsb])
                    gs2, gs2b = gs2_r.next()
                    op("pool", lambda: G.tensor_tensor(out=h3(gs2[:]), in0=h3(gs[:]), in1=retg.unsqueeze(1).to_broadcast([128, 8, 64]),
                                                       op=ALU.mult), R=[gsb, tb_], W=[gs2b])
                    kf, kfb = kfr.next()
                    op("pool", lambda: G.tensor_tensor(out=h3(kf[:, 0, :]), in0=h3(qkr[:, 512:1024]), in1=b3(dec[:, 2, :], 8), op=ALU.mult),
                       R=[qkrb, tb_], W=[kfb])
                    qkT, qkTb = qkT_r.next()
                    for j in range(8):
                        op("pe", lambda: P.transpose(bank_bf(0, 128, j * 128), qkr[:, j * 128:(j + 1) * 128], ident[:]),
                           R=[qkrb, b_const], W=[psb[0]], inc=(j == 7))
                    op("dve", lambda: V.tensor_copy(qkT[:].rearrange("p a b -> p (a b)"), bank_bf(0)), R=[psb[0]], W=[qkTb])
                    qz, qzb = qz_r.next()
                    for hh in range(2):
                        pr = slice(hh * 64, (hh + 1) * 64)
                        op("pool", lambda: G.tensor_copy(qz[pr, hh, :, :], qkT[pr, 0:4, :]), R=[qkTb], W=[qzb])
                    for h in range(8):
                        hp, hh = h // 2, h % 2
                        op("pe", lambda: P.matmul(bank(5 + h // 4, 128, (h % 4) * 128), qkT[:, 4 + hp, :], qz[:, hh, hp, :], start=True, stop=True),
                           R=[qkTb, qzb], W=[psb[5 + h // 4]], inc=(h % 4 == 3))
                    Pm, Pmb = Pm_r.next()
                    for b_ in range(2):
                        op("dve", lambda: V.tensor_tensor(out=Pm[:, 4 * b_:4 * b_ + 4, :], in0=bank(5 + b_).rearrange("p (a b) -> p a b", b=128),
                                                          in1=MT[:, 4 * b_:4 * b_ + 4, :], op=ALU.mult), R=[psb[5 + b_], tb_], W=[Pmb])
                    for h in range(8):
                        op("pe", lambda: P.matmul(bank(7, 64, h * 64), Pm[:, h, :], vb[:, h * 64:(h + 1) * 64], start=True, stop=True),
                           R=[Pmb, vbb], W=[psb[7]], inc=(h == 7))
                    for h in range(8):
                        hp, hh = h // 2, h % 2
                        op("pe", lambda: P.matmul(bank(3, 64, h * 64), qz[:, hh, hp, :], SFh[:, hp * 64:(hp + 1) * 64], start=True, stop=True),
                           R=[qzb, SFhb], W=[psb[3]], inc=(h == 7))
                    for h in range(8):
                        hp, hh = h // 2, h % 2
                        op("pe", lambda: P.matmul(bank(4, 64, h * 64), qz[:, hh, hp, :], SBst[:, c, hp * 64:(hp + 1) * 64], start=True, stop=True),
                           R=[qzb, sbb[c]], W=[psb[4]], inc=(h == 7))
                    o1, o1b = o_r.next()
                    o2, o2b = o_r.next()
                    op("dve", lambda: V.tensor_tensor(out=h3(o1[:]), in0=h3(bank(3)), in1=b3(dec[:, 0, :], 8), op=ALU.mult), R=[psb[3], tb_], W=[o1b])
                    op("dve", lambda: V.tensor_tensor(out=h3(o2[:]), in0=h3(bank(4)), in1=b3(dec[:, 1, :], 8), op=ALU.mult), R=[psb[4], tb_], W=[o2b])
                    op("dve", lambda: V.tensor_tensor(out=o1[:], in0=bank(7), in1=o1[:], op=ALU.add), R=[psb[7], o1b], W=[o1b])
                    op("pool", lambda: G.tensor_tensor(out=o1[:], in0=o1[:], in1=o2[:], op=ALU.add), R=[o1b, o2b], W=[o1b])
                    op("pool", lambda: G.tensor_tensor(out=o2[:], in0=o1[:], in1=o1[:], op=ALU.mult), R=[o1b], W=[o2b])
                    s8, s8b = s8r.next()
                    op("dve", lambda: V.tensor_reduce(out=s8[:, 0:8], in_=h3(o2[:]), op=ALU.add, axis=AX.X), R=[o2b], W=[s8b])
                    op("act", lambda: A.activation(out=s8[:, 8:16], in_=s8[:, 0:8], func=AF.Sqrt, scale=1.0 / 64, bias=eps[:, 0:1]),
                       R=[s8b, tb_], W=[s8b])
                    op("dve", lambda: V.reciprocal(s8[:, 16:24], s8[:, 8:16]), R=[s8b], W=[s8b])
                    op("pool", lambda: G.tensor_tensor(out=h3(o2[:]), in0=h3(o1[:]), in1=b3(s8[:, 16:24], 8), op=ALU.mult), R=[o1b, s8b], W=[o2b])
                    op("pool", lambda: G.tensor_tensor(out=ret_out[:, c, :], in0=o2[:], in1=gs2[:], op=ALU.mult), R=[o2b, gs2b], W=[retb[c]])
                    if c < NT - 1:
                        for h in range(8):
                            hp, hh = h // 2, h % 2
                            op("pe", lambda: P.matmul(ps[hh * 64:(hh + 1) * 64, 1 * 512 + hp * 64:1 * 512 + (hp + 1) * 64],
                                                      kf[:, 0, h * 64:(h + 1) * 64], vb[:, h * 64:(h + 1) * 64], start=True, stop=True),
                               R=[kfb, vbb], W=[psb[1]], inc=(h == 7))
                        tm, tmb_ = stmp.next()
                        op("pool", lambda: G.tensor_tensor(out=q4(tm[:]), in0=q4(SF[:]), in1=cdb(0), op=ALU.mult), R=[SFb_, tb_], W=[tmb_])
                        SF, SFb_ = SFr.next()
                        op("dve", lambda: V.tensor_tensor(out=SF[:], in0=bank(1, 256), in1=tm[:], op=ALU.add), R=[psb[1], tmb_], W=[SFb_])
                        SFh, SFhb = SFbr.next()
                        op("act", lambda: A.copy(out=SFh[:], in_=SF[:]), R=[SFb_], W=[SFhb])
                        RS_.update({"SF": SF, "SFb": SFb_, "SFh": SFh, "SFhb": SFhb})
                pipelined(NT, headL, bodyR)
                kb.barrier()
            if MS == "sweepR":
                for c in range(NT):
                    dma("sp", x_dst[c * 128:(c + 1) * 128, 0:256].bitcast(BF16), ret_out[:, c, :], R=[retb[c]])
                kb.barrier()
                return

            with ExitStack() as tN:
                wn3 = kb.sb(tN, "w_n3", [128, 8, 1536], BF16)
                wn3b = Buf("wn3")
                wo = kb.sb(tN, "w_o", [128, 8, D], BF16)
                wob = Buf("wo")
                KT = kb.sb(tN, "KT", [128, 4, 20 * 128], BF16)
                VE = kb.sb(tN, "VE", [128, 20, 512], BF16)
                kvb = [Buf("kv%d" % i) for i in range(20)]
                biasT = kb.sb(tN, "biasT", [128, 8, 896], BF16)
                rowm = kb.sb(tN, "rowm", [2, 192], BF16)
                A2 = kb.sb(tN, "A2", [2, 128], BF16)
                nb_ = Buf("naconst")
                for q in range(3):
                    dma("pool", wn3[:, :, q * 512:(q + 1) * 512],
                        ab_w_in[:, 2048 + q * 512:2048 + (q + 1) * 512].rearrange("(k p) n -> p k n", p=128), W=[wn3b])
                for q in range(2):
                    dma("pool", wo[:, :, q * 512:(q + 1) * 512], ab_w_out[:, q * 512:(q + 1) * 512].rearrange("(k p) n -> p k n", p=128), W=[wob])
                for h in range(8):
                    dma("pool", biasT[:, h, :], bias_tab[h, :, :], W=[nb_])
                dma("pool", rowm[:], rowmask[:, :], W=[nb_])
                dma("pool", A2[:], a2_d[:, :], W=[nb_])
                fr = make_front(tN, [0])
                xin = Ring(kb, tN, "nxin", 2, [128, D], F32)
                tmps = make_tmps(tN, [3])
                def headE2(e_):
                    xt, xbuf = xin.next()
                    if e_ < 2:
                        src = x_halo[e_ * 128:(e_ + 1) * 128, :]
                    elif e_ < 18:
                        src = x_loc[(e_ - 2) * 128:(e_ - 1) * 128, :]
                    else:
                        src = x_halo[256 + (e_ - 18) * 128:256 + (e_ - 17) * 128, :]
                    dma("sp", xt[:], src, W=[xbuf])
                    return xt, xbuf, front_a(xt[:], xbuf, G1m[:], SHm[:], mbuf, fr)

                def bodyE2(e_, hd):
                    xt, xbuf, fa = hd
                    hT, hTb, _, _ = front_b(fa, fr)
                    proj(hT, hTb, wn3, wn3b, 512, 1)
                    proj(hT, hTb, wn3, wn3b, 1024, 2)
                    norm_heads_T(1, kg, tmps, KT[:, :, e_ * 128:(e_ + 1) * 128], kvb[e_])
                    op("act", lambda: A.copy(out=VE[:, e_, :], in_=bank(2)), R=[psb[2]], W=[kvb[e_]])
                pipelined(20, headE2, bodyE2)
                if MS == "sweepE2":
                    for e_ in range(16):
                        dma("sp", x_dst[e_ * 128:(e_ + 1) * 128, 0:256].bitcast(BF16), KT[:, :, e_ * 128:(e_ + 1) * 128], R=[kvb[e_]])
                        dma("sp", x_dst[e_ * 128:(e_ + 1) * 128, 256:512].bitcast(BF16), VE[:, e_, :], R=[kvb[e_]])
                    kb.barrier()
                    return
                qnT_r = Ring(kb, tN, "qnT", 2, [128, 4, 128], BF16)
                qnz_r = Ring(kb, tN, "qnz", 2, [128, 2, 4, 128], BF16)
                for qi_ in range(2):
                    op("pool", lambda: G.memset(qnz_r.t[qi_][:], 0.0), W=[qnz_r.b[qi_]])
                Pe_r = Ring(kb, tN, "Pe", 2, [128, 1024], BF16)
                PT_r = Ring(kb, tN, "PT", 2, [128, 8, 128], BF16)
                sm_r = Ring(kb, tN, "sm", 2, [128, 24], F32)
                mix_r = Ring(kb, tN, "mix", 2, [128, D], BF16)
                mixT_r = Ring(kb, tN, "mixT", 2, [128, 8, 128], BF16)
                yt_r = Ring(kb, tN, "nyt", 2, [128, 512], F32)
                xo_r = Ring(kb, tN, "nxo", 2, [128, D], F32)
                ab_ring = PsRing([2, 4])
                def headN(t_):
                    xt, xbuf = xin.next()
                    dma("sp", xt[:], x_loc[t_ * 128:(t_ + 1) * 128, :], W=[xbuf])
                    return xt, xbuf, front_a(xt[:], xbuf, G1m[:], SHm[:], mbuf, fr)

                def geomN(t_):
                    lo, hi = t_ - 2, t_ + 2
                    if t_ < 2:
                        hi = 3
                    if t_ > 13:
                        lo = 12
                    n = hi - lo + 1
                    return lo, hi, n, (n - 4) * 128

                def stA(t_, hd):
                    xt, xbuf, fa = hd
                    hT, hTb, _, _ = front_b(fa, fr)
                    proj(hT, hTb, wn3, wn3b, 0, 1)
                    qnT, qnTb = qnT_r.next()
                    tmps["ps"] = PsRing([6])
                    norm_heads_T(1, qg, tmps, qnT[:], qnTb)
                    qnz, qnzb = qnz_r.next()
                    for hh in range(2):
                        pr = slice(hh * 64, (hh + 1) * 64)
                        op("pool", lambda: G.tensor_copy(qnz[pr, hh, :, :], qnT[pr, :, :]), R=[qnTb], W=[qnzb])
                    sm, smb = sm_r.next()
                    return xt, xbuf, qnz, qnzb, sm, smb

                def stS(t_, cx, h):
                    xt, xbuf, qnz, qnzb, sm, smb = cx
                    lo, hi, n, nbk = geomN(t_)
                    kv_need = [kvb[j + 2] for j in range(lo, hi + 1)]
                    hp, hh = h // 2, h % 2
                    ka = ab_ring.next()
                    kbk = ka + 1
                    e0 = (lo + 2) * 128
                    d0 = (lo - t_ + 3) * 128
                    op("pe", lambda: P.matmul(bank(ka), qnz[:, hh, hp, :], KT[:, hp, e0:e0 + 512], start=True, stop=False),
                       R=[qnzb] + kv_need, W=[psb[ka]], inc=False)
                    op("pe", lambda: P.matmul(bank(ka), ident[:], biasT[:, h, d0:d0 + 512], start=False, stop=False),
                       R=[nb_, b_const], W=[psb[ka]], inc=False)
                    op("pe", lambda: P.matmul(bank(ka), A2[:], rowm[:, t_ * 12:t_ * 12 + 8].unsqueeze(2).to_broadcast([2, 8, 64]),
                                              start=False, stop=True), R=[nb_], W=[psb[ka]])
                    op("pe", lambda: P.matmul(bank(kbk, nbk), qnz[:, hh, hp, :], KT[:, hp, e0 + 512:e0 + 512 + nbk], start=True, stop=False),
                       R=[qnzb] + kv_need, W=[psb[kbk]], inc=False)
                    op("pe", lambda: P.matmul(bank(kbk, nbk), ident[:], biasT[:, h, d0 + 512:d0 + 512 + nbk], start=False, stop=False),
                       R=[nb_, b_const], W=[psb[kbk]], inc=False)
                    op("pe", lambda: P.matmul(bank(kbk, nbk), A2[:],
                                              rowm[:, t_ * 12 + 8:t_ * 12 + 8 + (n - 4) * 2].unsqueeze(2).to_broadcast([2, (n - 4) * 2, 64]),
                                              start=False, stop=True), R=[nb_], W=[psb[kbk]], inc=False)
                    op("pe", lambda: P.matmul(bank(kbk, 256, nbk), qnz[:, hh, hp, :], KcT[:, hp, :], start=True, stop=True),
                       R=[qnzb, ctxb], W=[psb[kbk]])
                    return ka, kbk

                def stETV(t_, cx, h, ka, kbk):
                    xt, xbuf, qnz, qnzb, sm, smb = cx
                    lo, hi, n, nbk = geomN(t_)
                    kv_need = [kvb[j + 2] for j in range(lo, hi + 1)]
                    Pe, Peb = Pe_r.next()
                    op("act", lambda: A.activation(out=Pe[:, 0:512], in_=bank(ka), func=AF.Exp, accum_out=sm[:, 2 * h:2 * h + 1]),
                       R=[psb[ka]], W=[Peb, smb])
                    op("act", lambda: A.activation(out=Pe[:, 512:768 + nbk], in_=bank(kbk, nbk + 256), func=AF.Exp,
                                                   accum_out=sm[:, 2 * h + 1:2 * h + 2]), R=[psb[kbk]], W=[Peb, smb])
                    nblk = n + 2
                    for bk in range(nblk):
                        op("pe", lambda: P.transpose(bank_bf(6, 128, bk * 128), Pe[:, bk * 128:(bk + 1) * 128], ident[:]),
                           R=[Peb, b_const], W=[psb[6]], inc=(bk == nblk - 1))
                    PT, PTb = PT_r.next()
                    op("dve", lambda: V.tensor_copy(PT[:, 0:nblk, :].rearrange("p a b -> p (a b)"), bank_bf(6, nblk * 128)), R=[psb[6]], W=[PTb])
                    for bk in range(nblk):
                        if bk < n:
                            rhs = VE[:, lo + 2 + bk, h * 64:(h + 1) * 64]
                        else:
                            rhs = Vc[:, bk - n, h * 64:(h + 1) * 64]
                        op("pe", lambda: P.matmul(bank(7, 64, h * 64), PT[:, bk, :], rhs, start=(bk == 0), stop=(bk == nblk - 1)),
                           R=[PTb, ctxb] + kv_need, W=[psb[7]], inc=(bk == nblk - 1))

                def stF(t_, cx):
                    xt, xbuf, qnz, qnzb, sm, smb = cx
                    op("dve", lambda: V.tensor_reduce(out=sm[:, 16:24], in_=sm[:, 0:16].rearrange("p (h two) -> p h two", two=2),
                                                      op=ALU.add, axis=AX.X), R=[smb], W=[smb])
                    op("dve", lambda: V.reciprocal(sm[:, 16:24], sm[:, 16:24]), R=[smb], W=[smb])
                    mix, mixb = mix_r.next()
                    op("dve", lambda: V.tensor_tensor(out=h3(mix[:, 512:1024]), in0=h3(bank(7)), in1=b3(sm[:, 16:24], 8), op=ALU.mult),
                       R=[psb[7], smb], W=[mixb])
                    op("pool", lambda: G.tensor_copy(mix[:, 0:512], ret_out[:, t_, :]), R=[retb[t_]], W=[mixb])
                    mixT, mixTb = mixT_r.next()
                    for dk in range(8):
                        op("pe", lambda: P.transpose(bank_bf(0, 128, dk * 128), mix[:, dk * 128:(dk + 1) * 128], ident[:]),
                           R=[mixb, b_const], W=[psb[0]], inc=(dk == 7))
                    op("act", lambda: A.copy(out=mixT[:].rearrange("p a b -> p (a b)"), in_=bank_bf(0)), R=[psb[0]], W=[mixTb])
                    xo, xob = xo_r.next()
                    for hf in range(2):
                        k = 1 if hf == 0 else 6
                        for dk in range(8):
                            op("pe", lambda: P.matmul(bank(k), mixT[:, dk, :], wo[:, dk, hf * 512:(hf + 1) * 512], start=(dk == 0), stop=(dk == 7)),
                               R=[mixTb, wob], W=[psb[k]], inc=(dk == 7))
                        yt, ytb = yt_r.next()
                        op("dve", lambda: V.tensor_tensor(out=yt[:], in0=bank(k), in1=gmb[:, hf * 512:(hf + 1) * 512], op=ALU.mult),
                           R=[psb[k], mbuf], W=[ytb])
                        op("pool", lambda: G.tensor_tensor(out=xo[:, hf * 512:(hf + 1) * 512], in0=yt[:], in1=xt[:, hf * 512:(hf + 1) * 512],
                                                           op=ALU.add), R=[ytb, xbuf], W=[xob])
                    dma("sp", x_dst[t_ * 128:(t_ + 1) * 128, :], xo[:], R=[xob])
                hdsN = {0: headN(0)}
                cxN = {0: stA(0, hdsN[0])}
                for t_ in range(NT):
                    if t_ + 1 < NT:
                        hdsN[t_ + 1] = headN(t_ + 1)
                    cx = cxN.pop(t_)
                    pend = stS(t_, cx, 0)
                    for h in range(8):
                        cur = pend
                        if h < 7:
                            pend = stS(t_, cx, h + 1)
                        if h == 3 and t_ + 1 < NT:
                            cxN[t_ + 1] = stA(t_ + 1, hdsN.pop(t_ + 1))
                        stETV(t_, cx, h, *cur)
                    stF(t_, cx)
                kb.barrier()

    if mode == 'full':
        mixer0_phase(xa)
        moe_phase(0, xa, xb)
        sgu_phase(xb, xc)
        moe_phase(1, xc, y_out)
    elif mode == 'mix0':
        mixer0_phase(y_out)
    elif mode == 'pro':
        with ExitStack() as t:
            wring = Ring(kb, t, "adaw", 2, [128, 8, 1024], BF16)
            bring = Ring(kb, t, "adab", 2, [1, 1024], BF16)
            o1 = kb.sb(t, "o1", [128, D], F32)
            ob = Buf("o1")
            ada_chunk(0, 2, silc, o1[:], ob, wring, bring)
            dma("sp", y_out[0:128, :], o1[:], R=[ob])
            G1, SH, mb = make_mod(t, 1, 3, 4, nffn_g[1:2, :], silcc, "mf", wring, bring)
            dma("sp", y_out[128:256, :], G1[:], R=[mb])
            dma("sp", y_out[256:384, :], SH[:], R=[mb])
            kb.barrier()
    elif mode == 'moe0':
        moe_phase(0, dbg_x, y_out)
    elif mode == 'sgu':
        sgu_phase(dbg_x, y_out)
    elif mode == 'moe1':
        moe_phase(1, dbg_x, y_out)
    kb.barrier()
    return kb


def _bf(a):
    return np.ascontiguousarray(a).astype(np.float32)


def _rope_tables(pos, scale_k):
    inv = (10000.0 ** (-np.arange(16, dtype=np.float32) / 16.0)).astype(np.float32)
    rows = (pos // 64).astype(np.float32)
    cols = (pos % 64).astype(np.float32)
    ar = rows[:, None] * inv[None, :]
    ac = cols[:, None] * inv[None, :]
    cr, sr, cc_, sc = np.cos(ar), np.sin(ar), np.cos(ac), np.sin(ac)
    C = np.concatenate([cr, cr, cc_, cc_], axis=1).astype(np.float32)
    S = np.concatenate([-sr, sr, -sc, sc], axis=1).astype(np.float32)
    return C, S


def prep_inputs(inputs):
    f = lambda k: np.asarray(inputs[k], dtype=np.float32)
    x, c, ctx, c_ctx = f('x'), f('c'), f('ctx'), f('c_ctx')
    w_in = f('ab_w_in')[0]
    blk = lambda i: w_in[:, i * 512:(i + 1) * 512]
    w_in_p = np.ascontiguousarray(np.concatenate([blk(4), blk(0), blk(1), blk(5), blk(6), blk(2), blk(3)], axis=1))
    rpb = f('na_rpb')[0]
    tab = np.full((8, 2, 64, 7, 2, 64), NEG, dtype=np.float32)
    w = np.arange(64)
    c0 = np.clip(w - 8, 0, 48)
    for a in range(2):
        for di in range(7):
            for b in range(2):
                rel = 2 * di + b - a + 1
                if rel < 0 or rel > 14:
                    continue
                for wi in range(64):
                    ccs = np.arange(c0[wi], c0[wi] + 16)
                    tab[:, a, wi, di, b, ccs] = rpb[:, rel, ccs - wi + 15]
    tab = tab.reshape(8, 128, 896)
    shared = {
        'ada_w': f('ada_w'), 'ada_b': f('ada_b'), 'norm_mix_g': f('norm_mix_g'), 'norm_ffn_g': f('norm_ffn_g'),
        'router_w': f('router_w'), 'router_bias': f('router_bias').reshape(1, 16),
        'moe_w_gate': f('moe_w_gate'), 'moe_w_up': f('moe_w_up'), 'moe_w_down': f('moe_w_down'),
        'ab_w_in': w_in_p, 'ab_w_out': f('ab_w_out')[0], 'ret_decay': f('ret_decay')[0].reshape(1, 16),
        'head_g': np.concatenate([f('ret_norm_g')[0], f('na_q_g')[0], f('na_k_g')[0]]).reshape(1, 192),
        'bias_tab': tab,
        'sgu_w_in': f('sgu_w_in')[0], 'sgu_b_in': f('sgu_b_in')[0].reshape(1, 6144),
        'sgu_norm_g': f('sgu_norm_g')[0].reshape(1, 3072), 'sgu_w_s': f('sgu_w_s')[0],
        'sgu_bsT': np.ascontiguousarray(f('sgu_b_s')[0].T), 'sgu_w_out': f('sgu_w_out')[0],
        'cc_row': c_ctx.reshape(1, D),
        'ident': np.eye(128, dtype=np.float32),
        'a2': np.repeat(np.eye(2, dtype=np.float32), 64, axis=1),
    }
    jj = np.arange(128, dtype=np.float32)[:, None]
    ii = np.arange(128, dtype=np.float32)[None, :]
    DF = np.where(ii >= jj, ii - jj, BIG).astype(np.float32)
    DB = np.where(jj >= ii, jj - ii, BIG).astype(np.float32)
    p = np.arange(128, dtype=np.float32)
    pos = np.stack([p + 1, 128 - p, 127 - p, p], axis=1)
    shared['cst'] = np.concatenate([DF, DB, pos], axis=1).astype(np.float32)
    maps = []
    for core in range(8):
        b, s = core // 2, core % 2
        lo = s * TOK
        m = dict(shared)
        m['x_loc'] = np.ascontiguousarray(x[b, lo:lo + TOK])
        m['x_oth'] = np.ascontiguousarray(x[b, (1 - s) * TOK:(2 - s) * TOK])
        halo = np.zeros((512, D), np.float32)
        if s == 1:
            halo[0:256] = x[b, lo - 256:lo]
        else:
            halo[256:512] = x[b, lo + TOK:lo + TOK + 256]
        m['x_halo'] = halo
        m['x_ctx'] = np.ascontiguousarray(ctx[b])
        m['c_row'] = c[b].reshape(1, D)
        C, S = _rope_tables(np.arange(lo, lo + TOK), 0.125)
        m['rope_loc'] = np.concatenate([C, C * 0.125, S, S * 0.125], axis=1).astype(np.float32)
        Co, So = _rope_tables(np.arange((1 - s) * TOK, (2 - s) * TOK), 0.125)
        ro = np.concatenate([Co * 0.125, So * 0.125], axis=1)
        rc = np.concatenate([np.full((256, 64), 0.125, np.float32), np.zeros((256, 64), np.float32)], axis=1)
        m['rope_oth'] = np.concatenate([ro, rc], axis=0).astype(np.float32)
        mo = np.arange(TOK, dtype=np.float32)
        n = np.arange(256, dtype=np.float32)
        if s == 1:
            EFo = 2047.0 - mo
            EBo = np.full(TOK, BIG, np.float32)
        else:
            EFo = np.full(TOK, BIG, np.float32)
            EBo = mo
        EFc = 255.0 - n + 2048.0 * s
        EBc = n + 2048.0 * (1 - s)
        EF = np.concatenate([EFo, EFc]).reshape(18, 128).T
        EB = np.concatenate([EBo, EBc]).reshape(18, 128).T
        m['exp_init'] = np.ascontiguousarray(np.concatenate([EF, EB], axis=1)).astype(np.float32)
        rm = np.full((2, 16, 6, 2), NEG, np.float32)
        for t in range(16):
            lo_t, hi_t = t - 2, t + 2
            if t < 2:
                hi_t = 3
            if t > 13:
                lo_t = 12
            for a in range(2):
                r = 32 * s + 2 * t + a
                r0 = min(max(r - 4, 0), 56)
                for sl, j in enumerate(range(lo_t, hi_t + 1)):
                    for bb in range(2):
                        kr = 32 * s + 2 * j + bb
                        if r0 <= kr <= r0 + 7:
                            rm[a, t, sl, bb] = 0.0
        m['rowmask'] = rm.reshape(2, 192)
        m['dbg_x'] = np.zeros((TOK, D), np.float32)
        maps.append(m)
    return maps


_CACHE = {}


def kernel(**inputs):
    maps = prep_inputs(inputs)
    if 'full' not in _CACHE:
        _CACHE['full'] = build('full')
    kb = _CACHE['full']
    maps = [{k: v for k, v in m.items() if k in kb.declared} for m in maps]
    res = run_bass_kernel_spmd(kb.nc, maps, core_ids=list(range(8)))
    out = np.zeros((4, 4096, D), np.float32)
    for core in range(8):
        b, s = core // 2, core % 2
        out[b, s * TOK:(s + 1) * TOK] = res.results[core]['y_out']
    return out
```

```python
import numpy as np
import ml_dtypes
from contextlib import ExitStack
import concourse.bass as bass
import concourse.mybir as mybir
from concourse.bass_utils import run_bass_kernel_spmd

F32 = mybir.dt.float32
BF16 = mybir.dt.bfloat16
AF = mybir.ActivationFunctionType
ALU = mybir.AluOpType
AX = mybir.AxisListType

D = 1024
NT = 16
TOK = 2048
NEG = -30000.0
EPS = 1e-6
BIG = 1.0e9


class Buf:
    __slots__ = ("name", "w", "r", "excl")

    def __init__(self, name="", excl=False):
        self.name = name
        self.w = None
        self.r = []
        self.excl = excl


class Eng:
    def __init__(self, name, h, sem):
        self.name = name
        self.h = h
        self.sem = sem
        self.count = 0
        self.waited = {}
        self.dsems = []
        self.dcnt = []
        self.di = 0


class KB:
    def __init__(self):
        self.nc = bass.Bass("TRN2", target_bir_lowering=False)
        self.es = ExitStack()
        nc = self.nc
        self.E = {}
        for name, h in (("pe", nc.tensor), ("act", nc.scalar), ("dve", nc.vector),
                        ("pool", nc.gpsimd), ("sp", nc.sync)):
            sem = self.es.enter_context(nc.semaphore("s_" + name))
            self.E[name] = Eng(name, h, sem)
        for qn in ("sp", "pool", "act"):
            q = self.E[qn]
            for i in range(8):
                q.dsems.append(self.es.enter_context(nc.semaphore("d_%s%d" % (qn, i))))
                q.dcnt.append(0)
        self.dma_events = []

    def _wait(self, e, ev):
        key, sem, val = ev
        if e.waited.get(key, 0) >= val:
            return
        e.h.wait_ge(sem, val)
        e.waited[key] = val

    def _deps(self, e, R, W):
        for b in R:
            if b.w is not None:
                if b.w[0] == e.name and e.name == "pe":
                    continue
                self._wait(e, b.w)
        for b in W:
            if b.w is not None and b.w[0] != e.name:
                self._wait(e, b.w)
            for ev in b.r:
                if ev[0] != e.name:
                    self._wait(e, ev)

    def op(self, en, fn, R=(), W=(), inc=True):
        e = self.E[en]
        if any(b.excl for b in R):
            W = list(W) + [b for b in R if b.excl and b not in W]
            R = [b for b in R if not b.excl]
        self._deps(e, R, W)
        ins = fn()
        val = e.count + 1
        if inc:
            ins.then_inc(e.sem, 1)
            e.count = val
        ev = (en, e.sem, val)
        for b in W:
            b.w = ev
            b.r = []
        for b in R:
            b.r.append(ev)
        return ins

    def dma(self, qn, out, in_, R=(), W=()):
        q = self.E[qn]
        self._deps(q, R, W)
        slot = q.di % len(q.dsems)
        q.di += 1
        sem = q.dsems[slot]
        key = "d_%s%d" % (qn, slot)
        if q.dcnt[slot] > 0:
            self._wait(q, (key, sem, 16 * q.dcnt[slot]))
        q.dcnt[slot] += 1
        q.h.dma_start(out=out, in_=in_).then_inc(sem, 16)
        ev = (key, sem, 16 * q.dcnt[slot])
        for b in W:
            b.w = ev
            b.r = []
        for b in R:
            b.r.append(ev)
        self.dma_events.append(ev)

    def barrier(self):
        evs = [(n, e.sem, e.count) for n, e in self.E.items() if e.count > 0]
        last = {}
        for ev in self.dma_events:
            last[ev[0]] = ev
        evs += list(last.values())
        self.dma_events = list(last.values())
        for n, e in self.E.items():
            for ev in evs:
                if ev[0] != n:
                    self._wait(e, ev)

    def sb(self, es, name, shape, dt):
        self.uid = getattr(self, "uid", 0) + 1
        return es.enter_context(self.nc.sbuf_tensor("sb%d_%s" % (self.uid, name), shape, dt))


class Ring:
    def __init__(self, kb, es, name, n, shape, dt):
        self.t = [kb.sb(es, "%s%d" % (name, i), shape, dt) for i in range(n)]
        self.b = [Buf("%s%d" % (name, i)) for i in range(n)]
        self.i = 0

    def next(self):
        k = self.i % len(self.t)
        self.i += 1
        return self.t[k], self.b[k]


def build(mode='full'):
    kb = KB()
    nc = kb.nc
    es = kb.es
    op = kb.op
    dma = kb.dma
    V = nc.vector
    A = nc.scalar
    P = nc.tensor
    G = nc.gpsimd

    def din(name, shape, dt=F32):
        return nc.dram_tensor(name, list(shape), dt, kind="ExternalInput").ap()

    SHAPES = {
        "x_loc": [TOK, D], "x_oth": [TOK, D], "x_halo": [512, D], "x_ctx": [256, D], "c_row": [1, D], "cc_row": [1, D],
        "ada_w": [2, D, 6 * D], "ada_b": [2, 6 * D], "norm_mix_g": [2, D], "norm_ffn_g": [2, D],
        "router_w": [D, 16], "router_bias": [1, 16],
        "moe_w_gate": [2, 16, D, 512], "moe_w_up": [2, 16, D, 512], "moe_w_down": [2, 16, 512, D],
        "ab_w_in": [D, 3584], "ab_w_out": [D, D], "ret_decay": [1, 16], "head_g": [1, 192],
        "bias_tab": [8, 128, 896], "rowmask": [2, 192], "rope_loc": [TOK, 256], "rope_oth": [TOK + 256, 128],
        "exp_init": [128, 36], "cst": [128, 260], "ident": [128, 128], "a2": [2, 128],
        "sgu_w_in": [D, 6144], "sgu_b_in": [1, 6144], "sgu_norm_g": [1, 3072], "sgu_w_s": [8, 128, 128],
        "sgu_bsT": [128, 8], "sgu_w_out": [3072, D], "dbg_x": [TOK, D],
    }
    declared = {}

    class Lazy:
        def __init__(self, name):
            self.name = name

        def ap(self):
            if self.name not in declared:
                declared[self.name] = nc.dram_tensor(self.name, list(SHAPES[self.name]), F32, kind="ExternalInput").ap()
            return declared[self.name]

        def __getitem__(self, k):
            return self.ap()[k]

        def rearrange(self, *a, **kw):
            return self.ap().rearrange(*a, **kw)

    kb.declared = declared
    x_loc, x_oth, x_halo, x_ctx, c_row, cc_row = (Lazy(n) for n in ("x_loc", "x_oth", "x_halo", "x_ctx", "c_row", "cc_row"))
    ada_w, ada_b, nmix_g, nffn_g = (Lazy(n) for n in ("ada_w", "ada_b", "norm_mix_g", "norm_ffn_g"))
    router_w, router_b = Lazy("router_w"), Lazy("router_bias")
    w_gate, w_up, w_down = Lazy("moe_w_gate"), Lazy("moe_w_up"), Lazy("moe_w_down")
    ab_w_in, ab_w_out, ret_decay, hg = Lazy("ab_w_in"), Lazy("ab_w_out"), Lazy("ret_decay"), Lazy("head_g")
    bias_tab, rowmask, rope_loc, rope_oth = Lazy("bias_tab"), Lazy("rowmask"), Lazy("rope_loc"), Lazy("rope_oth")
    exp_init, cst, ident_d, a2_d = Lazy("exp_init"), Lazy("cst"), Lazy("ident"), Lazy("a2")
    sgu_w_in, sgu_b_in, sgu_ng, sgu_ws = Lazy("sgu_w_in"), Lazy("sgu_b_in"), Lazy("sgu_norm_g"), Lazy("sgu_w_s")
    sgu_bsT, sgu_w_out, dbg_x = Lazy("sgu_bsT"), Lazy("sgu_w_out"), Lazy("dbg_x")
    y_out = nc.dram_tensor("y_out", [TOK, D], F32, kind="ExternalOutput").ap()
    xa = nc.dram_tensor("xa_scr", [TOK, D], F32, kind="Internal").ap()
    xb = nc.dram_tensor("xb_scr", [TOK, D], F32, kind="Internal").ap()
    xc = nc.dram_tensor("xc_scr", [TOK, D], F32, kind="Internal").ap()

    ps = es.enter_context(nc.psum_tensor("ps", [128, 4096], F32))
    ps_bf = ps.bitcast(BF16)
    psb = [Buf("ps%d" % i, excl=True) for i in range(8)]

    class PsRing:
        def __init__(self, banks):
            self.banks = banks
            self.i = 0

        def next(self):
            k = self.banks[self.i % len(self.banks)]
            self.i += 1
            return k

    def bank(k, n=512, off=0):
        return ps[:, k * 512 + off:k * 512 + off + n]

    def bank_bf(k, n=1024, off=0):
        return ps_bf[:, k * 1024 + off:k * 1024 + off + n]

    g = ExitStack()
    es.enter_context(g)
    ident = kb.sb(g, "ident", [128, 128], BF16)
    identf = kb.sb(g, "identf", [128, 128], F32)
    ones1 = kb.sb(g, "ones1", [1, 128], BF16)
    silc = kb.sb(g, "silc", [128, 8, 128], BF16)
    silcc = kb.sb(g, "silcc", [128, 8, 128], BF16)
    b_const = Buf("const")
    dma("pool", ident[:], ident_d[:, :], W=[b_const])
    dma("sp", identf[:], ident_d[:, :], W=[b_const])
    op("dve", lambda: V.memset(ones1[:], 1.0), W=[b_const])

    with ExitStack() as t:
        crow = kb.sb(t, "crow", [128, 2, D], F32)
        csil = kb.sb(t, "csil", [128, 2, D], BF16)
        bt = Buf("crow")
        dma("sp", crow[:, 0, :], c_row[0:1, :].partition_broadcast(128), W=[bt])
        dma("sp", crow[:, 1, :], cc_row[0:1, :].partition_broadcast(128), W=[bt])
        op("act", lambda: A.activation(out=csil[:], in_=crow[:], func=AF.Silu), R=[bt], W=[bt])
        for j, dst in enumerate((silc, silcc)):
            for dk in range(8):
                op("pe", lambda: P.transpose(bank_bf(0, 128, dk * 128), csil[:, j, dk * 128:(dk + 1) * 128], ident[:]),
                   R=[bt, b_const], W=[psb[0]], inc=(dk == 7))
            op("dve", lambda: V.tensor_copy(dst[:].rearrange("p a b -> p (a b)"), bank_bf(0)), R=[psb[0]], W=[b_const])
        kb.barrier()

    def ada_chunk(l, j, lhs, out_ap, out_buf, wring, bring, post=None):
        wt, wb = wring.next()
        dma("pool", wt[:], ada_w[l, :, j * 1024:(j + 1) * 1024].rearrange("(k p) n -> p k n", p=128), W=[wb])
        bt_, bb = bring.next()
        dma("pool", bt_[:], ada_b[l:l + 1, j * 1024:(j + 1) * 1024], W=[bb])
        for hf in range(2):
            k = 6 + hf
            for dk in range(8):
                op("pe", lambda: P.matmul(bank(k), lhs[:, dk, :], wt[:, dk, hf * 512:(hf + 1) * 512],
                                          start=(dk == 0), stop=False), R=[wb, b_const], W=[psb[k]], inc=False)
            op("pe", lambda: P.matmul(bank(k), ones1[:], bt_[:, hf * 512:(hf + 1) * 512], start=False, stop=True),
               R=[bb, b_const], W=[psb[k]])
        if post is None:
            op("act", lambda: A.copy(out=out_ap, in_=ps[:, 6 * 512:8 * 512]), R=[psb[6], psb[7]], W=[out_buf])
        else:
            post(ps[:, 6 * 512:8 * 512], [psb[6], psb[7]])

    def front_a(xt, xbuf, G1, SH, gbuf, fr, want_f32T=False):
        junk, jb = fr["junk"].next()
        st, sbf = fr["st"].next()
        op("act", lambda: A.activation(out=junk[:], in_=xt, func=AF.Square, accum_out=st[:, 0:1]),
           R=[xbuf], W=[jb, sbf])
        op("act", lambda: A.activation(out=st[:, 1:2], in_=st[:, 0:1], func=AF.Sqrt, scale=1.0 / D, bias=fr["eps"][:, 0:1]),
           R=[sbf, b_const], W=[sbf])
        op("dve", lambda: V.reciprocal(st[:, 2:3], st[:, 1:2]), R=[sbf], W=[sbf])
        tmp, tb = fr["tmp"].next()
        op("dve", lambda: V.scalar_tensor_tensor(out=tmp[:], in0=xt, scalar=st[:, 2:3], in1=G1, op0=ALU.mult, op1=ALU.mult),
           R=[xbuf, sbf, gbuf], W=[tb])
        if want_f32T:
            h, hb = fr["h32"].next()
        else:
            h, hb = fr["h"].next()
        op("pool", lambda: G.tensor_tensor(out=h[:], in0=tmp[:], in1=SH, op=ALU.add), R=[tb, gbuf], W=[hb])
        return h, hb, want_f32T

    def front_b(fa, fr, bf_dst=None):
        h, hb, want_f32T = fa
        if not want_f32T:
            hT, hTb = fr["hT"].next()
            k = fr["ps"].next()
            for dk in range(8):
                op("pe", lambda: P.transpose(bank_bf(k, 128, dk * 128), h[:, dk * 128:(dk + 1) * 128], ident[:]),
                   R=[hb, b_const], W=[psb[k]], inc=(dk == 7))
            op("act", lambda: A.copy(out=hT[:].rearrange("p a b -> p (a b)"), in_=bank_bf(k)), R=[psb[k]], W=[hTb])
            return hT, hTb, None, None
        else:
            hT32, hT32b = fr["hT32"].next()
            dst, dstb = bf_dst
            for half in range(2):
                k = fr["ps"].next()
                for q in range(4):
                    dk = half * 4 + q
                    op("pe", lambda: P.transpose(bank(k, 128, q * 128), h[:, dk * 128:(dk + 1) * 128], identf[:]),
                       R=[hb, b_const], W=[psb[k]], inc=(q == 3))
                op("act", lambda: A.copy(out=dst[:, half * 4:(half + 1) * 4, :], in_=bank(k).rearrange("p (a b) -> p a b", b=128)),
                   R=[psb[k]], W=[dstb])
                op("dve", lambda: V.tensor_copy(hT32[:, half * 4:(half + 1) * 4, :].rearrange("p a b -> p (a b)"), bank(k)),
                   R=[psb[k]], W=[hT32b])
            return None, None, hT32, hT32b

    def front(xt, xbuf, G1, SH, gbuf, fr, want_f32T=False):
        return front_b(front_a(xt, xbuf, G1, SH, gbuf, fr, want_f32T), fr)

    def pipelined(n, head, body):
        nxt = head(0)
        for i in range(n):
            cur = nxt
            nxt = head(i + 1) if i + 1 < n else None
            body(i, cur)

    def make_front(t, ps_banks, f32T=False):
        fr = {}
        fr["junk"] = Ring(kb, t, "fjunk", 1, [128, D], BF16)
        fr["st"] = Ring(kb, t, "fst", 3, [128, 4], F32)
        fr["tmp"] = Ring(kb, t, "ftmp", 1, [128, D], F32)
        if f32T:
            fr["h32"] = Ring(kb, t, "fh32", 2, [128, D], F32)
            fr["hT32"] = Ring(kb, t, "fhT32", 2, [128, 8, 128], F32)
        else:
            fr["h"] = Ring(kb, t, "fh", 2, [128, D], BF16)
        if not f32T:
            fr["hT"] = Ring(kb, t, "fhT", 2, [128, 8, 128], BF16)
        fr["ps"] = PsRing(ps_banks)
        eps = kb.sb(t, "feps", [128, 1], F32)
        op("dve", lambda: V.memset(eps[:], EPS), W=[b_const])
        fr["eps"] = eps
        return fr

    def make_mod(t, l, jshift, jscale, gsrc, lhs, name, wring, bring):
        G1 = kb.sb(t, name + "G1", [128, D], F32)
        SH = kb.sb(t, name + "SH", [128, D], F32)
        gb = kb.sb(t, name + "gb", [128, D], F32)
        mb = Buf(name)
        dma("sp", gb[:], gsrc.partition_broadcast(128), W=[mb])
        ada_chunk(l, jshift, lhs, SH[:], mb, wring, bring)

        def post(psap, pbufs):
            op("dve", lambda: V.scalar_tensor_tensor(out=G1[:], in0=psap, scalar=1.0, in1=gb[:], op0=ALU.add, op1=ALU.mult),
               R=pbufs + [mb], W=[mb])
        ada_chunk(l, jscale, lhs, None, mb, wring, bring, post=post)
        return G1, SH, mb

    def moe_phase(l, x_src, x_dst):
        with ExitStack() as t:
            xres = kb.sb(t, "xres", [128, NT, D], F32)
            xrb = [Buf("xres%d" % i) for i in range(NT)]
            hfT = kb.sb(t, "hfT", [128, 8, TOK], BF16)
            hfTb = [Buf("hfT%d" % i) for i in range(NT)]
            comb = kb.sb(t, "comb", [128, NT, 16], F32)
            combb = [Buf("comb%d" % i) for i in range(NT)]
            rw = kb.sb(t, "rw", [128, 8, 16], F32)
            rbias = kb.sb(t, "rbias", [128, 16], F32)
            gfb = kb.sb(t, "gfb", [128, D], F32)
            gfbuf = Buf("gfb")
            cb = Buf("rconst")
            dma("sp", rw[:], router_w.rearrange("(k p) e -> p k e", p=128), W=[cb])
            dma("sp", rbias[:], router_b[0:1, :].partition_broadcast(128), W=[cb])
            for i in range(NT):
                dma("sp", xres[:, i, :], x_src[i * 128:(i + 1) * 128, :], W=[xrb[i]])
            with ExitStack() as t2:
                wring = Ring(kb, t2, "adaw", 2, [128, 8, 1024], BF16)
                bring = Ring(kb, t2, "adab", 2, [1, 1024], BF16)
                G1, SH, mb = make_mod(t2, l, 3, 4, nffn_g[l:l + 1, :], silc, "mf", wring, bring)
                ada_chunk(l, 5, silc, gfb[:], gfbuf, wring, bring)
                fr = make_front(t2, [0, 1], f32T=True)
                RB = kb.sb(t2, "RB", [128, 4, NT, 16], F32)
                RS = kb.sb(t2, "RS", [128, 6, NT, 4], F32)
                rb = Buf("RB")
                fas = {}
                t32 = {}
                for j in range(NT + 2):
                    if j < NT:
                        fas[j] = front_a(xres[:, j, :], xrb[j], G1[:], SH[:], mb, fr, want_f32T=True)
                    i = j - 1
                    if 0 <= i < NT:
                        _, _, hT32, hT32b = front_b(fas.pop(i), fr, bf_dst=(hfT[:, :, i * 128:(i + 1) * 128], hfTb[i]))
                        t32[i] = (hT32, hT32b)
                    i = j - 2
                    if 0 <= i < NT:
                        hT32, hT32b = t32.pop(i)
                        k = 2 + (i % 2)
                        for dk in range(8):
                            op("pe", lambda: P.matmul(bank(k, 16), hT32[:, dk, :], rw[:, dk, :], start=(dk == 0), stop=(dk == 7)),
                               R=[hT32b, cb], W=[psb[k]], inc=(dk == 7))
                        op("act", lambda: A.activation(out=RB[:, 0, i, :], in_=bank(k, 16), func=AF.Sigmoid), R=[psb[k]], W=[rb])
                aff = RB[:, 0]
                sel = RB[:, 1]
                tmp = RB[:, 2]
                msk = RB[:, 3]
                m1 = RS[:, 0]
                m2 = RS[:, 1]
                gs = RS[:, 2]
                goh = RS[:, 3]
                gm = RS[:, 4, :, 0]
                den = RS[:, 5, :, 0]
                rden = RS[:, 5, :, 1]
                g4 = lambda ap: ap.rearrange("p t (g e) -> p (t g) e", e=4)
                f4 = lambda ap: ap.rearrange("p t g -> p (t g)")
                b4 = lambda ap: f4(ap).unsqueeze(2).to_broadcast([128, NT * 4, 4])
                op("dve", lambda: V.tensor_tensor(out=sel, in0=aff, in1=rbias[:].unsqueeze(1).to_broadcast([128, NT, 16]), op=ALU.add),
                   R=[rb, cb], W=[rb])
                op("dve", lambda: V.tensor_reduce(out=f4(m1), in_=g4(sel), op=ALU.max, axis=AX.X), R=[rb], W=[rb])
                op("dve", lambda: V.tensor_tensor(out=g4(tmp), in0=g4(sel), in1=b4(m1), op=ALU.is_ge), R=[rb], W=[rb])
                op("dve", lambda: V.scalar_tensor_tensor(out=tmp, in0=tmp, scalar=-1.0e4, in1=sel, op0=ALU.mult, op1=ALU.add),
                   R=[rb], W=[rb])
                op("dve", lambda: V.tensor_reduce(out=f4(m2), in_=g4(tmp), op=ALU.max, axis=AX.X), R=[rb], W=[rb])
                op("dve", lambda: V.tensor_tensor(out=gs, in0=m1, in1=m2, op=ALU.add), R=[rb], W=[rb])
                op("dve", lambda: V.tensor_reduce(out=gm, in_=gs, op=ALU.max, axis=AX.X), R=[rb], W=[rb])
                op("dve", lambda: V.tensor_tensor(out=goh, in0=gs, in1=gm.unsqueeze(2).to_broadcast([128, NT, 4]), op=ALU.is_ge),
                   R=[rb], W=[rb])
                op("dve", lambda: V.tensor_tensor(out=g4(msk), in0=g4(sel), in1=b4(m2), op=ALU.is_ge), R=[rb], W=[rb])
                op("dve", lambda: V.tensor_tensor(out=g4(msk), in0=g4(msk), in1=b4(goh), op=ALU.mult), R=[rb], W=[rb])
                op("dve", lambda: V.tensor_tensor(out=msk, in0=msk, in1=aff, op=ALU.mult), R=[rb], W=[rb])
                op("dve", lambda: V.tensor_reduce(out=den, in_=msk, op=ALU.add, axis=AX.X), R=[rb], W=[rb])
                op("dve", lambda: V.reciprocal(rden, den), R=[rb], W=[rb])
                op("dve", lambda: V.tensor_tensor(out=comb[:], in0=msk, in1=rden.unsqueeze(2).to_broadcast([128, NT, 16]), op=ALU.mult),
                   R=[rb], W=combb)
                kb.barrier()
            with ExitStack() as t3:
                wg = Ring(kb, t3, "wg", 2, [128, 8, 512], BF16)
                wu = Ring(kb, t3, "wu", 2, [128, 8, 512], BF16)
                wd = Ring(kb, t3, "wd", 2, [128, 4, D], BF16)
                wd2 = Ring(kb, t3, "wd2", 2, [128, 4, D], BF16)
                sg = Ring(kb, t3, "sg", 2, [128, 512], BF16)
                at = Ring(kb, t3, "at", 2, [128, 4, 512], BF16)
                gu = PsRing([0, 1, 2, 3])
                yr = PsRing([4, 5, 6, 7])
                for e in range(16):
                    wgt, wgb = wg.next()
                    wut, wub = wu.next()
                    wdt, wdb = wd.next()
                    wd2t, wd2b = wd2.next()
                    dma("pool", wgt[:], w_gate[l, e].rearrange("(k p) n -> p k n", p=128), W=[wgb])
                    dma("pool", wut[:], w_up[l, e].rearrange("(k p) n -> p k n", p=128), W=[wub])
                    dma("pool", wdt[:], w_down[l, e].rearrange("(k p) n -> p k n", p=128), W=[wdb])
                    for fc in range(4):
                        op("pool", lambda: G.tensor_tensor(out=wd2t[:, fc, :], in0=wdt[:, fc, :], in1=gfb[:], op=ALU.mult),
                           R=[wdb, gfbuf], W=[wd2b])
                    for tg in range(4):
                        att, atb = at.next()
                        toks = slice(tg * 512, (tg + 1) * 512)
                        hb4 = hfTb[tg * 4:(tg + 1) * 4]
                        for fc in range(4):
                            kg = gu.next()
                            ku = gu.next()
                            for dk in range(8):
                                op("pe", lambda: P.matmul(bank(kg), wgt[:, dk, fc * 128:(fc + 1) * 128], hfT[:, dk, toks],
                                                          start=(dk == 0), stop=(dk == 7)), R=[wgb] + hb4, W=[psb[kg]], inc=(dk == 7))
                            for dk in range(8):
                                op("pe", lambda: P.matmul(bank(ku), wut[:, dk, fc * 128:(fc + 1) * 128], hfT[:, dk, toks],
                                                          start=(dk == 0), stop=(dk == 7)), R=[wub] + hb4, W=[psb[ku]], inc=(dk == 7))
                            sgt, sgb = sg.next()
                            op("act", lambda: A.activation(out=sgt[:], in_=bank(kg), func=AF.Silu), R=[psb[kg]], W=[sgb])
                            op("dve", lambda: V.tensor_tensor(out=att[:, fc, :], in0=bank(ku), in1=sgt[:], op=ALU.mult),
                               R=[psb[ku], sgb], W=[atb])
                        for ti in range(4):
                            i = tg * 4 + ti
                            for hf in range(2):
                                ky = yr.next()
                                for fc in range(4):
                                    op("pe", lambda: P.matmul(bank(ky), att[:, fc, ti * 128:(ti + 1) * 128],
                                                              wd2t[:, fc, hf * 512:(hf + 1) * 512], start=(fc == 0), stop=(fc == 3)),
                                       R=[atb, wd2b], W=[psb[ky]], inc=(fc == 3))
                                xs = xres[:, i, hf * 512:(hf + 1) * 512]
                                op("dve", lambda: V.scalar_tensor_tensor(out=xs, in0=bank(ky), scalar=comb[:, i, e:e + 1], in1=xs,
                                                                         op0=ALU.mult, op1=ALU.add),
                                   R=[psb[ky], combb[i], xrb[i]], W=[xrb[i]])
                for i in range(NT):
                    dma("sp", x_dst[i * 128:(i + 1) * 128, :], xres[:, i, :], R=[xrb[i]])
                kb.barrier()

    def sgu_phase(x_src, x_dst):
        l = 1
        with ExitStack() as t:
            wout = kb.sb(t, "swout", [128, 24, D], BF16)
            wsT = kb.sb(t, "swsT", [128, 8, 128], BF16)
            bsT = kb.sb(t, "sbsT", [128, 8], F32)
            ngb = kb.sb(t, "sngb", [128, 3072], BF16)
            gmb = kb.sb(t, "sgmb", [128, D], F32)
            gmbuf = Buf("gmb")
            cb = Buf("sconst")
            for q in range(4):
                dma("pool", wout[:, q * 6:(q + 1) * 6, :],
                    sgu_w_out[q * 768:(q + 1) * 768, :].rearrange("(k p) n -> p k n", p=128), W=[cb])
            dma("sp", bsT[:], sgu_bsT[:, :], W=[cb])
            dma("pool", ngb[:], sgu_ng[0:1, :].partition_broadcast(128), W=[cb])
            G1 = kb.sb(t, "smG1", [128, D], F32)
            SH = kb.sb(t, "smSH", [128, D], F32)
            with ExitStack() as t0:
                wring = Ring(kb, t0, "adaw", 2, [128, 8, 1024], BF16)
                bring = Ring(kb, t0, "adab", 2, [1, 1024], BF16)
                G1_, SH_, mb = make_mod(t0, l, 0, 1, nmix_g[l:l + 1, :], silc, "sm", wring, bring)
                op("pool", lambda: G.tensor_copy(G1[:], G1_[:]), R=[mb], W=[cb])
                op("pool", lambda: G.tensor_copy(SH[:], SH_[:]), R=[mb], W=[cb])
                ada_chunk(l, 2, silc, gmb[:], gmbuf, wring, bring)
                wsf = kb.sb(t0, "wsf", [128, 8, 128], F32)
                wsb = Buf("wsf")
                dma("sp", wsf[:], sgu_ws.rearrange("g i j -> i g j"), W=[wsb])
                for gi in range(8):
                    op("pe", lambda: P.transpose(bank(gi // 4, 128, (gi % 4) * 128), wsf[:, gi, :], identf[:]),
                       R=[wsb, b_const], W=[psb[gi // 4]])
                op("dve", lambda: V.tensor_copy(wsT[:].rearrange("p a b -> p (a b)"), ps[:, 0:1024]), R=[psb[0], psb[1]], W=[cb])
                kb.barrier()
            fr = make_front(t, [0])
            hTblk = kb.sb(t, "shTblk", [128, 8, 512], BF16)
            hTblkb = Buf("hTblk")
            uu = kb.sb(t, "suu", [128, 4, 3072], BF16)
            vv = kb.sb(t, "svv", [128, 4, 3072], BF16)
            uvb = [Buf("uv%d" % i) for i in range(4)]
            wblk = Ring(kb, t, "swblk", 2, [128, 8, 512], BF16)
            bblk = Ring(kb, t, "sbblk", 2, [1, 512], BF16)
            vn = Ring(kb, t, "svn", 2, [128, 3072], BF16)
            gt = Ring(kb, t, "sgt", 1, [128, 3072], BF16)
            gT = Ring(kb, t, "sgT", 1, [128, 24, 128], BF16)
            st = Ring(kb, t, "sst", 2, [128, 4], F32)
            yt = Ring(kb, t, "syt", 1, [128, 512], F32)
            xo = Ring(kb, t, "sxo", 1, [128, D], F32)
            eps = fr["eps"]
            zr = PsRing([1, 2, 3, 4])
            xin = Ring(kb, t, "sxin", 2, [128, D], F32)
            xres_r = Ring(kb, t, "sxres", 2, [128, D], F32)

            def ldA(tb, ti):
                i = tb * 4 + ti
                xt, xbuf = xin.next()
                dma("sp", xt[:], x_src[i * 128:(i + 1) * 128, :], W=[xbuf])
                return xt, xbuf

            def stAa(xa_):
                xt, xbuf = xa_
                return front_a(xt[:], xbuf, G1[:], SH[:], cb, fr)

            def stAb(ti, fa):
                hT, hTb, _, _ = front_b(fa, fr)
                op("pool", lambda: G.tensor_copy(hTblk[:, :, ti * 128:(ti + 1) * 128], hT[:]), R=[hTb], W=[hTblkb])

            def stB(tb):
                for cbk in range(12):
                    wt, wb = wblk.next()
                    bt_, bb = bblk.next()
                    dma("pool", wt[:], sgu_w_in[:, cbk * 512:(cbk + 1) * 512].rearrange("(k p) n -> p k n", p=128), W=[wb])
                    dma("pool", bt_[:], sgu_b_in[0:1, cbk * 512:(cbk + 1) * 512], W=[bb])
                    for ti in range(4):
                        k = zr.next()
                        for dk in range(8):
                            op("pe", lambda: P.matmul(bank(k), hTblk[:, dk, ti * 128:(ti + 1) * 128], wt[:, dk, :],
                                                      start=(dk == 0), stop=False), R=[hTblkb, wb], W=[psb[k]], inc=False)
                        op("pe", lambda: P.matmul(bank(k), ones1[:], bt_[:], start=False, stop=True), R=[bb, b_const], W=[psb[k]])
                        dst = uu[:, ti, cbk * 512:(cbk + 1) * 512] if cbk < 6 else vv[:, ti, (cbk - 6) * 512:(cbk - 5) * 512]
                        op("act", lambda: A.activation(out=dst, in_=bank(k), func=AF.Gelu), R=[psb[k]], W=[uvb[ti]])

            def ldR(tb, ti):
                i = tb * 4 + ti
                xr, xrb_ = xres_r.next()
                dma("sp", xr[:], x_src[i * 128:(i + 1) * 128, :], W=[xrb_])
                return xr, xrb_

            def stN(ti):
                s_, sbf = st.next()
                vnt, vnb = vn.next()
                op("act", lambda: A.activation(out=vnt[:], in_=vv[:, ti, :], func=AF.Square, accum_out=s_[:, 0:1]),
                   R=[uvb[ti]], W=[vnb, sbf])
                op("act", lambda: A.activation(out=s_[:, 1:2], in_=s_[:, 0:1], func=AF.Sqrt, scale=1.0 / 3072, bias=eps[:, 0:1]),
                   R=[sbf, b_const], W=[sbf])
                op("dve", lambda: V.reciprocal(s_[:, 2:3], s_[:, 1:2]), R=[sbf], W=[sbf])
                op("dve", lambda: V.scalar_tensor_tensor(out=vnt[:], in0=vv[:, ti, :], scalar=s_[:, 2:3], in1=ngb[:],
                                                         op0=ALU.mult, op1=ALU.mult), R=[uvb[ti], sbf, cb], W=[vnb])
                return vnt, vnb

            def stC12(ti, vn_):
                vnt, vnb = vn_
                gtt, gtb = gt.next()
                for gi in range(8):
                    k = zr.next()
                    op("pe", lambda: P.matmul(bank(k, 384), wsT[:, gi, :], vnt[:, gi * 384:(gi + 1) * 384], start=True, stop=True),
                       R=[vnb, cb], W=[psb[k]])
                    op("dve", lambda: V.scalar_tensor_tensor(out=gtt[:, gi * 384:(gi + 1) * 384], in0=bank(k, 384),
                                                             scalar=bsT[:, gi:gi + 1], in1=uu[:, ti, gi * 384:(gi + 1) * 384],
                                                             op0=ALU.add, op1=ALU.mult), R=[psb[k], cb, uvb[ti]], W=[gtb])
                gTt, gTb = gT.next()
                for q in range(3):
                    k = zr.next()
                    for c8 in range(8):
                        kc = q * 8 + c8
                        op("pe", lambda: P.transpose(bank_bf(k, 128, c8 * 128), gtt[:, kc * 128:(kc + 1) * 128], ident[:]),
                           R=[gtb, b_const], W=[psb[k]], inc=(c8 == 7))
                    op("act", lambda: A.copy(out=gTt[:, q * 8:(q + 1) * 8, :].rearrange("p a b -> p (a b)"), in_=bank_bf(k)),
                       R=[psb[k]], W=[gTb])
                return gTt, gTb

            def stC3(tb, ti, g_, xr_):
                i = tb * 4 + ti
                gTt, gTb = g_
                xr, xrb_ = xr_
                xot, xob = xo.next()
                for hf in range(2):
                    k = 5 + hf
                    for kc in range(24):
                        op("pe", lambda: P.matmul(bank(k), gTt[:, kc, :], wout[:, kc, hf * 512:(hf + 1) * 512],
                                                  start=(kc == 0), stop=(kc == 23)), R=[gTb, cb], W=[psb[k]], inc=(kc == 23))
                    ytt, ytb = yt.next()
                    op("dve", lambda: V.tensor_tensor(out=ytt[:], in0=bank(k), in1=gmb[:, hf * 512:(hf + 1) * 512], op=ALU.mult),
                       R=[psb[k], gmbuf], W=[ytb])
                    op("pool", lambda: G.tensor_tensor(out=xot[:, hf * 512:(hf + 1) * 512], in0=ytt[:],
                                                       in1=xr[:, hf * 512:(hf + 1) * 512], op=ALU.add),
                       R=[ytb, xrb_], W=[xob])
                dma("sp", x_dst[i * 128:(i + 1) * 128, :], xot[:], R=[xob])

            for ti in range(4):
                stAb(ti, stAa(ldA(0, ti)))
            for tb in range(4):
                stB(tb)
                more = tb + 1 < 4
                xr_ = ldR(tb, 0)
                vn_ = stN(0)
                if more:
                    stAb(0, stAa(ldA(tb + 1, 0)))
                for ti in range(4):
                    nxt = ti + 1 < 4
                    if nxt:
                        xr_n = ldR(tb, ti + 1)
                        if more:
                            xa_n = ldA(tb + 1, ti + 1)
                    g_ = stC12(ti, vn_)
                    if nxt:
                        vn_ = stN(ti + 1)
                        if more:
                            fa_n = stAa(xa_n)
                    stC3(tb, ti, g_, xr_)
                    if nxt:
                        if more:
                            stAb(ti + 1, fa_n)
                        xr_ = xr_n
            kb.barrier()


    def mixer0_phase(x_dst):
        l = 0
        import os
        MS = os.environ.get("MIX_STOP", "")
        b3 = lambda ap, n: ap.unsqueeze(2).to_broadcast([128, n, 64])
        h3 = lambda ap: ap.rearrange("p (h e) -> p h e", e=64)
        with ExitStack() as t:
            G1m = kb.sb(t, "G1m", [128, D], F32)
            SHm = kb.sb(t, "SHm", [128, D], F32)
            gmb = kb.sb(t, "gmb", [128, D], F32)
            mbuf = Buf("modm")
            lgb = kb.sb(t, "lgb", [128, 16], F32)
            hgb = kb.sb(t, "hgb", [128, 192], F32)
            eps = kb.sb(t, "eps0", [128, 1], F32)
            one = kb.sb(t, "one0", [128, 1], F32)
            ret_out = kb.sb(t, "ret_out", [128, NT, 512], BF16)
            retb = [Buf("ret%d" % i) for i in range(NT)]
            KcT = kb.sb(t, "KcT", [128, 4, 256], BF16)
            Vc = kb.sb(t, "Vc", [128, 2, 512], BF16)
            ctxb = Buf("ctxkv")
            tb_ = Buf("tables")
            retg = hgb[:, 0:64]
            qg = hgb[:, 64:128]
            kg = hgb[:, 128:192]
            op("dve", lambda: V.memset(eps[:], EPS), W=[tb_])
            op("dve", lambda: V.memset(one[:], 1.0), W=[tb_])
            dma("sp", hgb[:], hg[0:1, :].partition_broadcast(128), W=[tb_])
            dma("sp", lgb[:], ret_decay[0:1, :].partition_broadcast(128), W=[tb_])
            op("dve", lambda: V.tensor_scalar(out=qg, in0=qg, scalar1=0.125, scalar2=None, op0=ALU.mult), R=[tb_], W=[tb_])
            op("act", lambda: A.activation(out=lgb[:], in_=lgb[:], func=AF.Exp), R=[tb_], W=[tb_])
            op("act", lambda: A.activation(out=lgb[:], in_=lgb[:], func=AF.Ln, bias=one[:, 0:1]), R=[tb_], W=[tb_])
            op("dve", lambda: V.tensor_scalar(out=lgb[:], in0=lgb[:], scalar1=-1.0, scalar2=None, op0=ALU.mult), R=[tb_], W=[tb_])
            with ExitStack() as t0:
                wring = Ring(kb, t0, "adaw", 2, [128, 8, 1024], BF16)
                bring = Ring(kb, t0, "adab", 2, [1, 1024], BF16)
                G1_, SH_, mb_ = make_mod(t0, l, 0, 1, nmix_g[l:l + 1, :], silc, "mm", wring, bring)
                op("pool", lambda: G.tensor_copy(G1m[:], G1_[:]), R=[mb_], W=[mbuf])
                op("pool", lambda: G.tensor_copy(SHm[:], SH_[:]), R=[mb_], W=[mbuf])
                ada_chunk(l, 2, silc, gmb[:], mbuf, wring, bring)
                kb.barrier()

            def norm_heads_T(src_bank, gain, tmps, dstK, dstKb):
                sqt, sqb = tmps["sq"].next()
                s8, s8b = tmps["s8"].next()
                op("act", lambda: A.activation(out=sqt[:], in_=bank(src_bank), func=AF.Square), R=[psb[src_bank]], W=[sqb])
                op("dve", lambda: V.tensor_reduce(out=s8[:, 0:8], in_=h3(sqt[:]), op=ALU.add, axis=AX.X), R=[sqb], W=[s8b])
                op("act", lambda: A.activation(out=s8[:, 8:16], in_=s8[:, 0:8], func=AF.Sqrt, scale=1.0 / 64, bias=eps[:, 0:1]),
                   R=[s8b, tb_], W=[s8b])
                op("dve", lambda: V.reciprocal(s8[:, 16:24], s8[:, 8:16]), R=[s8b], W=[s8b])
                op("dve", lambda: V.tensor_tensor(out=h3(sqt[:]), in0=h3(bank(src_bank)), in1=b3(s8[:, 16:24], 8), op=ALU.mult),
                   R=[psb[src_bank], s8b], W=[sqb])
                knb, knbb = tmps["knb"].next()
                op("pool", lambda: G.tensor_tensor(out=h3(knb[:]), in0=h3(sqt[:]), in1=gain.unsqueeze(1).to_broadcast([128, 8, 64]),
                                                   op=ALU.mult), R=[sqb, tb_], W=[knbb])
                k = tmps["ps"].next()
                for hp in range(4):
                    op("pe", lambda: P.transpose(bank_bf(k, 128, hp * 128), knb[:, hp * 128:(hp + 1) * 128], ident[:]),
                       R=[knbb, b_const], W=[psb[k]], inc=(hp == 3))
                op("dve", lambda: V.tensor_copy(dstK, bank_bf(k, 512).rearrange("p (a b) -> p a b", b=128)), R=[psb[k]], W=[dstKb])

            def make_tmps(tt, ps_banks):
                return {"sq": Ring(kb, tt, "nsq", 2, [128, 512], F32), "s8": Ring(kb, tt, "ns8", 2, [128, 24], F32),
                        "knb": Ring(kb, tt, "nknb", 2, [128, 512], BF16), "ps": PsRing(ps_banks)}

            def proj(hT, hTb, w, wb, c0, k):
                for dk in range(8):
                    op("pe", lambda: P.matmul(bank(k), hT[:, dk, :], w[:, dk, c0:c0 + 512], start=(dk == 0), stop=(dk == 7)),
                       R=[hTb, wb], W=[psb[k]], inc=(dk == 7))

            def rope(src, srcbufs, Ct, St, tabb, t1, t1b, t2, t2b):
                s5 = lambda ap: ap.rearrange("p (h b f q) -> p h b f q", h=8, b=2, f=2, q=16)
                S4 = St.rearrange("p (b f q) -> p b f q", b=2, f=2, q=16)
                op("dve", lambda: V.tensor_tensor(out=h3(t1), in0=h3(src), in1=Ct.unsqueeze(1).to_broadcast([128, 8, 64]), op=ALU.mult),
                   R=srcbufs + [tabb], W=[t1b])
                for f in range(2):
                    op("dve", lambda: V.tensor_tensor(out=s5(t2)[:, :, :, f, :], in0=s5(src)[:, :, :, 1 - f, :],
                                                      in1=S4[:, :, f, :].unsqueeze(1).to_broadcast([128, 8, 2, 16]), op=ALU.mult),
                       R=srcbufs + [tabb], W=[t2b])

            if MS == "tables":
                return
            with ExitStack() as tR:
                cstt = kb.sb(tR, "cstt", [128, 260], F32)
                MT = kb.sb(tR, "MT", [128, 8, 128], F32)
                dec = kb.sb(tR, "dec", [128, 4, 8], F32)
                CD = kb.sb(tR, "CD", [128, 2, 4], F32)
                ei = kb.sb(tR, "ei", [128, 36], F32)
                Sinit = kb.sb(tR, "Sinit", [128, 2, 256], F32)
                sib = Buf("Sinit")
                G1c = kb.sb(tR, "G1c", [128, D], F32)
                SHc = kb.sb(tR, "SHc", [128, D], F32)
                cbuf = Buf("modc")
                wq = kb.sb(tR, "w_qkvg", [128, 8, 2048], BF16)
                wqb = Buf("wq")
                wn = kb.sb(tR, "w_nkv", [128, 8, 1024], BF16)
                wnb = Buf("wn")
                DBt = kb.sb(tR, "DBt", [128, NT, 256], BF16)
                SBst = kb.sb(tR, "SBst", [128, NT, 256], BF16)
                dbb = [Buf("db%d" % i) for i in range(NT)]
                sbb = [Buf("sb%d" % i) for i in range(NT)]
                for q in range(4):
                    dma("pool", wq[:, :, q * 512:(q + 1) * 512], ab_w_in[:, q * 512:(q + 1) * 512].rearrange("(k p) n -> p k n", p=128), W=[wqb])
                for q in range(2):
                    dma("pool", wn[:, :, q * 512:(q + 1) * 512],
                        ab_w_in[:, 2560 + q * 512:2560 + (q + 1) * 512].rearrange("(k p) n -> p k n", p=128), W=[wnb])
                dma("sp", cstt[:], cst[:, :], W=[tb_])
                dma("sp", ei[:], exp_init[:, :], W=[tb_])
                DFt = cstt[:, 0:128]
                DBe = cstt[:, 128:256]
                with ExitStack() as t0:
                    wring = Ring(kb, t0, "adaw", 2, [128, 8, 1024], BF16)
                    bring = Ring(kb, t0, "adab", 2, [1, 1024], BF16)
                    G1_, SH_, mb_ = make_mod(t0, l, 0, 1, nmix_g[l:l + 1, :], silcc, "mc", wring, bring)
                    op("pool", lambda: G.tensor_copy(G1c[:], G1_[:]), R=[mb_], W=[cbuf])
                    op("pool", lambda: G.tensor_copy(SHc[:], SH_[:]), R=[mb_], W=[cbuf])
                    tmpM = kb.sb(t0, "tmpM", [128, 128], F32)
                    tmb = Buf("tmpM")
                    for h in range(8):
                        op("act", lambda: A.activation(out=MT[:, h, :], in_=DFt, func=AF.Exp, scale=lgb[:, h:h + 1]), R=[tb_], W=[tb_])
                        op("act", lambda: A.activation(out=tmpM[:], in_=DBe, func=AF.Exp, scale=lgb[:, 8 + h:9 + h]), R=[tb_], W=[tmb])
                        op("dve", lambda: V.tensor_tensor(out=MT[:, h, :], in0=MT[:, h, :], in1=tmpM[:], op=ALU.add), R=[tb_, tmb], W=[tb_])
                    for j, (c0, pc) in enumerate(((0, 256), (8, 257), (0, 258), (8, 259))):
                        op("act", lambda: A.activation(out=dec[:, j, :], in_=lgb[:, c0:c0 + 8], func=AF.Exp, scale=cstt[:, pc:pc + 1]),
                           R=[tb_], W=[tb_])
                    lg4 = lgb[:].rearrange("p (d q h) -> p d q h", d=2, q=4, h=2)
                    for hh in range(2):
                        ps_ = slice(hh * 64, (hh + 1) * 64)
                        op("act", lambda: A.activation(out=CD[ps_, :, :], in_=lg4[ps_, :, :, hh], func=AF.Exp, scale=128.0), R=[tb_], W=[tb_])
                    kb.barrier()
                if MS == "tables2":
                    dma("sp", x_dst[0:128, 0:1024], MT[:].rearrange("p a b -> p (a b)"), R=[tb_])
                    dma("sp", x_dst[128:256, 0:32], dec[:].rearrange("p a b -> p (a b)"), R=[tb_])
                    dma("sp", x_dst[128:256, 32:40], CD[:].rearrange("p a b -> p (a b)"), R=[tb_])
                    dma("sp", x_dst[128:256, 64:80], lgb[:], R=[tb_])
                    kb.barrier()
                    return
                fr = make_front(tR, [0])
                xin = Ring(kb, tR, "rxin", 2, [128, D], F32)
                ropt = Ring(kb, tR, "ropt", 2, [128, 256], F32)
                t1r = Ring(kb, tR, "t1r", 1, [128, 1024], F32)
                t2r = Ring(kb, tR, "t2r", 1, [128, 1024], F32)
                krr = Ring(kb, tR, "krr", 1, [128, 512], F32)
                wtr = Ring(kb, tR, "wtr", 2, [128, 2, 8], F32)
                kfr = Ring(kb, tR, "kfr", 2, [128, 2, 512], BF16)
                vbr = Ring(kb, tR, "vbr", 2, [128, 512], BF16)
                tmps = make_tmps(tR, [5])

                NO = 18
                def headO(i):
                    isctx = i >= 16
                    xt, xbuf = xin.next()
                    src = x_ctx[(i - 16) * 128:(i - 15) * 128, :] if isctx else x_oth[i * 128:(i + 1) * 128, :]
                    dma("sp", xt[:], src, W=[xbuf])
                    rt, rtb = ropt.next()
                    dma("sp", rt[:, 0:128], rope_oth[i * 128:(i + 1) * 128, :], W=[rtb])
                    fa = front_a(xt[:], xbuf, (G1c if isctx else G1m)[:], (SHc if isctx else SHm)[:], cbuf if isctx else mbuf, fr)
                    return xt, xbuf, rt, rtb, fa

                def bodyO(i, hd):
                    isctx = i >= 16
                    xt, xbuf, rt, rtb, fa = hd
                    hT, hTb, _, _ = front_b(fa, fr)
                    proj(hT, hTb, wq, wqb, 512, 1)
                    proj(hT, hTb, wq, wqb, 1024, 2)
                    t1, t1b = t1r.next()
                    t2, t2b = t2r.next()
                    rope(bank(1), [psb[1]], rt[:, 0:64], rt[:, 64:128], rtb, t1[:, 0:512], t1b, t2[:, 0:512], t2b)
                    kr, krb = krr.next()
                    op("pool", lambda: G.tensor_tensor(out=kr[:], in0=t1[:, 0:512], in1=t2[:, 0:512], op=ALU.add), R=[t1b, t2b], W=[krb])
                    wt, wtb = wtr.next()
                    for d_ in range(2):
                        op("act", lambda: A.activation(out=wt[:, d_, :], in_=lgb[:, d_ * 8:(d_ + 1) * 8], func=AF.Exp,
                                                       scale=ei[:, d_ * 18 + i:d_ * 18 + i + 1]), R=[tb_], W=[wtb])
                    kf, kfb = kfr.next()
                    for d_ in range(2):
                        op("pool", lambda: G.tensor_tensor(out=h3(kf[:, d_, :]), in0=h3(kr[:]), in1=b3(wt[:, d_, :], 8), op=ALU.mult),
                           R=[krb, wtb], W=[kfb])
                    vb, vbb = vbr.next()
                    op("act", lambda: A.copy(out=vb[:], in_=bank(2)), R=[psb[2]], W=[vbb])
                    for d_ in range(2):
                        kk = 6 + d_
                        for h in range(8):
                            hp, hh = h // 2, h % 2
                            op("pe", lambda: P.matmul(ps[hh * 64:(hh + 1) * 64, kk * 512 + hp * 64:kk * 512 + (hp + 1) * 64],
                                                      kf[:, d_, h * 64:(h + 1) * 64], vb[:, h * 64:(h + 1) * 64],
                                                      start=(i == 0 and h < 2), stop=(i == NO - 1), skip_group_check=True),
                               R=[kfb, vbb], W=[psb[kk]], inc=(h == 7))
                    if isctx:
                        proj(hT, hTb, wn, wnb, 0, 3)
                        proj(hT, hTb, wn, wnb, 512, 4)
                        norm_heads_T(3, kg, tmps, KcT[:, :, (i - 16) * 128:(i - 15) * 128], ctxb)
                        op("act", lambda: A.copy(out=Vc[:, i - 16, :], in_=bank(4)), R=[psb[4]], W=[ctxb])
                pipelined(NO, headO, bodyO)
                for d_ in range(2):
                    op("act", lambda: A.copy(out=Sinit[:, d_, :], in_=bank(6 + d_, 256)), R=[psb[6 + d_]], W=[sib])
                if MS == "sweepO":
                    dma("sp", x_dst[0:128, 0:512], Sinit[:].rearrange("p a b -> p (a b)"), R=[sib])
                    dma("sp", x_dst[128:256, 0:512].bitcast(BF16), KcT[:].rearrange("p a b -> p (a b)"), R=[ctxb])
                    dma("sp", x_dst[256:384, 0:512].bitcast(BF16), Vc[:].rearrange("p a b -> p (a b)"), R=[ctxb])
                    kb.barrier()
                    return

                def headL(c):
                    xt, xbuf = xin.next()
                    dma("sp", xt[:], x_loc[c * 128:(c + 1) * 128, :], W=[xbuf])
                    rt, rtb = ropt.next()
                    dma("sp", rt[:], rope_loc[c * 128:(c + 1) * 128, :], W=[rtb])
                    fa = front_a(xt[:], xbuf, G1m[:], SHm[:], mbuf, fr)
                    return xt, xbuf, rt, rtb, fa

                def bodyE1(c, hd):
                    xt, xbuf, rt, rtb, fa = hd
                    hT, hTb, _, _ = front_b(fa, fr)
                    proj(hT, hTb, wq, wqb, 512, 1)
                    proj(hT, hTb, wq, wqb, 1024, 2)
                    t1, t1b = t1r.next()
                    t2, t2b = t2r.next()
                    rope(bank(1), [psb[1]], rt[:, 64:128], rt[:, 192:256], rtb, t1[:, 0:512], t1b, t2[:, 0:512], t2b)
                    kr, krb = krr.next()
                    op("pool", lambda: G.tensor_tensor(out=kr[:], in0=t1[:, 0:512], in1=t2[:, 0:512], op=ALU.add), R=[t1b, t2b], W=[krb])
                    kf, kfb = kfr.next()
                    op("pool", lambda: G.tensor_tensor(out=h3(kf[:, 0, :]), in0=h3(kr[:]), in1=b3(dec[:, 3, :], 8), op=ALU.mult),
                       R=[krb, tb_], W=[kfb])
                    vb, vbb = vbr.next()
                    op("act", lambda: A.copy(out=vb[:], in_=bank(2)), R=[psb[2]], W=[vbb])
                    kk = 3 + (c % 2)
                    for h in range(8):
                        hp, hh = h // 2, h % 2
                        op("pe", lambda: P.matmul(ps[hh * 64:(hh + 1) * 64, kk * 512 + hp * 64:kk * 512 + (hp + 1) * 64],
                                                  kf[:, 0, h * 64:(h + 1) * 64], vb[:, h * 64:(h + 1) * 64], start=True, stop=True),
                           R=[kfb, vbb], W=[psb[kk]], inc=(h == 7))
                    op("act", lambda: A.copy(out=DBt[:, c, :], in_=bank(kk, 256)), R=[psb[kk]], W=[dbb[c]])
                pipelined(NT, headL, bodyE1)
                srun = Ring(kb, tR, "srun", 2, [128, 256], F32)
                stmp = Ring(kb, tR, "stmp", 2, [128, 256], F32)
                q4 = lambda ap: ap.rearrange("p (q e) -> p q e", e=64)
                cdb = lambda d_: CD[:, d_, :].unsqueeze(2).to_broadcast([128, 4, 64])
                cur, curb = srun.next()
                op("dve", lambda: V.tensor_copy(cur[:], Sinit[:, 1, :]), R=[sib], W=[curb])
                for c in range(NT - 1, -1, -1):
                    op("act", lambda: A.copy(out=SBst[:, c, :], in_=cur[:]), R=[curb], W=[sbb[c]])
                    if c == 0:
                        break
                    tm, tmb_ = stmp.next()
                    op("pool", lambda: G.tensor_tensor(out=q4(tm[:]), in0=q4(cur[:]), in1=cdb(1), op=ALU.mult), R=[curb, tb_], W=[tmb_])
                    nxt, nxtb = srun.next()
                    op("dve", lambda: V.tensor_tensor(out=nxt[:], in0=tm[:], in1=DBt[:, c, :], op=ALU.add), R=[tmb_, dbb[c]], W=[nxtb])
                    cur, curb = nxt, nxtb
                if MS == "sweepE1":
                    for c in range(NT):
                        dma("sp", x_dst[c * 128:(c + 1) * 128, 0:128].bitcast(BF16), SBst[:, c, :], R=[sbb[c]])
                    kb.barrier()
                    return

                qkr_r = Ring(kb, tR, "qkr", 2, [128, 1024], BF16)
                qkT_r = Ring(kb, tR, "qkT", 2, [128, 8, 128], BF16)
                qz_r = Ring(kb, tR, "qz", 2, [128, 2, 4, 128], BF16)
                for qi_ in range(2):
                    op("pool", lambda: G.memset(qz_r.t[qi_][:], 0.0), W=[qz_r.b[qi_]])
                gs_r = Ring(kb, tR, "gsr", 2, [128, 512], BF16)
                gs2_r = Ring(kb, tR, "gs2r", 2, [128, 512], BF16)
                Pm_r = Ring(kb, tR, "Pmr", 1, [128, 8, 128], BF16)
                o_r = Ring(kb, tR, "or", 2, [128, 512], F32)
                s8r = Ring(kb, tR, "rs8", 2, [128, 24], F32)
                SFr = Ring(kb, tR, "SFr", 2, [128, 256], F32)
                SFbr = Ring(kb, tR, "SFbr", 2, [128, 256], BF16)
                SF, SFb_ = SFr.next()
                op("dve", lambda: V.tensor_copy(SF[:], Sinit[:, 0, :]), R=[sib], W=[SFb_])
                SFh, SFhb = SFbr.next()
                op("act", lambda: A.copy(out=SFh[:], in_=SF[:]), R=[SFb_], W=[SFhb])
                RS_ = {"SF": SF, "SFb": SFb_, "SFh": SFh, "SFhb": SFhb}

                def bodyR(c, hd):
                    xt, xbuf, rt, rtb, fa = hd
                    SF, SFb_, SFh, SFhb = RS_["SF"], RS_["SFb"], RS_["SFh"], RS_["SFhb"]
                    hT, hTb, _, _ = front_b(fa, fr)
                    for j in range(4):
                        proj(hT, hTb, wq, wqb, j * 512, 1 + j)
                    t1, t1b = t1r.next()
                    t2, t2b = t2r.next()
                    for j in range(2):
                        rope(bank(1 + j), [psb[1 + j]], rt[:, j * 64:(j + 1) * 64], rt[:, 128 + j * 64:128 + (j + 1) * 64], rtb,
                             t1[:, j * 512:(j + 1) * 512], t1b, t2[:, j * 512:(j + 1) * 512], t2b)
                    qkr, qkrb = qkr_r.next()
                    op("pool", lambda: G.tensor_tensor(out=qkr[:], in0=t1[:], in1=t2[:], op=ALU.add), R=[t1b, t2b], W=[qkrb])
                    vb, vbb = vbr.next()
                    op("act", lambda: A.copy(out=vb[:], in_=bank(3)), R=[psb[3]], W=[vbb])
                    gs, gsb = gs_r.next()
                    op("act", lambda: A.activation(out=gs[:], in_=bank(4), func=AF.Silu), R=[psb[4]], W=[gsb])
                    gs2, gs2b = gs2_r.next()
                    op("pool", lambda: G.tensor_tensor(out=h3(gs2[:]), in0=h3(gs[:]), in1=retg.unsqueeze(1).to_broadcast([128, 8, 64]),
                                                       op=ALU.mult), R=[gsb, tb_], W=[gs2b])
                    kf, kfb = kfr.next()
                    op("pool", lambda: G.tensor_tensor(out=h3(kf[:, 0, :]), in0=h3(qkr[:, 512:1024]), in1=b3(dec[:, 2, :], 8), op=ALU.mult),
                       R=[qkrb, tb_], W=[kfb])
                    qkT, qkTb = qkT_r.next()
                    for j in range(8):
                        op("pe", lambda: P.transpose(bank_bf(0, 128, j * 128), qkr[:, j * 128:(j + 1) * 128], ident[:]),
                           R=[qkrb, b_const], W=[psb[0]], inc=(j == 7))
                    op("dve", lambda: V.tensor_copy(qkT[:].rearrange("p a b -> p (a b)"), bank_bf(0)), R=[psb[0]], W=[qkTb])
                    qz, qzb = qz_r.next()
                    for hh in range(2):
                        pr = slice(hh * 64, (hh + 1) * 64)
                        op("pool", lambda: G.tensor_copy(qz[pr, hh, :, :], qkT[pr, 0:4, :]), R=[qkTb], W=[qzb])
                    for h in range(8):
                        hp, hh = h // 2, h % 2
                        op("pe", lambda: P.matmul(bank(5 + h // 4, 128, (h % 4) * 128), qkT[:, 4 + hp, :], qz[:, hh, hp, :], start=True, stop=True),
                           R=[qkTb, qzb], W=[psb[5 + h // 4]], inc=(h % 4 == 3))
                    Pm, Pmb = Pm_r.next()
                    for b_ in range(2):
                        op("dve", lambda: V.tensor_tensor(out=Pm[:, 4 * b_:4 * b_ + 4, :], in0=bank(5 + b_).rearrange("p (a b) -> p a b", b=128),
                                                          in1=MT[:, 4 * b_:4 * b_ + 4, :], op=ALU.mult), R=[psb[5 + b_], tb_], W=[Pmb])
                    for h in range(8):
                        op("pe", lambda: P.matmul(bank(7, 64, h * 64), Pm[:, h, :], vb[:, h * 64:(h + 1) * 64], start=True, stop=True),
                           R=[Pmb, vbb], W=[psb[7]], inc=(h == 7))
                    for h in range(8):
                        hp, hh = h // 2, h % 2
                        op("pe", lambda: P.matmul(bank(3, 64, h * 64), qz[:, hh, hp, :], SFh[:, hp * 64:(hp + 1) * 64], start=True, stop=True),
                           R=[qzb, SFhb], W=[psb[3]], inc=(h == 7))
                    for h in range(8):
                        hp, hh = h // 2, h % 2
                        op("pe", lambda: P.matmul(bank(4, 64, h * 64), qz[:, hh, hp, :], SBst[:, c, hp * 64:(hp + 1) * 64], start=True, stop=True),
                           R=[qzb, sbb[c]], W=[psb[4]], inc=(h == 7))
                    o1, o1b = o_r.next()
                    o2, o2b = o_r.next()
                    op("dve", lambda: V.tensor_tensor(out=h3(o1[:]), in0=h3(bank(3)), in1=b3(dec[:, 0, :], 8), op=ALU.mult), R=[psb[3], tb_], W=[o1b])
                    op("dve", lambda: V.tensor_tensor(out=h3(o2[:]), in0=h3(bank(4)), in1=b3(dec[:, 1, :], 8), op=ALU.mult), R=[psb[4], tb_], W=[o2b])
                    op("dve", lambda: V.tensor_tensor(out=o1[:], in0=bank(7), in1=o1[:], op=ALU.add), R=[psb[7], o1b], W=[o1b])
                    op("pool", lambda: G.tensor_tensor(out=o1[:], in0=o1[:], in1=o2[:], op=ALU.add), R=[o1b, o2b], W=[o1b])
                    op("pool", lambda: G.tensor_tensor(out=o2[:], in0=o1[:], in1=o1[:], op=ALU.mult), R=[o1b], W=[o2b])
                    s8, s8b = s8r.next()
                    op("dve", lambda: V.tensor_reduce(out=s8[:, 0:8], in_=h3(o2[:]), op=ALU.add, axis=AX.X), R=[o2b], W=[s8b])
                    op("act", lambda: A.activation(out=s8[:, 8:16], in_=s8[:, 0:8], func=AF.Sqrt, scale=1.0 / 64, bias=eps[:, 0:1]),
                       R=[s8b, tb_], W=[s8b])
                    op("dve", lambda: V.reciprocal(s8[:, 16:24], s8[:, 8:16]), R=[s8b], W=[s8b])
                    op("pool", lambda: G.tensor_tensor(out=h3(o2[:]), in0=h3(o1[:]), in1=b3(s8[:, 16:24], 8), op=ALU.mult), R=[o1b, s8b], W=[o2b])
                    op("pool", lambda: G.tensor_tensor(out=ret_out[:, c, :], in0=o2[:], in1=gs2[:], op=ALU.mult), R=[o2b, gs2b], W=[retb[c]])
                    if c < NT - 1:
                        for h in range(8):
                            hp, hh = h // 2, h % 2
                            op("pe", lambda: P.matmul(ps[hh * 64:(hh + 1) * 64, 1 * 512 + hp * 64:1 * 512 + (hp + 1) * 64],
                                                      kf[:, 0, h * 64:(h + 1) * 64], vb[:, h * 64:(h + 1) * 64], start=True, stop=True),
                               R=[kfb, vbb], W=[psb[1]], inc=(h == 7))
                        tm, tmb_ = stmp.next()
                        op("pool", lambda: G.tensor_tensor(out=q4(tm[:]), in0=q4(SF[:]), in1=cdb(0), op=ALU.mult), R=[SFb_, tb_], W=[tmb_])
                        SF, SFb_ = SFr.next()
                        op("dve", lambda: V.tensor_tensor(out=SF[:], in0=bank(1, 256), in1=tm[:], op=ALU.add), R=[psb[1], tmb_], W=[SFb_])
                        SFh, SFhb = SFbr.next()
                        op("act", lambda: A.copy(out=SFh[:], in_=SF[:]), R=[SFb_], W=[SFhb])
                        RS_.update({"SF": SF, "SFb": SFb_, "SFh": SFh, "SFhb": SFhb})
                pipelined(NT, headL, bodyR)
                kb.barrier()
            if MS == "sweepR":
                for c in range(NT):
                    dma("sp", x_dst[c * 128:(c + 1) * 128, 0:256].bitcast(BF16), ret_out[:, c, :], R=[retb[c]])
                kb.barrier()
                return

            with ExitStack() as tN:
                wn3 = kb.sb(tN, "w_n3", [128, 8, 1536], BF16)
                wn3b = Buf("wn3")
                wo = kb.sb(tN, "w_o", [128, 8, D], BF16)
                wob = Buf("wo")
                KT = kb.sb(tN, "KT", [128, 4, 20 * 128], BF16)
                VE = kb.sb(tN, "VE", [128, 20, 512], BF16)
                kvb = [Buf("kv%d" % i) for i in range(20)]
                biasT = kb.sb(tN, "biasT", [128, 8, 896], BF16)
                rowm = kb.sb(tN, "rowm", [2, 192], BF16)
                A2 = kb.sb(tN, "A2", [2, 128], BF16)
                nb_ = Buf("naconst")
                for q in range(3):
                    dma("pool", wn3[:, :, q * 512:(q + 1) * 512],
                        ab_w_in[:, 2048 + q * 512:2048 + (q + 1) * 512].rearrange("(k p) n -> p k n", p=128), W=[wn3b])
                for q in range(2):
                    dma("pool", wo[:, :, q * 512:(q + 1) * 512], ab_w_out[:, q * 512:(q + 1) * 512].rearrange("(k p) n -> p k n", p=128), W=[wob])
                for h in range(8):
                    dma("pool", biasT[:, h, :], bias_tab[h, :, :], W=[nb_])
                dma("pool", rowm[:], rowmask[:, :], W=[nb_])
                dma("pool", A2[:], a2_d[:, :], W=[nb_])
                fr = make_front(tN, [0])
                xin = Ring(kb, tN, "nxin", 2, [128, D], F32)
                tmps = make_tmps(tN, [3])
                def headE2(e_):
                    xt, xbuf = xin.next()
                    if e_ < 2:
                        src = x_halo[e_ * 128:(e_ + 1) * 128, :]
                    elif e_ < 18:
                        src = x_loc[(e_ - 2) * 128:(e_ - 1) * 128, :]
                    else:
                        src = x_halo[256 + (e_ - 18) * 128:256 + (e_ - 17) * 128, :]
                    dma("sp", xt[:], src, W=[xbuf])
                    return xt, xbuf, front_a(xt[:], xbuf, G1m[:], SHm[:], mbuf, fr)

                def bodyE2(e_, hd):
                    xt, xbuf, fa = hd
                    hT, hTb, _, _ = front_b(fa, fr)
                    proj(hT, hTb, wn3, wn3b, 512, 1)
                    proj(hT, hTb, wn3, wn3b, 1024, 2)
                    norm_heads_T(1, kg, tmps, KT[:, :, e_ * 128:(e_ + 1) * 128], kvb[e_])
                    op("act", lambda: A.copy(out=VE[:, e_, :], in_=bank(2)), R=[psb[2]], W=[kvb[e_]])
                pipelined(20, headE2, bodyE2)
                if MS == "sweepE2":
                    for e_ in range(16):
                        dma("sp", x_dst[e_ * 128:(e_ + 1) * 128, 0:256].bitcast(BF16), KT[:, :, e_ * 128:(e_ + 1) * 128], R=[kvb[e_]])
                        dma("sp", x_dst[e_ * 128:(e_ + 1) * 128, 256:512].bitcast(BF16), VE[:, e_, :], R=[kvb[e_]])
                    kb.barrier()
                    return
                qnT_r = Ring(kb, tN, "qnT", 2, [128, 4, 128], BF16)
                qnz_r = Ring(kb, tN, "qnz", 2, [128, 2, 4, 128], BF16)
                for qi_ in range(2):
                    op("pool", lambda: G.memset(qnz_r.t[qi_][:], 0.0), W=[qnz_r.b[qi_]])
                Pe_r = Ring(kb, tN, "Pe", 2, [128, 1024], BF16)
                PT_r = Ring(kb, tN, "PT", 2, [128, 8, 128], BF16)
                sm_r = Ring(kb, tN, "sm", 2, [128, 24], F32)
                mix_r = Ring(kb, tN, "mix", 2, [128, D], BF16)
                mixT_r = Ring(kb, tN, "mixT", 2, [128, 8, 128], BF16)
                yt_r = Ring(kb, tN, "nyt", 2, [128, 512], F32)
                xo_r = Ring(kb, tN, "nxo", 2, [128, D], F32)
                ab_ring = PsRing([2, 4])
                def headN(t_):
                    xt, xbuf = xin.next()
                    dma("sp", xt[:], x_loc[t_ * 128:(t_ + 1) * 128, :], W=[xbuf])
                    return xt, xbuf, front_a(xt[:], xbuf, G1m[:], SHm[:], mbuf, fr)

                def geomN(t_):
                    lo, hi = t_ - 2, t_ + 2
                    if t_ < 2:
                        hi = 3
                    if t_ > 13:
                        lo = 12
                    n = hi - lo + 1
                    return lo, hi, n, (n - 4) * 128

                def stA(t_, hd):
                    xt, xbuf, fa = hd
                    hT, hTb, _, _ = front_b(fa, fr)
                    proj(hT, hTb, wn3, wn3b, 0, 1)
                    qnT, qnTb = qnT_r.next()
                    tmps["ps"] = PsRing([6])
                    norm_heads_T(1, qg, tmps, qnT[:], qnTb)
                    qnz, qnzb = qnz_r.next()
                    for hh in range(2):
                        pr = slice(hh * 64, (hh + 1) * 64)
                        op("pool", lambda: G.tensor_copy(qnz[pr, hh, :, :], qnT[pr, :, :]), R=[qnTb], W=[qnzb])
                    sm, smb = sm_r.next()
                    return xt, xbuf, qnz, qnzb, sm, smb

                def stS(t_, cx, h):
                    xt, xbuf, qnz, qnzb, sm, smb = cx
                    lo, hi, n, nbk = geomN(t_)
                    kv_need = [kvb[j + 2] for j in range(lo, hi + 1)]
                    hp, hh = h // 2, h % 2
                    ka = ab_ring.next()
                    kbk = ka + 1
                    e0 = (lo + 2) * 128
                    d0 = (lo - t_ + 3) * 128
                    op("pe", lambda: P.matmul(bank(ka), qnz[:, hh, hp, :], KT[:, hp, e0:e0 + 512], start=True, stop=False),
                       R=[qnzb] + kv_need, W=[psb[ka]], inc=False)
                    op("pe", lambda: P.matmul(bank(ka), ident[:], biasT[:, h, d0:d0 + 512], start=False, stop=False),
                       R=[nb_, b_const], W=[psb[ka]], inc=False)
                    op("pe", lambda: P.matmul(bank(ka), A2[:], rowm[:, t_ * 12:t_ * 12 + 8].unsqueeze(2).to_broadcast([2, 8, 64]),
                                              start=False, stop=True), R=[nb_], W=[psb[ka]])
                    op("pe", lambda: P.matmul(bank(kbk, nbk), qnz[:, hh, hp, :], KT[:, hp, e0 + 512:e0 + 512 + nbk], start=True, stop=False),
                       R=[qnzb] + kv_need, W=[psb[kbk]], inc=False)
                    op("pe", lambda: P.matmul(bank(kbk, nbk), ident[:], biasT[:, h, d0 + 512:d0 + 512 + nbk], start=False, stop=False),
                       R=[nb_, b_const], W=[psb[kbk]], inc=False)
                    op("pe", lambda: P.matmul(bank(kbk, nbk), A2[:],
                                              rowm[:, t_ * 12 + 8:t_ * 12 + 8 + (n - 4) * 2].unsqueeze(2).to_broadcast([2, (n - 4) * 2, 64]),
                                              start=False, stop=True), R=[nb_], W=[psb[kbk]], inc=False)
                    op("pe", lambda: P.matmul(bank(kbk, 256, nbk), qnz[:, hh, hp, :], KcT[:, hp, :], start=True, stop=True),
                       R=[qnzb, ctxb], W=[psb[kbk]])
                    return ka, kbk

                def stETV(t_, cx, h, ka, kbk):
                    xt, xbuf, qnz, qnzb, sm, smb = cx
                    lo, hi, n, nbk = geomN(t_)
                    kv_need = [kvb[j + 2] for j in range(lo, hi + 1)]
                    Pe, Peb = Pe_r.next()
                    op("act", lambda: A.activation(out=Pe[:, 0:512], in_=bank(ka), func=AF.Exp, accum_out=sm[:, 2 * h:2 * h + 1]),
                       R=[psb[ka]], W=[Peb, smb])
                    op("act", lambda: A.activation(out=Pe[:, 512:768 + nbk], in_=bank(kbk, nbk + 256), func=AF.Exp,
                                                   accum_out=sm[:, 2 * h + 1:2 * h + 2]), R=[psb[kbk]], W=[Peb, smb])
                    nblk = n + 2
                    for bk in range(nblk):
                        op("pe", lambda: P.transpose(bank_bf(6, 128, bk * 128), Pe[:, bk * 128:(bk + 1) * 128], ident[:]),
                           R=[Peb, b_const], W=[psb[6]], inc=(bk == nblk - 1))
                    PT, PTb = PT_r.next()
                    op("dve", lambda: V.tensor_copy(PT[:, 0:nblk, :].rearrange("p a b -> p (a b)"), bank_bf(6, nblk * 128)), R=[psb[6]], W=[PTb])
                    for bk in range(nblk):
                        if bk < n:
                            rhs = VE[:, lo + 2 + bk, h * 64:(h + 1) * 64]
                        else:
                            rhs = Vc[:, bk - n, h * 64:(h + 1) * 64]
                        op("pe", lambda: P.matmul(bank(7, 64, h * 64), PT[:, bk, :], rhs, start=(bk == 0), stop=(bk == nblk - 1)),
                           R=[PTb, ctxb] + kv_need, W=[psb[7]], inc=(bk == nblk - 1))

                def stF(t_, cx):
                    xt, xbuf, qnz, qnzb, sm, smb = cx
                    op("dve", lambda: V.tensor_reduce(out=sm[:, 16:24], in_=sm[:, 0:16].rearrange("p (h two) -> p h two", two=2),
                                                      op=ALU.add, axis=AX.X), R=[smb], W=[smb])
                    op("dve", lambda: V.reciprocal(sm[:, 16:24], sm[:, 16:24]), R=[smb], W=[smb])
                    mix, mixb = mix_r.next()
                    op("dve", lambda: V.tensor_tensor(out=h3(mix[:, 512:1024]), in0=h3(bank(7)), in1=b3(sm[:, 16:24], 8), op=ALU.mult),
                       R=[psb[7], smb], W=[mixb])
                    op("pool", lambda: G.tensor_copy(mix[:, 0:512], ret_out[:, t_, :]), R=[retb[t_]], W=[mixb])
                    mixT, mixTb = mixT_r.next()
                    for dk in range(8):
                        op("pe", lambda: P.transpose(bank_bf(0, 128, dk * 128), mix[:, dk * 128:(dk + 1) * 128], ident[:]),
                           R=[mixb, b_const], W=[psb[0]], inc=(dk == 7))
                    op("act", lambda: A.copy(out=mixT[:].rearrange("p a b -> p (a b)"), in_=bank_bf(0)), R=[psb[0]], W=[mixTb])
                    xo, xob = xo_r.next()
                    for hf in range(2):
                        k = 1 if hf == 0 else 6
                        for dk in range(8):
                            op("pe", lambda: P.matmul(bank(k), mixT[:, dk, :], wo[:, dk, hf * 512:(hf + 1) * 512], start=(dk == 0), stop=(dk == 7)),
                               R=[mixTb, wob], W=[psb[k]], inc=(dk == 7))
                        yt, ytb = yt_r.next()
                        op("dve", lambda: V.tensor_tensor(out=yt[:], in0=bank(k), in1=gmb[:, hf * 512:(hf + 1) * 512], op=ALU.mult),
                           R=[psb[k], mbuf], W=[ytb])
                        op("pool", lambda: G.tensor_tensor(out=xo[:, hf * 512:(hf + 1) * 512], in0=yt[:], in1=xt[:, hf * 512:(hf + 1) * 512],
                                                           op=ALU.add), R=[ytb, xbuf], W=[xob])
                    dma("sp", x_dst[t_ * 128:(t_ + 1) * 128, :], xo[:], R=[xob])
                hdsN = {0: headN(0)}
                cxN = {0: stA(0, hdsN[0])}
                for t_ in range(NT):
                    if t_ + 1 < NT:
                        hdsN[t_ + 1] = headN(t_ + 1)
                    cx = cxN.pop(t_)
                    pend = stS(t_, cx, 0)
                    for h in range(8):
                        cur = pend
                        if h < 7:
                            pend = stS(t_, cx, h + 1)
                        if h == 3 and t_ + 1 < NT:
                            cxN[t_ + 1] = stA(t_ + 1, hdsN.pop(t_ + 1))
                        stETV(t_, cx, h, *cur)
                    stF(t_, cx)
                kb.barrier()

    if mode == 'full':
        mixer0_phase(xa)
        moe_phase(0, xa, xb)
        sgu_phase(xb, xc)
        moe_phase(1, xc, y_out)
    elif mode == 'mix0':
        mixer0_phase(y_out)
    elif mode == 'pro':
        with ExitStack() as t:
            wring = Ring(kb, t, "adaw", 2, [128, 8, 1024], BF16)
            bring = Ring(kb, t, "adab", 2, [1, 1024], BF16)
            o1 = kb.sb(t, "o1", [128, D], F32)
            ob = Buf("o1")
            ada_chunk(0, 2, silc, o1[:], ob, wring, bring)
            dma("sp", y_out[0:128, :], o1[:], R=[ob])
            G1, SH, mb = make_mod(t, 1, 3, 4, nffn_g[1:2, :], silcc, "mf", wring, bring)
            dma("sp", y_out[128:256, :], G1[:], R=[mb])
            dma("sp", y_out[256:384, :], SH[:], R=[mb])
            kb.barrier()
    elif mode == 'moe0':
        moe_phase(0, dbg_x, y_out)
    elif mode == 'sgu':
        sgu_phase(dbg_x, y_out)
    elif mode == 'moe1':
        moe_phase(1, dbg_x, y_out)
    kb.barrier()
    return kb


def _bf(a):
    return np.ascontiguousarray(a).astype(np.float32)


def _rope_tables(pos, scale_k):
    inv = (10000.0 ** (-np.arange(16, dtype=np.float32) / 16.0)).astype(np.float32)
    rows = (pos // 64).astype(np.float32)
    cols = (pos % 64).astype(np.float32)
    ar = rows[:, None] * inv[None, :]
    ac = cols[:, None] * inv[None, :]
    cr, sr, cc_, sc = np.cos(ar), np.sin(ar), np.cos(ac), np.sin(ac)
    C = np.concatenate([cr, cr, cc_, cc_], axis=1).astype(np.float32)
    S = np.concatenate([-sr, sr, -sc, sc], axis=1).astype(np.float32)
    return C, S


def prep_inputs(inputs):
    f = lambda k: np.asarray(inputs[k], dtype=np.float32)
    x, c, ctx, c_ctx = f('x'), f('c'), f('ctx'), f('c_ctx')
    w_in = f('ab_w_in')[0]
    blk = lambda i: w_in[:, i * 512:(i + 1) * 512]
    w_in_p = np.ascontiguousarray(np.concatenate([blk(4), blk(0), blk(1), blk(5), blk(6), blk(2), blk(3)], axis=1))
    rpb = f('na_rpb')[0]
    tab = np.full((8, 2, 64, 7, 2, 64), NEG, dtype=np.float32)
    w = np.arange(64)
    c0 = np.clip(w - 8, 0, 48)
    for a in range(2):
        for di in range(7):
            for b in range(2):
                rel = 2 * di + b - a + 1
                if rel < 0 or rel > 14:
                    continue
                for wi in range(64):
                    ccs = np.arange(c0[wi], c0[wi] + 16)
                    tab[:, a, wi, di, b, ccs] = rpb[:, rel, ccs - wi + 15]
    tab = tab.reshape(8, 128, 896)
    shared = {
        'ada_w': f('ada_w'), 'ada_b': f('ada_b'), 'norm_mix_g': f('norm_mix_g'), 'norm_ffn_g': f('norm_ffn_g'),
        'router_w': f('router_w'), 'router_bias': f('router_bias').reshape(1, 16),
        'moe_w_gate': f('moe_w_gate'), 'moe_w_up': f('moe_w_up'), 'moe_w_down': f('moe_w_down'),
        'ab_w_in': w_in_p, 'ab_w_out': f('ab_w_out')[0], 'ret_decay': f('ret_decay')[0].reshape(1, 16),
        'head_g': np.concatenate([f('ret_norm_g')[0], f('na_q_g')[0], f('na_k_g')[0]]).reshape(1, 192),
        'bias_tab': tab,
        'sgu_w_in': f('sgu_w_in')[0], 'sgu_b_in': f('sgu_b_in')[0].reshape(1, 6144),
        'sgu_norm_g': f('sgu_norm_g')[0].reshape(1, 3072), 'sgu_w_s': f('sgu_w_s')[0],
        'sgu_bsT': np.ascontiguousarray(f('sgu_b_s')[0].T), 'sgu_w_out': f('sgu_w_out')[0],
        'cc_row': c_ctx.reshape(1, D),
        'ident': np.eye(128, dtype=np.float32),
        'a2': np.repeat(np.eye(2, dtype=np.float32), 64, axis=1),
    }
    jj = np.arange(128, dtype=np.float32)[:, None]
    ii = np.arange(128, dtype=np.float32)[None, :]
    DF = np.where(ii >= jj, ii - jj, BIG).astype(np.float32)
    DB = np.where(jj >= ii, jj - ii, BIG).astype(np.float32)
    p = np.arange(128, dtype=np.float32)
    pos = np.stack([p + 1, 128 - p, 127 - p, p], axis=1)
    shared['cst'] = np.concatenate([DF, DB, pos], axis=1).astype(np.float32)
    maps = []
    for core in range(8):
        b, s = core // 2, core % 2
        lo = s * TOK
        m = dict(shared)
        m['x_loc'] = np.ascontiguousarray(x[b, lo:lo + TOK])
        m['x_oth'] = np.ascontiguousarray(x[b, (1 - s) * TOK:(2 - s) * TOK])
        halo = np.zeros((512, D), np.float32)
        if s == 1:
            halo[0:256] = x[b, lo - 256:lo]
        else:
            halo[256:512] = x[b, lo + TOK:lo + TOK + 256]
        m['x_halo'] = halo
        m['x_ctx'] = np.ascontiguousarray(ctx[b])
        m['c_row'] = c[b].reshape(1, D)
        C, S = _rope_tables(np.arange(lo, lo + TOK), 0.125)
        m['rope_loc'] = np.concatenate([C, C * 0.125, S, S * 0.125], axis=1).astype(np.float32)
        Co, So = _rope_tables(np.arange((1 - s) * TOK, (2 - s) * TOK), 0.125)
        ro = np.concatenate([Co * 0.125, So * 0.125], axis=1)
        rc = np.concatenate([np.full((256, 64), 0.125, np.float32), np.zeros((256, 64), np.float32)], axis=1)
        m['rope_oth'] = np.concatenate([ro, rc], axis=0).astype(np.float32)
        mo = np.arange(TOK, dtype=np.float32)
        n = np.arange(256, dtype=np.float32)
        if s == 1:
            EFo = 2047.0 - mo
            EBo = np.full(TOK, BIG, np.float32)
        else:
            EFo = np.full(TOK, BIG, np.float32)
            EBo = mo
        EFc = 255.0 - n + 2048.0 * s
        EBc = n + 2048.0 * (1 - s)
        EF = np.concatenate([EFo, EFc]).reshape(18, 128).T
        EB = np.concatenate([EBo, EBc]).reshape(18, 128).T
        m['exp_init'] = np.ascontiguousarray(np.concatenate([EF, EB], axis=1)).astype(np.float32)
        rm = np.full((2, 16, 6, 2), NEG, np.float32)
        for t in range(16):
            lo_t, hi_t = t - 2, t + 2
            if t < 2:
                hi_t = 3
            if t > 13:
                lo_t = 12
            for a in range(2):
                r = 32 * s + 2 * t + a
                r0 = min(max(r - 4, 0), 56)
                for sl, j in enumerate(range(lo_t, hi_t + 1)):
                    for bb in range(2):
                        kr = 32 * s + 2 * j + bb
                        if r0 <= kr <= r0 + 7:
                            rm[a, t, sl, bb] = 0.0
        m['rowmask'] = rm.reshape(2, 192)
        m['dbg_x'] = np.zeros((TOK, D), np.float32)
        maps.append(m)
    return maps


_CACHE = {}


def kernel(**inputs):
    maps = prep_inputs(inputs)
    if 'full' not in _CACHE:
        _CACHE['full'] = build('full')
    kb = _CACHE['full']
    maps = [{k: v for k, v in m.items() if k in kb.declared} for m in maps]
    res = run_bass_kernel_spmd(kb.nc, maps, core_ids=list(range(8)))
    out = np.zeros((4, 4096, D), np.float32)
    for core in range(8):
        b, s = core // 2, core % 2
        out[b, s * TOK:(s + 1) * TOK] = res.results[core]['y_out']
    return out
```

```python
import numpy as np
import ml_dtypes
from contextlib import ExitStack
import concourse.bass as bass
import concourse.mybir as mybir
from concourse.bass_utils import run_bass_kernel_spmd

F32 = mybir.dt.float32
BF16 = mybir.dt.bfloat16
AF = mybir.ActivationFunctionType
ALU = mybir.AluOpType
AX = mybir.AxisListType

D = 1024
NT = 16
TOK = 2048
NEG = -30000.0
EPS = 1e-6
BIG = 1.0e9


class Buf:
    __slots__ = ("name", "w", "r", "excl")

    def __init__(self, name="", excl=False):
        self.name = name
        self.w = None
        self.r = []
        self.excl = excl


class Eng:
    def __init__(self, name, h, sem):
        self.name = name
        self.h = h
        self.sem = sem
        self.count = 0
        self.waited = {}
        self.dsems = []
        self.dcnt = []
        self.di = 0


class KB:
    def __init__(self):
        self.nc = bass.Bass("TRN2", target_bir_lowering=False)
        self.es = ExitStack()
        nc = self.nc
        self.E = {}
        for name, h in (("pe", nc.tensor), ("act", nc.scalar), ("dve", nc.vector),
                        ("pool", nc.gpsimd), ("sp", nc.sync)):
            sem = self.es.enter_context(nc.semaphore("s_" + name))
            self.E[name] = Eng(name, h, sem)
        for qn in ("sp", "pool", "act"):
            q = self.E[qn]
            for i in range(8):
                q.dsems.append(self.es.enter_context(nc.semaphore("d_%s%d" % (qn, i))))
                q.dcnt.append(0)
        self.dma_events = []

    def _wait(self, e, ev):
        key, sem, val = ev
        if e.waited.get(key, 0) >= val:
            return
        e.h.wait_ge(sem, val)
        e.waited[key] = val

    def _deps(self, e, R, W):
        for b in R:
            if b.w is not None:
                if b.w[0] == e.name and e.name == "pe":
                    continue
                self._wait(e, b.w)
        for b in W:
            if b.w is not None and b.w[0] != e.name:
                self._wait(e, b.w)
            for ev in b.r:
                if ev[0] != e.name:
                    self._wait(e, ev)

    def op(self, en, fn, R=(), W=(), inc=True):
        e = self.E[en]
        if any(b.excl for b in R):
            W = list(W) + [b for b in R if b.excl and b not in W]
            R = [b for b in R if not b.excl]
        self._deps(e, R, W)
        ins = fn()
        val = e.count + 1
        if inc:
            ins.then_inc(e.sem, 1)
            e.count = val
        ev = (en, e.sem, val)
        for b in W:
            b.w = ev
            b.r = []
        for b in R:
            b.r.append(ev)
        return ins

    def dma(self, qn, out, in_, R=(), W=()):
        q = self.E[qn]
        self._deps(q, R, W)
        slot = q.di % len(q.dsems)
        q.di += 1
        sem = q.dsems[slot]
        key = "d_%s%d" % (qn, slot)
        if q.dcnt[slot] > 0:
            self._wait(q, (key, sem, 16 * q.dcnt[slot]))
        q.dcnt[slot] += 1
        q.h.dma_start(out=out, in_=in_).then_inc(sem, 16)
        ev = (key, sem, 16 * q.dcnt[slot])
        for b in W:
            b.w = ev
            b.r = []
        for b in R:
            b.r.append(ev)
        self.dma_events.append(ev)

    def barrier(self):
        evs = [(n, e.sem, e.count) for n, e in self.E.items() if e.count > 0]
        last = {}
        for ev in self.dma_events:
            last[ev[0]] = ev
        evs += list(last.values())
        self.dma_events = list(last.values())
        for n, e in self.E.items():
            for ev in evs:
                if ev[0] != n:
                    self._wait(e, ev)

    def sb(self, es, name, shape, dt):
        self.uid = getattr(self, "uid", 0) + 1
        return es.enter_context(self.nc.sbuf_tensor("sb%d_%s" % (self.uid, name), shape, dt))


class Ring:
    def __init__(self, kb, es, name, n, shape, dt):
        self.t = [kb.sb(es, "%s%d" % (name, i), shape, dt) for i in range(n)]
        self.b = [Buf("%s%d" % (name, i)) for i in range(n)]
        self.i = 0

    def next(self):
        k = self.i % len(self.t)
        self.i += 1
        return self.t[k], self.b[k]


def build(mode='full'):
    kb = KB()
    nc = kb.nc
    es = kb.es
    op = kb.op
    dma = kb.dma
    V = nc.vector
    A = nc.scalar
    P = nc.tensor
    G = nc.gpsimd

    def din(name, shape, dt=F32):
        return nc.dram_tensor(name, list(shape), dt, kind="ExternalInput").ap()

    SHAPES = {
        "x_loc": [TOK, D], "x_oth": [TOK, D], "x_halo": [512, D], "x_ctx": [256, D], "c_row": [1, D], "cc_row": [1, D],
        "ada_w": [2, D, 6 * D], "ada_b": [2, 6 * D], "norm_mix_g": [2, D], "norm_ffn_g": [2, D],
        "router_w": [D, 16], "router_bias": [1, 16],
        "moe_w_gate": [2, 16, D, 512], "moe_w_up": [2, 16, D, 512], "moe_w_down": [2, 16, 512, D],
        "ab_w_in": [D, 3584], "ab_w_out": [D, D], "ret_decay": [1, 16], "head_g": [1, 192],
        "bias_tab": [8, 128, 896], "rowmask": [2, 192], "rope_loc": [TOK, 256], "rope_oth": [TOK + 256, 128],
        "exp_init": [128, 36], "cst": [128, 260], "ident": [128, 128], "a2": [2, 128],
        "sgu_w_in": [D, 6144], "sgu_b_in": [1, 6144], "sgu_norm_g": [1, 3072], "sgu_w_s": [8, 128, 128],
        "sgu_bsT": [128, 8], "sgu_w_out": [3072, D], "dbg_x": [TOK, D],
    }
    declared = {}

    class Lazy:
        def __init__(self, name):
            self.name = name

        def ap(self):
            if self.name not in declared:
                declared[self.name] = nc.dram_tensor(self.name, list(SHAPES[self.name]), F32, kind="ExternalInput").ap()
            return declared[self.name]

        def __getitem__(self, k):
            return self.ap()[k]

        def rearrange(self, *a, **kw):
            return self.ap().rearrange(*a, **kw)

    kb.declared = declared
    x_loc, x_oth, x_halo, x_ctx, c_row, cc_row = (Lazy(n) for n in ("x_loc", "x_oth", "x_halo", "x_ctx", "c_row", "cc_row"))
    ada_w, ada_b, nmix_g, nffn_g = (Lazy(n) for n in ("ada_w", "ada_b", "norm_mix_g", "norm_ffn_g"))
    router_w, router_b = Lazy("router_w"), Lazy("router_bias")
    w_gate, w_up, w_down = Lazy("moe_w_gate"), Lazy("moe_w_up"), Lazy("moe_w_down")
    ab_w_in, ab_w_out, ret_decay, hg = Lazy("ab_w_in"), Lazy("ab_w_out"), Lazy("ret_decay"), Lazy("head_g")
    bias_tab, rowmask, rope_loc, rope_oth = Lazy("bias_tab"), Lazy("rowmask"), Lazy("rope_loc"), Lazy("rope_oth")
    exp_init, cst, ident_d, a2_d = Lazy("exp_init"), Lazy("cst"), Lazy("ident"), Lazy("a2")
    sgu_w_in, sgu_b_in, sgu_ng, sgu_ws = Lazy("sgu_w_in"), Lazy("sgu_b_in"), Lazy("sgu_norm_g"), Lazy("sgu_w_s")
    sgu_bsT, sgu_w_out, dbg_x = Lazy("sgu_bsT"), Lazy("sgu_w_out"), Lazy("dbg_x")
    y_out = nc.dram_tensor("y_out", [TOK, D], F32, kind="ExternalOutput").ap()
    xa = nc.dram_tensor("xa_scr", [TOK, D], F32, kind="Internal").ap()
    xb = nc.dram_tensor("xb_scr", [TOK, D], F32, kind="Internal").ap()
    xc = nc.dram_tensor("xc_scr", [TOK, D], F32, kind="Internal").ap()

    ps = es.enter_context(nc.psum_tensor("ps", [128, 4096], F32))
    ps_bf = ps.bitcast(BF16)
    psb = [Buf("ps%d" % i, excl=True) for i in range(8)]

    class PsRing:
        def __init__(self, banks):
            self.banks = banks
            self.i = 0

        def next(self):
            k = self.banks[self.i % len(self.banks)]
            self.i += 1
            return k

    def bank(k, n=512, off=0):
        return ps[:, k * 512 + off:k * 512 + off + n]

    def bank_bf(k, n=1024, off=0):
        return ps_bf[:, k * 1024 + off:k * 1024 + off + n]

    g = ExitStack()
    es.enter_context(g)
    ident = kb.sb(g, "ident", [128, 128], BF16)
    identf = kb.sb(g, "identf", [128, 128], F32)
    ones1 = kb.sb(g, "ones1", [1, 128], BF16)
    silc = kb.sb(g, "silc", [128, 8, 128], BF16)
    silcc = kb.sb(g, "silcc", [128, 8, 128], BF16)
    b_const = Buf("const")
    dma("pool", ident[:], ident_d[:, :], W=[b_const])
    dma("sp", identf[:], ident_d[:, :], W=[b_const])
    op("dve", lambda: V.memset(ones1[:], 1.0), W=[b_const])

    with ExitStack() as t:
        crow = kb.sb(t, "crow", [128, 2, D], F32)
        csil = kb.sb(t, "csil", [128, 2, D], BF16)
        bt = Buf("crow")
        dma("sp", crow[:, 0, :], c_row[0:1, :].partition_broadcast(128), W=[bt])
        dma("sp", crow[:, 1, :], cc_row[0:1, :].partition_broadcast(128), W=[bt])
        op("act", lambda: A.activation(out=csil[:], in_=crow[:], func=AF.Silu), R=[bt], W=[bt])
        for j, dst in enumerate((silc, silcc)):
            for dk in range(8):
                op("pe", lambda: P.transpose(bank_bf(0, 128, dk * 128), csil[:, j, dk * 128:(dk + 1) * 128], ident[:]),
                   R=[bt, b_const], W=[psb[0]], inc=(dk == 7))
            op("dve", lambda: V.tensor_copy(dst[:].rearrange("p a b -> p (a b)"), bank_bf(0)), R=[psb[0]], W=[b_const])
        kb.barrier()

    def ada_chunk(l, j, lhs, out_ap, out_buf, wring, bring, post=None):
        wt, wb = wring.next()
        dma("pool", wt[:], ada_w[l, :, j * 1024:(j + 1) * 1024].rearrange("(k p) n -> p k n", p=128), W=[wb])
        bt_, bb = bring.next()
        dma("pool", bt_[:], ada_b[l:l + 1, j * 1024:(j + 1) * 1024], W=[bb])
        for hf in range(2):
            k = 6 + hf
            for dk in range(8):
                op("pe", lambda: P.matmul(bank(k), lhs[:, dk, :], wt[:, dk, hf * 512:(hf + 1) * 512],
                                          start=(dk == 0), stop=False), R=[wb, b_const], W=[psb[k]], inc=False)
            op("pe", lambda: P.matmul(bank(k), ones1[:], bt_[:, hf * 512:(hf + 1) * 512], start=False, stop=True),
               R=[bb, b_const], W=[psb[k]])
        if post is None:
            op("act", lambda: A.copy(out=out_ap, in_=ps[:, 6 * 512:8 * 512]), R=[psb[6], psb[7]], W=[out_buf])
        else:
            post(ps[:, 6 * 512:8 * 512], [psb[6], psb[7]])

    def front_a(xt, xbuf, G1, SH, gbuf, fr, want_f32T=False):
        junk, jb = fr["junk"].next()
        st, sbf = fr["st"].next()
        op("act", lambda: A.activation(out=junk[:], in_=xt, func=AF.Square, accum_out=st[:, 0:1]),
           R=[xbuf], W=[jb, sbf])
        op("act", lambda: A.activation(out=st[:, 1:2], in_=st[:, 0:1], func=AF.Sqrt, scale=1.0 / D, bias=fr["eps"][:, 0:1]),
           R=[sbf, b_const], W=[sbf])
        op("dve", lambda: V.reciprocal(st[:, 2:3], st[:, 1:2]), R=[sbf], W=[sbf])
        tmp, tb = fr["tmp"].next()
        op("dve", lambda: V.scalar_tensor_tensor(out=tmp[:], in0=xt, scalar=st[:, 2:3], in1=G1, op0=ALU.mult, op1=ALU.mult),
           R=[xbuf, sbf, gbuf], W=[tb])
        if want_f32T:
            h, hb = fr["h32"].next()
        else:
            h, hb = fr["h"].next()
        op("pool", lambda: G.tensor_tensor(out=h[:], in0=tmp[:], in1=SH, op=ALU.add), R=[tb, gbuf], W=[hb])
        return h, hb, want_f32T

    def front_b(fa, fr, bf_dst=None):
        h, hb, want_f32T = fa
        if not want_f32T:
            hT, hTb = fr["hT"].next()
            k = fr["ps"].next()
            for dk in range(8):
                op("pe", lambda: P.transpose(bank_bf(k, 128, dk * 128), h[:, dk * 128:(dk + 1) * 128], ident[:]),
                   R=[hb, b_const], W=[psb[k]], inc=(dk == 7))
            op("act", lambda: A.copy(out=hT[:].rearrange("p a b -> p (a b)"), in_=bank_bf(k)), R=[psb[k]], W=[hTb])
            return hT, hTb, None, None
        else:
            hT32, hT32b = fr["hT32"].next()
            dst, dstb = bf_dst
            for half in range(2):
                k = fr["ps"].next()
                for q in range(4):
                    dk = half * 4 + q
                    op("pe", lambda: P.transpose(bank(k, 128, q * 128), h[:, dk * 128:(dk + 1) * 128], identf[:]),
                       R=[hb, b_const], W=[psb[k]], inc=(q == 3))
                op("act", lambda: A.copy(out=dst[:, half * 4:(half + 1) * 4, :], in_=bank(k).rearrange("p (a b) -> p a b", b=128)),
                   R=[psb[k]], W=[dstb])
                op("dve", lambda: V.tensor_copy(hT32[:, half * 4:(half + 1) * 4, :].rearrange("p a b -> p (a b)"), bank(k)),
                   R=[psb[k]], W=[hT32b])
            return None, None, hT32, hT32b

    def front(xt, xbuf, G1, SH, gbuf, fr, want_f32T=False):
        return front_b(front_a(xt, xbuf, G1, SH, gbuf, fr, want_f32T), fr)

    def pipelined(n, head, body):
        nxt = head(0)
        for i in range(n):
            cur = nxt
            nxt = head(i + 1) if i + 1 < n else None
            body(i, cur)

    def make_front(t, ps_banks, f32T=False):
        fr = {}
        fr["junk"] = Ring(kb, t, "fjunk", 1, [128, D], BF16)
        fr["st"] = Ring(kb, t, "fst", 3, [128, 4], F32)
        fr["tmp"] = Ring(kb, t, "ftmp", 1, [128, D], F32)
        if f32T:
            fr["h32"] = Ring(kb, t, "fh32", 2, [128, D], F32)
            fr["hT32"] = Ring(kb, t, "fhT32", 2, [128, 8, 128], F32)
        else:
            fr["h"] = Ring(kb, t, "fh", 2, [128, D], BF16)
        if not f32T:
            fr["hT"] = Ring(kb, t, "fhT", 2, [128, 8, 128], BF16)
        fr["ps"] = PsRing(ps_banks)
        eps = kb.sb(t, "feps", [128, 1], F32)
        op("dve", lambda: V.memset(eps[:], EPS), W=[b_const])
        fr["eps"] = eps
        return fr

    def make_mod(t, l, jshift, jscale, gsrc, lhs, name, wring, bring):
        G1 = kb.sb(t, name + "G1", [128, D], F32)
        SH = kb.sb(t, name + "SH", [128, D], F32)
        gb = kb.sb(t, name + "gb", [128, D], F32)
        mb = Buf(name)
        dma("sp", gb[:], gsrc.partition_broadcast(128), W=[mb])
        ada_chunk(l, jshift, lhs, SH[:], mb, wring, bring)

        def post(psap, pbufs):
            op("dve", lambda: V.scalar_tensor_tensor(out=G1[:], in0=psap, scalar=1.0, in1=gb[:], op0=ALU.add, op1=ALU.mult),
               R=pbufs + [mb], W=[mb])
        ada_chunk(l, jscale, lhs, None, mb, wring, bring, post=post)
        return G1, SH, mb

    def moe_phase(l, x_src, x_dst):
        with ExitStack() as t:
            xres = kb.sb(t, "xres", [128, NT, D], F32)
            xrb = [Buf("xres%d" % i) for i in range(NT)]
            hfT = kb.sb(t, "hfT", [128, 8, TOK], BF16)
            hfTb = [Buf("hfT%d" % i) for i in range(NT)]
            comb = kb.sb(t, "comb", [128, NT, 16], F32)
            combb = [Buf("comb%d" % i) for i in range(NT)]
            rw = kb.sb(t, "rw", [128, 8, 16], F32)
            rbias = kb.sb(t, "rbias", [128, 16], F32)
            gfb = kb.sb(t, "gfb", [128, D], F32)
            gfbuf = Buf("gfb")
            cb = Buf("rconst")
            dma("sp", rw[:], router_w.rearrange("(k p) e -> p k e", p=128), W=[cb])
            dma("sp", rbias[:], router_b[0:1, :].partition_broadcast(128), W=[cb])
            for i in range(NT):
                dma("sp", xres[:, i, :], x_src[i * 128:(i + 1) * 128, :], W=[xrb[i]])
            with ExitStack() as t2:
                wring = Ring(kb, t2, "adaw", 2, [128, 8, 1024], BF16)
                bring = Ring(kb, t2, "adab", 2, [1, 1024], BF16)
                G1, SH, mb = make_mod(t2, l, 3, 4, nffn_g[l:l + 1, :], silc, "mf", wring, bring)
                ada_chunk(l, 5, silc, gfb[:], gfbuf, wring, bring)
                fr = make_front(t2, [0, 1], f32T=True)
                RB = kb.sb(t2, "RB", [128, 4, NT, 16], F32)
                RS = kb.sb(t2, "RS", [128, 6, NT, 4], F32)
                rb = Buf("RB")
                fas = {}
                t32 = {}
                for j in range(NT + 2):
                    if j < NT:
                        fas[j] = front_a(xres[:, j, :], xrb[j], G1[:], SH[:], mb, fr, want_f32T=True)
                    i = j - 1
                    if 0 <= i < NT:
                        _, _, hT32, hT32b = front_b(fas.pop(i), fr, bf_dst=(hfT[:, :, i * 128:(i + 1) * 128], hfTb[i]))
                        t32[i] = (hT32, hT32b)
                    i = j - 2
                    if 0 <= i < NT:
                        hT32, hT32b = t32.pop(i)
                        k = 2 + (i % 2)
                        for dk in range(8):
                            op("pe", lambda: P.matmul(bank(k, 16), hT32[:, dk, :], rw[:, dk, :], start=(dk == 0), stop=(dk == 7)),
                               R=[hT32b, cb], W=[psb[k]], inc=(dk == 7))
                        op("act", lambda: A.activation(out=RB[:, 0, i, :], in_=bank(k, 16), func=AF.Sigmoid), R=[psb[k]], W=[rb])
                aff = RB[:, 0]
                sel = RB[:, 1]
                tmp = RB[:, 2]
                msk = RB[:, 3]
                m1 = RS[:, 0]
                m2 = RS[:, 1]
                gs = RS[:, 2]
                goh = RS[:, 3]
                gm = RS[:, 4, :, 0]
                den = RS[:, 5, :, 0]
                rden = RS[:, 5, :, 1]
                g4 = lambda ap: ap.rearrange("p t (g e) -> p (t g) e", e=4)
                f4 = lambda ap: ap.rearrange("p t g -> p (t g)")
                b4 = lambda ap: f4(ap).unsqueeze(2).to_broadcast([128, NT * 4, 4])
                op("dve", lambda: V.tensor_tensor(out=sel, in0=aff, in1=rbias[:].unsqueeze(1).to_broadcast([128, NT, 16]), op=ALU.add),
                   R=[rb, cb], W=[rb])
                op("dve", lambda: V.tensor_reduce(out=f4(m1), in_=g4(sel), op=ALU.max, axis=AX.X), R=[rb], W=[rb])
                op("dve", lambda: V.tensor_tensor(out=g4(tmp), in0=g4(sel), in1=b4(m1), op=ALU.is_ge), R=[rb], W=[rb])
                op("dve", lambda: V.scalar_tensor_tensor(out=tmp, in0=tmp, scalar=-1.0e4, in1=sel, op0=ALU.mult, op1=ALU.add),
                   R=[rb], W=[rb])
                op("dve", lambda: V.tensor_reduce(out=f4(m2), in_=g4(tmp), op=ALU.max, axis=AX.X), R=[rb], W=[rb])
                op("dve", lambda: V.tensor_tensor(out=gs, in0=m1, in1=m2, op=ALU.add), R=[rb], W=[rb])
                op("dve", lambda: V.tensor_reduce(out=gm, in_=gs, op=ALU.max, axis=AX.X), R=[rb], W=[rb])
                op("dve", lambda: V.tensor_tensor(out=goh, in0=gs, in1=gm.unsqueeze(2).to_broadcast([128, NT, 4]), op=ALU.is_ge),
                   R=[rb], W=[rb])
                op("dve", lambda: V.tensor_tensor(out=g4(msk), in0=g4(sel), in1=b4(m2), op=ALU.is_ge), R=[rb], W=[rb])
                op("dve", lambda: V.tensor_tensor(out=g4(msk), in0=g4(msk), in1=b4(goh), op=ALU.mult), R=[rb], W=[rb])
                op("dve", lambda: V.tensor_tensor(out=msk, in0=msk, in1=aff, op=ALU.mult), R=[rb], W=[rb])
                op("dve", lambda: V.tensor_reduce(out=den, in_=msk, op=ALU.add, axis=AX.X), R=[rb], W=[rb])
                op("dve", lambda: V.reciprocal(rden, den), R=[rb], W=[rb])
                op("dve", lambda: V.tensor_tensor(out=comb[:], in0=msk, in1=rden.unsqueeze(2).to_broadcast([128, NT, 16]), op=ALU.mult),
                   R=[rb], W=combb)
                kb.barrier()
            with ExitStack() as t3:
                wg = Ring(kb, t3, "wg", 2, [128, 8, 512], BF16)
                wu = Ring(kb, t3, "wu", 2, [128, 8, 512], BF16)
                wd = Ring(kb, t3, "wd", 2, [128, 4, D], BF16)
                wd2 = Ring(kb, t3, "wd2", 2, [128, 4, D], BF16)
                sg = Ring(kb, t3, "sg", 2, [128, 512], BF16)
                at = Ring(kb, t3, "at", 2, [128, 4, 512], BF16)
                gu = PsRing([0, 1, 2, 3])
                yr = PsRing([4, 5, 6, 7])
                for e in range(16):
                    wgt, wgb = wg.next()
                    wut, wub = wu.next()
                    wdt, wdb = wd.next()
                    wd2t, wd2b = wd2.next()
                    dma("pool", wgt[:], w_gate[l, e].rearrange("(k p) n -> p k n", p=128), W=[wgb])
                    dma("pool", wut[:], w_up[l, e].rearrange("(k p) n -> p k n", p=128), W=[wub])
                    dma("pool", wdt[:], w_down[l, e].rearrange("(k p) n -> p k n", p=128), W=[wdb])
                    for fc in range(4):
                        op("pool", lambda: G.tensor_tensor(out=wd2t[:, fc, :], in0=wdt[:, fc, :], in1=gfb[:], op=ALU.mult),
                           R=[wdb, gfbuf], W=[wd2b])
                    for tg in range(4):
                        att, atb = at.next()
                        toks = slice(tg * 512, (tg + 1) * 512)
                        hb4 = hfTb[tg * 4:(tg + 1) * 4]
                        for fc in range(4):
                            kg = gu.next()
                            ku = gu.next()
                            for dk in range(8):
                                op("pe", lambda: P.matmul(bank(kg), wgt[:, dk, fc * 128:(fc + 1) * 128], hfT[:, dk, toks],
                                                          start=(dk == 0), stop=(dk == 7)), R=[wgb] + hb4, W=[psb[kg]], inc=(dk == 7))
                            for dk in range(8):
                                op("pe", lambda: P.matmul(bank(ku), wut[:, dk, fc * 128:(fc + 1) * 128], hfT[:, dk, toks],
                                                          start=(dk == 0), stop=(dk == 7)), R=[wub] + hb4, W=[psb[ku]], inc=(dk == 7))
                            sgt, sgb = sg.next()
                            op("act", lambda: A.activation(out=sgt[:], in_=bank(kg), func=AF.Silu), R=[psb[kg]], W=[sgb])
                            op("dve", lambda: V.tensor_tensor(out=att[:, fc, :], in0=bank(ku), in1=sgt[:], op=ALU.mult),
                               R=[psb[ku], sgb], W=[atb])
                        for ti in range(4):
                            i = tg * 4 + ti
                            for hf in range(2):
                                ky = yr.next()
                                for fc in range(4):
                                    op("pe", lambda: P.matmul(bank(ky), att[:, fc, ti * 128:(ti + 1) * 128],
                                                              wd2t[:, fc, hf * 512:(hf + 1) * 512], start=(fc == 0), stop=(fc == 3)),
                                       R=[atb, wd2b], W=[psb[ky]], inc=(fc == 3))
                                xs = xres[:, i, hf * 512:(hf + 1) * 512]
                                op("dve", lambda: V.scalar_tensor_tensor(out=xs, in0=bank(ky), scalar=comb[:, i, e:e + 1], in1=xs,
                                                                         op0=ALU.mult, op1=ALU.add),
                                   R=[psb[ky], combb[i], xrb[i]], W=[xrb[i]])
                for i in range(NT):
                    dma("sp", x_dst[i * 128:(i + 1) * 128, :], xres[:, i, :], R=[xrb[i]])
                kb.barrier()

    def sgu_phase(x_src, x_dst):
        l = 1
        with ExitStack() as t:
            wout = kb.sb(t, "swout", [128, 24, D], BF16)
            wsT = kb.sb(t, "swsT", [128, 8, 128], BF16)
            bsT = kb.sb(t, "sbsT", [128, 8], F32)
            ngb = kb.sb(t, "sngb", [128, 3072], BF16)
            gmb = kb.sb(t, "sgmb", [128, D], F32)
            gmbuf = Buf("gmb")
            cb = Buf("sconst")
            for q in range(4):
                dma("pool", wout[:, q * 6:(q + 1) * 6, :],
                    sgu_w_out[q * 768:(q + 1) * 768, :].rearrange("(k p) n -> p k n", p=128), W=[cb])
            dma("sp", bsT[:], sgu_bsT[:, :], W=[cb])
            dma("pool", ngb[:], sgu_ng[0:1, :].partition_broadcast(128), W=[cb])
            G1 = kb.sb(t, "smG1", [128, D], F32)
            SH = kb.sb(t, "smSH", [128, D], F32)
            with ExitStack() as t0:
                wring = Ring(kb, t0, "adaw", 2, [128, 8, 1024], BF16)
                bring = Ring(kb, t0, "adab", 2, [1, 1024], BF16)
                G1_, SH_, mb = make_mod(t0, l, 0, 1, nmix_g[l:l + 1, :], silc, "sm", wring, bring)
                op("pool", lambda: G.tensor_copy(G1[:], G1_[:]), R=[mb], W=[cb])
                op("pool", lambda: G.tensor_copy(SH[:], SH_[:]), R=[mb], W=[cb])
                ada_chunk(l, 2, silc, gmb[:], gmbuf, wring, bring)
                wsf = kb.sb(t0, "wsf", [128, 8, 128], F32)
                wsb = Buf("wsf")
                dma("sp", wsf[:], sgu_ws.rearrange("g i j -> i g j"), W=[wsb])
                for gi in range(8):
                    op("pe", lambda: P.transpose(bank(gi // 4, 128, (gi % 4) * 128), wsf[:, gi, :], identf[:]),
                       R=[wsb, b_const], W=[psb[gi // 4]])
                op("dve", lambda: V.tensor_copy(wsT[:].rearrange("p a b -> p (a b)"), ps[:, 0:1024]), R=[psb[0], psb[1]], W=[cb])
                kb.barrier()
            fr = make_front(t, [0])
            hTblk = kb.sb(t, "shTblk", [128, 8, 512], BF16)
            hTblkb = Buf("hTblk")
            uu = kb.sb(t, "suu", [128, 4, 3072], BF16)
            vv = kb.sb(t, "svv", [128, 4, 3072], BF16)
            uvb = [Buf("uv%d" % i) for i in range(4)]
            wblk = Ring(kb, t, "swblk", 2, [128, 8, 512], BF16)
            bblk = Ring(kb, t, "sbblk", 2, [1, 512], BF16)
            vn = Ring(kb, t, "svn", 2, [128, 3072], BF16)
            gt = Ring(kb, t, "sgt", 1, [128, 3072], BF16)
            gT = Ring(kb, t, "sgT", 1, [128, 24, 128], BF16)
            st = Ring(kb, t, "sst", 2, [128, 4], F32)
            yt = Ring(kb, t, "syt", 1, [128, 512], F32)
            xo = Ring(kb, t, "sxo", 1, [128, D], F32)
            eps = fr["eps"]
            zr = PsRing([1, 2, 3, 4])
            xin = Ring(kb, t, "sxin", 2, [128, D], F32)
            xres_r = Ring(kb, t, "sxres", 2, [128, D], F32)

            def ldA(tb, ti):
                i = tb * 4 + ti
                xt, xbuf = xin.next()
                dma("sp", xt[:], x_src[i * 128:(i + 1) * 128, :], W=[xbuf])
                return xt, xbuf

            def stAa(xa_):
                xt, xbuf = xa_
                return front_a(xt[:], xbuf, G1[:], SH[:], cb, fr)

            def stAb(ti, fa):
                hT, hTb, _, _ = front_b(fa, fr)
                op("pool", lambda: G.tensor_copy(hTblk[:, :, ti * 128:(ti + 1) * 128], hT[:]), R=[hTb], W=[hTblkb])

            def stB(tb):
                for cbk in range(12):
                    wt, wb = wblk.next()
                    bt_, bb = bblk.next()
                    dma("pool", wt[:], sgu_w_in[:, cbk * 512:(cbk + 1) * 512].rearrange("(k p) n -> p k n", p=128), W=[wb])
                    dma("pool", bt_[:], sgu_b_in[0:1, cbk * 512:(cbk + 1) * 512], W=[bb])
                    for ti in range(4):
                        k = zr.next()
                        for dk in range(8):
                            op("pe", lambda: P.matmul(bank(k), hTblk[:, dk, ti * 128:(ti + 1) * 128], wt[:, dk, :],
                                                      start=(dk == 0), stop=False), R=[hTblkb, wb], W=[psb[k]], inc=False)
                        op("pe", lambda: P.matmul(bank(k), ones1[:], bt_[:], start=False, stop=True), R=[bb, b_const], W=[psb[k]])
                        dst = uu[:, ti, cbk * 512:(cbk + 1) * 512] if cbk < 6 else vv[:, ti, (cbk - 6) * 512:(cbk - 5) * 512]
                        op("act", lambda: A.activation(out=dst, in_=bank(k), func=AF.Gelu), R=[psb[k]], W=[uvb[ti]])

            def ldR(tb, ti):
                i = tb * 4 + ti
                xr, xrb_ = xres_r.next()
                dma("sp", xr[:], x_src[i * 128:(i + 1) * 128, :], W=[xrb_])
                return xr, xrb_

            def stN(ti):
                s_, sbf = st.next()
                vnt, vnb = vn.next()
                op("act", lambda: A.activation(out=vnt[:], in_=vv[:, ti, :], func=AF.Square, accum_out=s_[:, 0:1]),
                   R=[uvb[ti]], W=[vnb, sbf])
                op("act", lambda: A.activation(out=s_[:, 1:2], in_=s_[:, 0:1], func=AF.Sqrt, scale=1.0 / 3072, bias=eps[:, 0:1]),
                   R=[sbf, b_const], W=[sbf])
                op("dve", lambda: V.reciprocal(s_[:, 2:3], s_[:, 1:2]), R=[sbf], W=[sbf])
                op("dve", lambda: V.scalar_tensor_tensor(out=vnt[:], in0=vv[:, ti, :], scalar=s_[:, 2:3], in1=ngb[:],
                                                         op0=ALU.mult, op1=ALU.mult), R=[uvb[ti], sbf, cb], W=[vnb])
                return vnt, vnb

            def stC12(ti, vn_):
                vnt, vnb = vn_
                gtt, gtb = gt.next()
                for gi in range(8):
                    k = zr.next()
                    op("pe", lambda: P.matmul(bank(k, 384), wsT[:, gi, :], vnt[:, gi * 384:(gi + 1) * 384], start=True, stop=True),
                       R=[vnb, cb], W=[psb[k]])
                    op("dve", lambda: V.scalar_tensor_tensor(out=gtt[:, gi * 384:(gi + 1) * 384], in0=bank(k, 384),
                                                             scalar=bsT[:, gi:gi + 1], in1=uu[:, ti, gi * 384:(gi + 1) * 384],
                                                             op0=ALU.add, op1=ALU.mult), R=[psb[k], cb, uvb[ti]], W=[gtb])
                gTt, gTb = gT.next()
                for q in range(3):
                    k = zr.next()
                    for c8 in range(8):
                        kc = q * 8 + c8
                        op("pe", lambda: P.transpose(bank_bf(k, 128, c8 * 128), gtt[:, kc * 128:(kc + 1) * 128], ident[:]),
                           R=[gtb, b_const], W=[psb[k]], inc=(c8 == 7))
                    op("act", lambda: A.copy(out=gTt[:, q * 8:(q + 1) * 8, :].rearrange("p a b -> p (a b)"), in_=bank_bf(k)),
                       R=[psb[k]], W=[gTb])
                return gTt, gTb

            def stC3(tb, ti, g_, xr_):
                i = tb * 4 + ti
                gTt, gTb = g_
                xr, xrb_ = xr_
                xot, xob = xo.next()
                for hf in range(2):
                    k = 5 + hf
                    for kc in range(24):
                        op("pe", lambda: P.matmul(bank(k), gTt[:, kc, :], wout[:, kc, hf * 512:(hf + 1) * 512],
                                                  start=(kc == 0), stop=(kc == 23)), R=[gTb, cb], W=[psb[k]], inc=(kc == 23))
                    ytt, ytb = yt.next()
                    op("dve", lambda: V.tensor_tensor(out=ytt[:], in0=bank(k), in1=gmb[:, hf * 512:(hf + 1) * 512], op=ALU.mult),
                       R=[psb[k], gmbuf], W=[ytb])
                    op("pool", lambda: G.tensor_tensor(out=xot[:, hf * 512:(hf + 1) * 512], in0=ytt[:],
                                                       in1=xr[:, hf * 512:(hf + 1) * 512], op=ALU.add),
                       R=[ytb, xrb_], W=[xob])
                dma("sp", x_dst[i * 128:(i + 1) * 128, :], xot[:], R=[xob])

            for ti in range(4):
                stAb(ti, stAa(ldA(0, ti)))
            for tb in range(4):
                stB(tb)
                more = tb + 1 < 4
                xr_ = ldR(tb, 0)
                vn_ = stN(0)
                if more:
                    stAb(0, stAa(ldA(tb + 1, 0)))
                for ti in range(4):
                    nxt = ti + 1 < 4
                    if nxt:
                        xr_n = ldR(tb, ti + 1)
                        if more:
                            xa_n = ldA(tb + 1, ti + 1)
                    g_ = stC12(ti, vn_)
                    if nxt:
                        vn_ = stN(ti + 1)
                        if more:
                            fa_n = stAa(xa_n)
                    stC3(tb, ti, g_, xr_)
                    if nxt:
                        if more:
                            stAb(ti + 1, fa_n)
                        xr_ = xr_n
            kb.barrier()


    def mixer0_phase(x_dst):
        l = 0
        import os
        MS = os.environ.get("MIX_STOP", "")
        b3 = lambda ap, n: ap.unsqueeze(2).to_broadcast([128, n, 64])
        h3 = lambda ap: ap.rearrange("p (h e) -> p h e", e=64)
        with ExitStack() as t:
            G1m = kb.sb(t, "G1m", [128, D], F32)
            SHm = kb.sb(t, "SHm", [128, D], F32)
            gmb = kb.sb(t, "gmb", [128, D], F32)
            mbuf = Buf("modm")
            lgb = kb.sb(t, "lgb", [128, 16], F32)
            hgb = kb.sb(t, "hgb", [128, 192], F32)
            eps = kb.sb(t, "eps0", [128, 1], F32)
            one = kb.sb(t, "one0", [128, 1], F32)
            ret_out = kb.sb(t, "ret_out", [128, NT, 512], BF16)
            retb = [Buf("ret%d" % i) for i in range(NT)]
            KcT = kb.sb(t, "KcT", [128, 4, 256], BF16)
            Vc = kb.sb(t, "Vc", [128, 2, 512], BF16)
            ctxb = Buf("ctxkv")
            tb_ = Buf("tables")
            retg = hgb[:, 0:64]
            qg = hgb[:, 64:128]
            kg = hgb[:, 128:192]
            op("dve", lambda: V.memset(eps[:], EPS), W=[tb_])
            op("dve", lambda: V.memset(one[:], 1.0), W=[tb_])
            dma("sp", hgb[:], hg[0:1, :].partition_broadcast(128), W=[tb_])
            dma("sp", lgb[:], ret_decay[0:1, :].partition_broadcast(128), W=[tb_])
            op("dve", lambda: V.tensor_scalar(out=qg, in0=qg, scalar1=0.125, scalar2=None, op0=ALU.mult), R=[tb_], W=[tb_])
            op("act", lambda: A.activation(out=lgb[:], in_=lgb[:], func=AF.Exp), R=[tb_], W=[tb_])
            op("act", lambda: A.activation(out=lgb[:], in_=lgb[:], func=AF.Ln, bias=one[:, 0:1]), R=[tb_], W=[tb_])
            op("dve", lambda: V.tensor_scalar(out=lgb[:], in0=lgb[:], scalar1=-1.0, scalar2=None, op0=ALU.mult), R=[tb_], W=[tb_])
            with ExitStack() as t0:
                wring = Ring(kb, t0, "adaw", 2, [128, 8, 1024], BF16)
                bring = Ring(kb, t0, "adab", 2, [1, 1024], BF16)
                G1_, SH_, mb_ = make_mod(t0, l, 0, 1, nmix_g[l:l + 1, :], silc, "mm", wring, bring)
                op("pool", lambda: G.tensor_copy(G1m[:], G1_[:]), R=[mb_], W=[mbuf])
                op("pool", lambda: G.tensor_copy(SHm[:], SH_[:]), R=[mb_], W=[mbuf])
                ada_chunk(l, 2, silc, gmb[:], mbuf, wring, bring)
                kb.barrier()

            def norm_heads_T(src_bank, gain, tmps, dstK, dstKb):
                sqt, sqb = tmps["sq"].next()
                s8, s8b = tmps["s8"].next()
                op("act", lambda: A.activation(out=sqt[:], in_=bank(src_bank), func=AF.Square), R=[psb[src_bank]], W=[sqb])
                op("dve", lambda: V.tensor_reduce(out=s8[:, 0:8], in_=h3(sqt[:]), op=ALU.add, axis=AX.X), R=[sqb], W=[s8b])
                op("act", lambda: A.activation(out=s8[:, 8:16], in_=s8[:, 0:8], func=AF.Sqrt, scale=1.0 / 64, bias=eps[:, 0:1]),
                   R=[s8b, tb_], W=[s8b])
                op("dve", lambda: V.reciprocal(s8[:, 16:24], s8[:, 8:16]), R=[s8b], W=[s8b])
                op("dve", lambda: V.tensor_tensor(out=h3(sqt[:]), in0=h3(bank(src_bank)), in1=b3(s8[:, 16:24], 8), op=ALU.mult),
                   R=[psb[src_bank], s8b], W=[sqb])
                knb, knbb = tmps["knb"].next()
                op("pool", lambda: G.tensor_tensor(out=h3(knb[:]), in0=h3(sqt[:]), in1=gain.unsqueeze(1).to_broadcast([128, 8, 64]),
                                                   op=ALU.mult), R=[sqb, tb_], W=[knbb])
                k = tmps["ps"].next()
                for hp in range(4):
                    op("pe", lambda: P.transpose(bank_bf(k, 128, hp * 128), knb[:, hp * 128:(hp + 1) * 128], ident[:]),
                       R=[knbb, b_const], W=[psb[k]], inc=(hp == 3))
                op("dve", lambda: V.tensor_copy(dstK, bank_bf(k, 512).rearrange("p (a b) -> p a b", b=128)), R=[psb[k]], W=[dstKb])

            def make_tmps(tt, ps_banks):
                return {"sq": Ring(kb, tt, "nsq", 2, [128, 512], F32), "s8": Ring(kb, tt, "ns8", 2, [128, 24], F32),
                        "knb": Ring(kb, tt, "nknb", 2, [128, 512], BF16), "ps": PsRing(ps_banks)}

            def proj(hT, hTb, w, wb, c0, k):
                for dk in range(8):
                    op("pe", lambda: P.matmul(bank(k), hT[:, dk, :], w[:, dk, c0:c0 + 512], start=(dk == 0), stop=(dk == 7)),
                       R=[hTb, wb], W=[psb[k]], inc=(dk == 7))

            def rope(src, srcbufs, Ct, St, tabb, t1, t1b, t2, t2b):
                s5 = lambda ap: ap.rearrange("p (h b f q) -> p h b f q", h=8, b=2, f=2, q=16)
                S4 = St.rearrange("p (b f q) -> p b f q", b=2, f=2, q=16)
                op("dve", lambda: V.tensor_tensor(out=h3(t1), in0=h3(src), in1=Ct.unsqueeze(1).to_broadcast([128, 8, 64]), op=ALU.mult),
                   R=srcbufs + [tabb], W=[t1b])
                for f in range(2):
                    op("dve", lambda: V.tensor_tensor(out=s5(t2)[:, :, :, f, :], in0=s5(src)[:, :, :, 1 - f, :],
                                                      in1=S4[:, :, f, :].unsqueeze(1).to_broadcast([128, 8, 2, 16]), op=ALU.mult),
                       R=srcbufs + [tabb], W=[t2b])

            if MS == "tables":
                return
            with ExitStack() as tR:
                cstt = kb.sb(tR, "cstt", [128, 260], F32)
                MT = kb.sb(tR, "MT", [128, 8, 128], F32)
                dec = kb.sb(tR, "dec", [128, 4, 8], F32)
                CD = kb.sb(tR, "CD", [128, 2, 4], F32)
                ei = kb.sb(tR, "ei", [128, 36], F32)
                Sinit = kb.sb(tR, "Sinit", [128, 2, 256], F32)
                sib = Buf("Sinit")
                G1c = kb.sb(tR, "G1c", [128, D], F32)
                SHc = kb.sb(tR, "SHc", [128, D], F32)
                cbuf = Buf("modc")
                wq = kb.sb(tR, "w_qkvg", [128, 8, 2048], BF16)
                wqb = Buf("wq")
                wn = kb.sb(tR, "w_nkv", [128, 8, 1024], BF16)
                wnb = Buf("wn")
                DBt = kb.sb(tR, "DBt", [128, NT, 256], BF16)
                SBst = kb.sb(tR, "SBst", [128, NT, 256], BF16)
                dbb = [Buf("db%d" % i) for i in range(NT)]
                sbb = [Buf("sb%d" % i) for i in range(NT)]
                for q in range(4):
                    dma("pool", wq[:, :, q * 512:(q + 1) * 512], ab_w_in[:, q * 512:(q + 1) * 512].rearrange("(k p) n -> p k n", p=128), W=[wqb])
                for q in range(2):
                    dma("pool", wn[:, :, q * 512:(q + 1) * 512],
                        ab_w_in[:, 2560 + q * 512:2560 + (q + 1) * 512].rearrange("(k p) n -> p k n", p=128), W=[wnb])
                dma("sp", cstt[:], cst[:, :], W=[tb_])
                dma("sp", ei[:], exp_init[:, :], W=[tb_])
                DFt = cstt[:, 0:128]
                DBe = cstt[:, 128:256]
                with ExitStack() as t0:
                    wring = Ring(kb, t0, "adaw", 2, [128, 8, 1024], BF16)
                    bring = Ring(kb, t0, "adab", 2, [1, 1024], BF16)
                    G1_, SH_, mb_ = make_mod(t0, l, 0, 1, nmix_g[l:l + 1, :], silcc, "mc", wring, bring)
                    op("pool", lambda: G.tensor_copy(G1c[:], G1_[:]), R=[mb_], W=[cbuf])
                    op("pool", lambda: G.tensor_copy(SHc[:], SH_[:]), R=[mb_], W=[cbuf])
                    tmpM = kb.sb(t0, "tmpM", [128, 128], F32)
                    tmb = Buf("tmpM")
                    for h in range(8):
                        op("act", lambda: A.activation(out=MT[:, h, :], in_=DFt, func=AF.Exp, scale=lgb[:, h:h + 1]), R=[tb_], W=[tb_])
                        op("act", lambda: A.activation(out=tmpM[:], in_=DBe, func=AF.Exp, scale=lgb[:, 8 + h:9 + h]), R=[tb_], W=[tmb])
                        op("dve", lambda: V.tensor_tensor(out=MT[:, h, :], in0=MT[:, h, :], in1=tmpM[:], op=ALU.add), R=[tb_, tmb], W=[tb_])
                    for j, (c0, pc) in enumerate(((0, 256), (8, 257), (0, 258), (8, 259))):
                        op("act", lambda: A.activation(out=dec[:, j, :], in_=lgb[:, c0:c0 + 8], func=AF.Exp, scale=cstt[:, pc:pc + 1]),
                           R=[tb_], W=[tb_])
                    lg4 = lgb[:].rearrange("p (d q h) -> p d q h", d=2, q=4, h=2)
                    for hh in range(2):
                        ps_ = slice(hh * 64, (hh + 1) * 64)
                        op("act", lambda: A.activation(out=CD[ps_, :, :], in_=lg4[ps_, :, :, hh], func=AF.Exp, scale=128.0), R=[tb_], W=[tb_])
                    kb.barrier()
                if MS == "tables2":
                    dma("sp", x_dst[0:128, 0:1024], MT[:].rearrange("p a b -> p (a b)"), R=[tb_])
                    dma("sp", x_dst[128:256, 0:32], dec[:].rearrange("p a b -> p (a b)"), R=[tb_])
                    dma("sp", x_dst[128:256, 32:40], CD[:].rearrange("p a b -> p (a b)"), R=[tb_])
                    dma("sp", x_dst[128:256, 64:80], lgb[:], R=[tb_])
                    kb.barrier()
                    return
                fr = make_front(tR, [0])
                xin = Ring(kb, tR, "rxin", 2, [128, D], F32)
                ropt = Ring(kb, tR, "ropt", 2, [128, 256], F32)
                t1r = Ring(kb, tR, "t1r", 1, [128, 1024], F32)
                t2r = Ring(kb, tR, "t2r", 1, [128, 1024], F32)
                krr = Ring(kb, tR, "krr", 1, [128, 512], F32)
                wtr = Ring(kb, tR, "wtr", 2, [128, 2, 8], F32)
                kfr = Ring(kb, tR, "kfr", 2, [128, 2, 512], BF16)
                vbr = Ring(kb, tR, "vbr", 2, [128, 512], BF16)
                tmps = make_tmps(tR, [5])

                NO = 18
                def headO(i):
                    isctx = i >= 16
                    xt, xbuf = xin.next()
                    src = x_ctx[(i - 16) * 128:(i - 15) * 128, :] if isctx else x_oth[i * 128:(i + 1) * 128, :]
                    dma("sp", xt[:], src, W=[xbuf])
                    rt, rtb = ropt.next()
                    dma("sp", rt[:, 0:128], rope_oth[i * 128:(i + 1) * 128, :], W=[rtb])
                    fa = front_a(xt[:], xbuf, (G1c if isctx else G1m)[:], (SHc if isctx else SHm)[:], cbuf if isctx else mbuf, fr)
                    return xt, xbuf, rt, rtb, fa

                def bodyO(i, hd):
                    isctx = i >= 16
                    xt, xbuf, rt, rtb, fa = hd
                    hT, hTb, _, _ = front_b(fa, fr)
                    proj(hT, hTb, wq, wqb, 512, 1)
                    proj(hT, hTb, wq, wqb, 1024, 2)
                    t1, t1b = t1r.next()
                    t2, t2b = t2r.next()
                    rope(bank(1), [psb[1]], rt[:, 0:64], rt[:, 64:128], rtb, t1[:, 0:512], t1b, t2[:, 0:512], t2b)
                    kr, krb = krr.next()
                    op("pool", lambda: G.tensor_tensor(out=kr[:], in0=t1[:, 0:512], in1=t2[:, 0:512], op=ALU.add), R=[t1b, t2b], W=[krb])
                    wt, wtb = wtr.next()
                    for d_ in range(2):
                        op("act", lambda: A.activation(out=wt[:, d_, :], in_=lgb[:, d_ * 8:(d_ + 1) * 8], func=AF.Exp,
                                                       scale=ei[:, d_ * 18 + i:d_ * 18 + i + 1]), R=[tb_], W=[wtb])
                    kf, kfb = kfr.next()
                    for d_ in range(2):
                        op("pool", lambda: G.tensor_tensor(out=h3(kf[:, d_, :]), in0=h3(kr[:]), in1=b3(wt[:, d_, :], 8), op=ALU.mult),
                           R=[krb, wtb], W=[kfb])
                    vb, vbb = vbr.next()
                    op("act", lambda: A.copy(out=vb[:], in_=bank(2)), R=[psb[2]], W=[vbb])
                    for d_ in range(2):
                        kk = 6 + d_
                        for h in range(8):
                            hp, hh = h // 2, h % 2
                            op("pe", lambda: P.matmul(ps[hh * 64:(hh + 1) * 64, kk * 512 + hp * 64:kk * 512 + (hp + 1) * 64],
                                                      kf[:, d_, h * 64:(h + 1) * 64], vb[:, h * 64:(h + 1) * 64],
                                                      start=(i == 0 and h < 2), stop=(i == NO - 1), skip_group_check=True),
                               R=[kfb, vbb], W=[psb[kk]], inc=(h == 7))
                    if isctx:
                        proj(hT, hTb, wn, wnb, 0, 3)
                        proj(hT, hTb, wn, wnb, 512, 4)
                        norm_heads_T(3, kg, tmps, KcT[:, :, (i - 16) * 128:(i - 15) * 128], ctxb)
                        op("act", lambda: A.copy(out=Vc[:, i - 16, :], in_=bank(4)), R=[psb[4]], W=[ctxb])
                pipelined(NO, headO, bodyO)
                for d_ in range(2):
                    op("act", lambda: A.copy(out=Sinit[:, d_, :], in_=bank(6 + d_, 256)), R=[psb[6 + d_]], W=[sib])
                if MS == "sweepO":
                    dma("sp", x_dst[0:128, 0:512], Sinit[:].rearrange("p a b -> p (a b)"), R=[sib])
                    dma("sp", x_dst[128:256, 0:512].bitcast(BF16), KcT[:].rearrange("p a b -> p (a b)"), R=[ctxb])
                    dma("sp", x_dst[256:384, 0:512].bitcast(BF16), Vc[:].rearrange("p a b -> p (a b)"), R=[ctxb])
                    kb.barrier()
                    return

                def headL(c):
                    xt, xbuf = xin.next()
                    dma("sp", xt[:], x_loc[c * 128:(c + 1) * 128, :], W=[xbuf])
                    rt, rtb = ropt.next()
                    dma("sp", rt[:], rope_loc[c * 128:(c + 1) * 128, :], W=[rtb])
                    fa = front_a(xt[:], xbuf, G1m[:], SHm[:], mbuf, fr)
                    return xt, xbuf, rt, rtb, fa

                def bodyE1(c, hd):
                    xt, xbuf, rt, rtb, fa = hd
                    hT, hTb, _, _ = front_b(fa, fr)
                    proj(hT, hTb, wq, wqb, 512, 1)
                    proj(hT, hTb, wq, wqb, 1024, 2)
                    t1, t1b = t1r.next()
                    t2, t2b = t2r.next()
                    rope(bank(1), [psb[1]], rt[:, 64:128], rt[:, 192:256], rtb, t1[:, 0:512], t1b, t2[:, 0:512], t2b)
                    kr, krb = krr.next()
                    op("pool", lambda: G.tensor_tensor(out=kr[:], in0=t1[:, 0:512], in1=t2[:, 0:512], op=ALU.add), R=[t1b, t2b], W=[krb])
                    kf, kfb = kfr.next()
                    op("pool", lambda: G.tensor_tensor(out=h3(kf[:, 0, :]), in0=h3(kr[:]), in1=b3(dec[:, 3, :], 8), op=ALU.mult),
                       R=[krb, tb_], W=[kfb])
                    vb, vbb = vbr.next()
                    op("act", lambda: A.copy(out=vb[:], in_=bank(2)), R=[psb[2]], W=[vbb])
                    kk = 3 + (c % 2)
                    for h in range(8):
                        hp, hh = h // 2, h % 2
                        op("pe", lambda: P.matmul(ps[hh * 64:(hh + 1) * 64, kk * 512 + hp * 64:kk * 512 + (hp + 1) * 64],
                                                  kf[:, 0, h * 64:(h + 1) * 64], vb[:, h * 64:(h + 1) * 64], start=True, stop=True),
                           R=[kfb, vbb], W=[psb[kk]], inc=(h == 7))
                    op("act", lambda: A.copy(out=DBt[:, c, :], in_=bank(kk, 256)), R=[psb[kk]], W=[dbb[c]])
                pipelined(NT, headL, bodyE1)
                srun = Ring(kb, tR, "srun", 2, [128, 256], F32)
                stmp = Ring(kb, tR, "stmp", 2, [128, 256], F32)
                q4 = lambda ap: ap.rearrange("p (q e) -> p q e", e=64)
                cdb = lambda d_: CD[:, d_, :].unsqueeze(2).to_broadcast([128, 4, 64])
                cur, curb = srun.next()
                op("dve", lambda: V.tensor_copy(cur[:], Sinit[:, 1, :]), R=[sib], W=[curb])
                for c in range(NT - 1, -1, -1):
                    op("act", lambda: A.copy(out=SBst[:, c, :], in_=cur[:]), R=[curb], W=[sbb[c]])
                    if c == 0:
                        break
                    tm, tmb_ = stmp.next()
                    op("pool", lambda: G.tensor_tensor(out=q4(tm[:]), in0=q4(cur[:]), in1=cdb(1), op=ALU.mult), R=[curb, tb_], W=[tmb_])
                    nxt, nxtb = srun.next()
                    op("dve", lambda: V.tensor_tensor(out=nxt[:], in0=tm[:], in1=DBt[:, c, :], op=ALU.add), R=[tmb_, dbb[c]], W=[nxtb])
                    cur, curb = nxt, nxtb
                if MS == "sweepE1":
                    for c in range(NT):
                        dma("sp", x_dst[c * 128:(c + 1) * 128, 0:128].bitcast(BF16), SBst[:, c, :], R=[sbb[c]])
                    kb.barrier()
                    return

                qkr_r = Ring(kb, tR, "qkr", 2, [128, 1024], BF16)
                qkT_r = Ring(kb, tR, "qkT", 2, [128, 8, 128], BF16)
                qz_r = Ring(kb, tR, "qz", 2, [128, 2, 4, 128], BF16)
                for qi_ in range(2):
                    op("pool", lambda: G.memset(qz_r.t[qi_][:], 0.0), W=[qz_r.b[qi_]])
                gs_r = Ring(kb, tR, "gsr", 2, [128, 512], BF16)
                gs2_r = Ring(kb, tR, "gs2r", 2, [128, 512], BF16)
                Pm_r = Ring(kb, tR, "Pmr", 1, [128, 8, 128], BF16)
                o_r = Ring(kb, tR, "or", 2, [128, 512], F32)
                s8r = Ring(kb, tR, "rs8", 2, [128, 24], F32)
                SFr = Ring(kb, tR, "SFr", 2, [128, 256], F32)
                SFbr = Ring(kb, tR, "SFbr", 2, [128, 256], BF16)
                SF, SFb_ = SFr.next()
                op("dve", lambda: V.tensor_copy(SF[:], Sinit[:, 0, :]), R=[sib], W=[SFb_])
                SFh, SFhb = SFbr.next()
                op("act", lambda: A.copy(out=SFh[:], in_=SF[:]), R=[SFb_], W=[SFhb])
                RS_ = {"SF": SF, "SFb": SFb_, "SFh": SFh, "SFhb": SFhb}

                def bodyR(c, hd):
                    xt, xbuf, rt, rtb, fa = hd
                    SF, SFb_, SFh, SFhb = RS_["SF"], RS_["SFb"], RS_["SFh"], RS_["SFhb"]
                    hT, hTb, _, _ = front_b(fa, fr)
                    for j in range(4):
                        proj(hT, hTb, wq, wqb, j * 512, 1 + j)
                    t1, t1b = t1r.next()
                    t2, t2b = t2r.next()
                    for j in range(2):
                        rope(bank(1 + j), [psb[1 + j]], rt[:, j * 64:(j + 1) * 64], rt[:, 128 + j * 64:128 + (j + 1) * 64], rtb,
                             t1[:, j * 512:(j + 1) * 512], t1b, t2[:, j * 512:(j + 1) * 512], t2b)
                    qkr, qkrb = qkr_r.next()
                    op("pool", lambda: G.tensor_tensor(out=qkr[:], in0=t1[:], in1=t2[:], op=ALU.add), R=[t1b, t2b], W=[qkrb])
                    vb, vbb = vbr.next()
                    op("act", lambda: A.copy(out=vb[:], in_=bank(3)), R=[psb[3]], W=[vbb])
                    gs, gsb = gs_r.next()
                    op("act", lambda: A.activation(out=gs[:], in_=bank(4), func=AF.Silu), R=[psb[4]], W=[gsb])
                    gs2, gs2b = gs2_r.next()
                    op("pool", lambda: G.tensor_tensor(out=h3(gs2[:]), in0=h3(gs[:]), in1=retg.unsqueeze(1).to_broadcast([128, 8, 64]),
                                                       op=ALU.mult), R=[gsb, tb_], W=[gs2b])
                    kf, kfb = kfr.next()
                    op("pool", lambda: G.tensor_tensor(out=h3(kf[:, 0, :]), in0=h3(qkr[:, 512:1024]), in1=b3(dec[:, 2, :], 8), op=ALU.mult),
                       R=[qkrb, tb_], W=[kfb])
                    qkT, qkTb = qkT_r.next()
                    for j in range(8):
                        op("pe", lambda: P.transpose(bank_bf(0, 128, j * 128), qkr[:, j * 128:(j + 1) * 128], ident[:]),
                           R=[qkrb, b_const], W=[psb[0]], inc=(j == 7))
                    op("dve", lambda: V.tensor_copy(qkT[:].rearrange("p a b -> p (a b)"), bank_bf(0)), R=[psb[0]], W=[qkTb])
                    qz, qzb = qz_r.next()
                    for hh in range(2):
                        pr = slice(hh * 64, (hh + 1) * 64)
                        op("act", lambda: A.copy(out=qz[pr, hh, :, :], in_=qkT[pr, 0:4, :]), R=[qkTb], W=[qzb])
                    for h in range(8):
                        hp, hh = h // 2, h % 2
                        op("pe", lambda: P.matmul(bank(5 + h // 4, 128, (h % 4) * 128), qkT[:, 4 + hp, :], qz[:, hh, hp, :], start=True, stop=True),
                           R=[qkTb, qzb], W=[psb[5 + h // 4]], inc=(h % 4 == 3))
                    Pm, Pmb = Pm_r.next()
                    for b_ in range(2):
                        op("dve", lambda: V.tensor_tensor(out=Pm[:, 4 * b_:4 * b_ + 4, :], in0=bank(5 + b_).rearrange("p (a b) -> p a b", b=128),
                                                          in1=MT[:, 4 * b_:4 * b_ + 4, :], op=ALU.mult), R=[psb[5 + b_], tb_], W=[Pmb])
                    for h in range(8):
                        op("pe", lambda: P.matmul(bank(7, 64, h * 64), Pm[:, h, :], vb[:, h * 64:(h + 1) * 64], start=True, stop=True),
                           R=[Pmb, vbb], W=[psb[7]], inc=(h == 7))
                    for h in range(8):
                        hp, hh = h // 2, h % 2
                        op("pe", lambda: P.matmul(bank(3, 64, h * 64), qz[:, hh, hp, :], SFh[:, hp * 64:(hp + 1) * 64], start=True, stop=True),
                           R=[qzb, SFhb], W=[psb[3]], inc=(h == 7))
                    for h in range(8):
                        hp, hh = h // 2, h % 2
                        op("pe", lambda: P.matmul(bank(4, 64, h * 64), qz[:, hh, hp, :], SBst[:, c, hp * 64:(hp + 1) * 64], start=True, stop=True),
                           R=[qzb, sbb[c]], W=[psb[4]], inc=(h == 7))
                    o1, o1b = o_r.next()
                    o2, o2b = o_r.next()
                    op("dve", lambda: V.tensor_tensor(out=h3(o1[:]), in0=h3(bank(3)), in1=b3(dec[:, 0, :], 8), op=ALU.mult), R=[psb[3], tb_], W=[o1b])
                    op("dve", lambda: V.tensor_tensor(out=h3(o2[:]), in0=h3(bank(4)), in1=b3(dec[:, 1, :], 8), op=ALU.mult), R=[psb[4], tb_], W=[o2b])
                    op("dve", lambda: V.tensor_tensor(out=o1[:], in0=bank(7), in1=o1[:], op=ALU.add), R=[psb[7], o1b], W=[o1b])
                    op("pool", lambda: G.tensor_tensor(out=o1[:], in0=o1[:], in1=o2[:], op=ALU.add), R=[o1b, o2b], W=[o1b])
                    op("pool", lambda: G.tensor_tensor(out=o2[:], in0=o1[:], in1=o1[:], op=ALU.mult), R=[o1b], W=[o2b])
                    s8, s8b = s8r.next()
                    op("dve", lambda: V.tensor_reduce(out=s8[:, 0:8], in_=h3(o2[:]), op=ALU.add, axis=AX.X), R=[o2b], W=[s8b])
                    op("act", lambda: A.activation(out=s8[:, 8:16], in_=s8[:, 0:8], func=AF.Sqrt, scale=1.0 / 64, bias=eps[:, 0:1]),
                       R=[s8b, tb_], W=[s8b])
                    op("dve", lambda: V.reciprocal(s8[:, 16:24], s8[:, 8:16]), R=[s8b], W=[s8b])
                    op("pool", lambda: G.tensor_tensor(out=h3(o2[:]), in0=h3(o1[:]), in1=b3(s8[:, 16:24], 8), op=ALU.mult), R=[o1b, s8b], W=[o2b])
                    op("pool", lambda: G.tensor_tensor(out=ret_out[:, c, :], in0=o2[:], in1=gs2[:], op=ALU.mult), R=[o2b, gs2b], W=[retb[c]])
                    if c < NT - 1:
                        for h in range(8):
                            hp, hh = h // 2, h % 2
                            op("pe", lambda: P.matmul(ps[hh * 64:(hh + 1) * 64, 1 * 512 + hp * 64:1 * 512 + (hp + 1) * 64],
                                                      kf[:, 0, h * 64:(h + 1) * 64], vb[:, h * 64:(h + 1) * 64], start=True, stop=True),
                               R=[kfb, vbb], W=[psb[1]], inc=(h == 7))
                        tm, tmb_ = stmp.next()
                        op("pool", lambda: G.tensor_tensor(out=q4(tm[:]), in0=q4(SF[:]), in1=cdb(0), op=ALU.mult), R=[SFb_, tb_], W=[tmb_])
                        SF, SFb_ = SFr.next()
                        op("dve", lambda: V.tensor_tensor(out=SF[:], in0=bank(1, 256), in1=tm[:], op=ALU.add), R=[psb[1], tmb_], W=[SFb_])
                        SFh, SFhb = SFbr.next()
                        op("act", lambda: A.copy(out=SFh[:], in_=SF[:]), R=[SFb_], W=[SFhb])
                        RS_.update({"SF": SF, "SFb": SFb_, "SFh": SFh, "SFhb": SFhb})
                pipelined(NT, headL, bodyR)
                kb.barrier()
            if MS == "sweepR":
                for c in range(NT):
                    dma("sp", x_dst[c * 128:(c + 1) * 128, 0:256].bitcast(BF16), ret_out[:, c, :], R=[retb[c]])
                kb.barrier()
                return

            with ExitStack() as tN:
                wn3 = kb.sb(tN, "w_n3", [128, 8, 1536], BF16)
                wn3b = Buf("wn3")
                wo = kb.sb(tN, "w_o", [128, 8, D], BF16)
                wob = Buf("wo")
                KT = kb.sb(tN, "KT", [128, 4, 20 * 128], BF16)
                VE = kb.sb(tN, "VE", [128, 20, 512], BF16)
                kvb = [Buf("kv%d" % i) for i in range(20)]
                biasT = kb.sb(tN, "biasT", [128, 8, 896], BF16)
                rowm = kb.sb(tN, "rowm", [2, 192], BF16)
                A2 = kb.sb(tN, "A2", [2, 128], BF16)
                nb_ = Buf("naconst")
                for q in range(3):
                    dma("pool", wn3[:, :, q * 512:(q + 1) * 512],
                        ab_w_in[:, 2048 + q * 512:2048 + (q + 1) * 512].rearrange("(k p) n -> p k n", p=128), W=[wn3b])
                for q in range(2):
                    dma("pool", wo[:, :, q * 512:(q + 1) * 512], ab_w_out[:, q * 512:(q + 1) * 512].rearrange("(k p) n -> p k n", p=128), W=[wob])
                for h in range(8):
                    dma("pool", biasT[:, h, :], bias_tab[h, :, :], W=[nb_])
                dma("pool", rowm[:], rowmask[:, :], W=[nb_])
                dma("pool", A2[:], a2_d[:, :], W=[nb_])
                fr = make_front(tN, [0])
                xin = Ring(kb, tN, "nxin", 2, [128, D], F32)
                tmps = make_tmps(tN, [3])
                def headE2(e_):
                    xt, xbuf = xin.next()
                    if e_ < 2:
                        src = x_halo[e_ * 128:(e_ + 1) * 128, :]
                    elif e_ < 18:
                        src = x_loc[(e_ - 2) * 128:(e_ - 1) * 128, :]
                    else:
                        src = x_halo[256 + (e_ - 18) * 128:256 + (e_ - 17) * 128, :]
                    dma("sp", xt[:], src, W=[xbuf])
                    return xt, xbuf, front_a(xt[:], xbuf, G1m[:], SHm[:], mbuf, fr)

                def bodyE2(e_, hd):
                    xt, xbuf, fa = hd
                    hT, hTb, _, _ = front_b(fa, fr)
                    proj(hT, hTb, wn3, wn3b, 512, 1)
                    proj(hT, hTb, wn3, wn3b, 1024, 2)
                    norm_heads_T(1, kg, tmps, KT[:, :, e_ * 128:(e_ + 1) * 128], kvb[e_])
                    op("act", lambda: A.copy(out=VE[:, e_, :], in_=bank(2)), R=[psb[2]], W=[kvb[e_]])
                pipelined(20, headE2, bodyE2)
                if MS == "sweepE2":
                    for e_ in range(16):
                        dma("sp", x_dst[e_ * 128:(e_ + 1) * 128, 0:256].bitcast(BF16), KT[:, :, e_ * 128:(e_ + 1) * 128], R=[kvb[e_]])
                        dma("sp", x_dst[e_ * 128:(e_ + 1) * 128, 256:512].bitcast(BF16), VE[:, e_, :], R=[kvb[e_]])
                    kb.barrier()
                    return
                qnT_r = Ring(kb, tN, "qnT", 2, [128, 4, 128], BF16)
                qnz_r = Ring(kb, tN, "qnz", 2, [128, 2, 4, 128], BF16)
                for qi_ in range(2):
                    op("pool", lambda: G.memset(qnz_r.t[qi_][:], 0.0), W=[qnz_r.b[qi_]])
                Pe_r = Ring(kb, tN, "Pe", 2, [128, 1024], BF16)
                PT_r = Ring(kb, tN, "PT", 2, [128, 8, 128], BF16)
                sm_r = Ring(kb, tN, "sm", 2, [128, 24], F32)
                mix_r = Ring(kb, tN, "mix", 2, [128, D], BF16)
                mixT_r = Ring(kb, tN, "mixT", 2, [128, 8, 128], BF16)
                yt_r = Ring(kb, tN, "nyt", 2, [128, 512], F32)
                xo_r = Ring(kb, tN, "nxo", 2, [128, D], F32)
                ab_ring = PsRing([2, 4])
                def headN(t_):
                    xt, xbuf = xin.next()
                    dma("sp", xt[:], x_loc[t_ * 128:(t_ + 1) * 128, :], W=[xbuf])
                    return xt, xbuf, front_a(xt[:], xbuf, G1m[:], SHm[:], mbuf, fr)

                def geomN(t_):
                    lo, hi = t_ - 2, t_ + 2
                    if t_ < 2:
                        hi = 3
                    if t_ > 13:
                        lo = 12
                    n = hi - lo + 1
                    return lo, hi, n, (n - 4) * 128

                def stA(t_, hd):
                    xt, xbuf, fa = hd
                    hT, hTb, _, _ = front_b(fa, fr)
                    proj(hT, hTb, wn3, wn3b, 0, 1)
                    qnT, qnTb = qnT_r.next()
                    tmps["ps"] = PsRing([6])
                    norm_heads_T(1, qg, tmps, qnT[:], qnTb)
                    qnz, qnzb = qnz_r.next()
                    for hh in range(2):
                        pr = slice(hh * 64, (hh + 1) * 64)
                        op("pool", lambda: G.tensor_copy(qnz[pr, hh, :, :], qnT[pr, :, :]), R=[qnTb], W=[qnzb])
                    sm, smb = sm_r.next()
                    return xt, xbuf, qnz, qnzb, sm, smb

                def stS(t_, cx, h):
                    xt, xbuf, qnz, qnzb, sm, smb = cx
                    lo, hi, n, nbk = geomN(t_)
                    kv_need = [kvb[j + 2] for j in range(lo, hi + 1)]
                    hp, hh = h // 2, h % 2
                    ka = ab_ring.next()
                    kbk = ka + 1
                    e0 = (lo + 2) * 128
                    d0 = (lo - t_ + 3) * 128
                    op("pe", lambda: P.matmul(bank(ka), qnz[:, hh, hp, :], KT[:, hp, e0:e0 + 512], start=True, stop=False),
                       R=[qnzb] + kv_need, W=[psb[ka]], inc=False)
                    op("pe", lambda: P.matmul(bank(ka), ident[:], biasT[:, h, d0:d0 + 512], start=False, stop=False),
                       R=[nb_, b_const], W=[psb[ka]], inc=False)
                    op("pe", lambda: P.matmul(bank(ka), A2[:], rowm[:, t_ * 12:t_ * 12 + 8].unsqueeze(2).to_broadcast([2, 8, 64]),
                                              start=False, stop=True), R=[nb_], W=[psb[ka]])
                    op("pe", lambda: P.matmul(bank(kbk, nbk), qnz[:, hh, hp, :], KT[:, hp, e0 + 512:e0 + 512 + nbk], start=True, stop=False),
                       R=[qnzb] + kv_need, W=[psb[kbk]], inc=False)
                    op("pe", lambda: P.matmul(bank(kbk, nbk), ident[:], biasT[:, h, d0 + 512:d0 + 512 + nbk], start=False, stop=False),
                       R=[nb_, b_const], W=[psb[kbk]], inc=False)
                    op("pe", lambda: P.matmul(bank(kbk, nbk), A2[:],
                                              rowm[:, t_ * 12 + 8:t_ * 12 + 8 + (n - 4) * 2].unsqueeze(2).to_broadcast([2, (n - 4) * 2, 64]),
                                              start=False, stop=True), R=[nb_], W=[psb[kbk]], inc=False)
                    op("pe", lambda: P.matmul(bank(kbk, 256, nbk), qnz[:, hh, hp, :], KcT[:, hp, :], start=True, stop=True),
                       R=[qnzb, ctxb], W=[psb[kbk]])
                    return ka, kbk

                def stETV(t_, cx, h, ka, kbk):
                    xt, xbuf, qnz, qnzb, sm, smb = cx
                    lo, hi, n, nbk = geomN(t_)
                    kv_need = [kvb[j + 2] for j in range(lo, hi + 1)]
                    Pe, Peb = Pe_r.next()
                    op("act", lambda: A.activation(out=Pe[:, 0:512], in_=bank(ka), func=AF.Exp, accum_out=sm[:, 2 * h:2 * h + 1]),
                       R=[psb[ka]], W=[Peb, smb])
                    op("act", lambda: A.activation(out=Pe[:, 512:768 + nbk], in_=bank(kbk, nbk + 256), func=AF.Exp,
                                                   accum_out=sm[:, 2 * h + 1:2 * h + 2]), R=[psb[kbk]], W=[Peb, smb])
                    nblk = n + 2
                    for bk in range(nblk):
                        op("pe", lambda: P.transpose(bank_bf(6, 128, bk * 128), Pe[:, bk * 128:(bk + 1) * 128], ident[:]),
                           R=[Peb, b_const], W=[psb[6]], inc=(bk == nblk - 1))
                    PT, PTb = PT_r.next()
                    op("dve", lambda: V.tensor_copy(PT[:, 0:nblk, :].rearrange("p a b -> p (a b)"), bank_bf(6, nblk * 128)), R=[psb[6]], W=[PTb])
                    for bk in range(nblk):
                        if bk < n:
                            rhs = VE[:, lo + 2 + bk, h * 64:(h + 1) * 64]
                        else:
                            rhs = Vc[:, bk - n, h * 64:(h + 1) * 64]
                        op("pe", lambda: P.matmul(bank(7, 64, h * 64), PT[:, bk, :], rhs, start=(bk == 0), stop=(bk == nblk - 1)),
                           R=[PTb, ctxb] + kv_need, W=[psb[7]], inc=(bk == nblk - 1))

                def stF(t_, cx):
                    xt, xbuf, qnz, qnzb, sm, smb = cx
                    op("dve", lambda: V.tensor_reduce(out=sm[:, 16:24], in_=sm[:, 0:16].rearrange("p (h two) -> p h two", two=2),
                                                      op=ALU.add, axis=AX.X), R=[smb], W=[smb])
                    op("dve", lambda: V.reciprocal(sm[:, 16:24], sm[:, 16:24]), R=[smb], W=[smb])
                    mix, mixb = mix_r.next()
                    op("dve", lambda: V.tensor_tensor(out=h3(mix[:, 512:1024]), in0=h3(bank(7)), in1=b3(sm[:, 16:24], 8), op=ALU.mult),
                       R=[psb[7], smb], W=[mixb])
                    op("pool", lambda: G.tensor_copy(mix[:, 0:512], ret_out[:, t_, :]), R=[retb[t_]], W=[mixb])
                    mixT, mixTb = mixT_r.next()
                    for dk in range(8):
                        op("pe", lambda: P.transpose(bank_bf(0, 128, dk * 128), mix[:, dk * 128:(dk + 1) * 128], ident[:]),
                           R=[mixb, b_const], W=[psb[0]], inc=(dk == 7))
                    op("act", lambda: A.copy(out=mixT[:].rearrange("p a b -> p (a b)"), in_=bank_bf(0)), R=[psb[0]], W=[mixTb])
                    xo, xob = xo_r.next()
                    for hf in range(2):
                        k = 1 if hf == 0 else 6
                        for dk in range(8):
                            op("pe", lambda: P.matmul(bank(k), mixT[:, dk, :], wo[:, dk, hf * 512:(hf + 1) * 512], start=(dk == 0), stop=(dk == 7)),
                               R=[mixTb, wob], W=[psb[k]], inc=(dk == 7))
                        yt, ytb = yt_r.next()
                        op("dve", lambda: V.tensor_tensor(out=yt[:], in0=bank(k), in1=gmb[:, hf * 512:(hf + 1) * 512], op=ALU.mult),
                           R=[psb[k], mbuf], W=[ytb])
                        op("pool", lambda: G.tensor_tensor(out=xo[:, hf * 512:(hf + 1) * 512], in0=yt[:], in1=xt[:, hf * 512:(hf + 1) * 512],
                                                           op=ALU.add), R=[ytb, xbuf], W=[xob])
                    dma("sp", x_dst[t_ * 128:(t_ + 1) * 128, :], xo[:], R=[xob])
                hdsN = {0: headN(0)}
                cxN = {0: stA(0, hdsN[0])}
                for t_ in range(NT):
                    if t_ + 1 < NT:
                        hdsN[t_ + 1] = headN(t_ + 1)
                    cx = cxN.pop(t_)
                    pend = stS(t_, cx, 0)
                    for h in range(8):
                        cur = pend
                        if h < 7:
                            pend = stS(t_, cx, h + 1)
                        if h == 3 and t_ + 1 < NT:
                            cxN[t_ + 1] = stA(t_ + 1, hdsN.pop(t_ + 1))
                        stETV(t_, cx, h, *cur)
                    stF(t_, cx)
                kb.barrier()

    if mode == 'full':
        mixer0_phase(xa)
        moe_phase(0, xa, xb)
        sgu_phase(xb, xc)
        moe_phase(1, xc, y_out)
    elif mode == 'mix0':
        mixer0_phase(y_out)
    elif mode == 'pro':
        with ExitStack() as t:
            wring = Ring(kb, t, "adaw", 2, [128, 8, 1024], BF16)
            bring = Ring(kb, t, "adab", 2, [1, 1024], BF16)
            o1 = kb.sb(t, "o1", [128, D], F32)
            ob = Buf("o1")
            ada_chunk(0, 2, silc, o1[:], ob, wring, bring)
            dma("sp", y_out[0:128, :], o1[:], R=[ob])
            G1, SH, mb = make_mod(t, 1, 3, 4, nffn_g[1:2, :], silcc, "mf", wring, bring)
            dma("sp", y_out[128:256, :], G1[:], R=[mb])
            dma("sp", y_out[256:384, :], SH[:], R=[mb])
            kb.barrier()
    elif mode == 'moe0':
        moe_phase(0, dbg_x, y_out)
    elif mode == 'sgu':
        sgu_phase(dbg_x, y_out)
    elif mode == 'moe1':
        moe_phase(1, dbg_x, y_out)
    kb.barrier()
    return kb


def _bf(a):
    return np.ascontiguousarray(a).astype(np.float32)


def _rope_tables(pos, scale_k):
    inv = (10000.0 ** (-np.arange(16, dtype=np.float32) / 16.0)).astype(np.float32)
    rows = (pos // 64).astype(np.float32)
    cols = (pos % 64).astype(np.float32)
    ar = rows[:, None] * inv[None, :]
    ac = cols[:, None] * inv[None, :]
    cr, sr, cc_, sc = np.cos(ar), np.sin(ar), np.cos(ac), np.sin(ac)
    C = np.concatenate([cr, cr, cc_, cc_], axis=1).astype(np.float32)
    S = np.concatenate([-sr, sr, -sc, sc], axis=1).astype(np.float32)
    return C, S


def prep_inputs(inputs):
    f = lambda k: np.asarray(inputs[k], dtype=np.float32)
    x, c, ctx, c_ctx = f('x'), f('c'), f('ctx'), f('c_ctx')
    w_in = f('ab_w_in')[0]
    blk = lambda i: w_in[:, i * 512:(i + 1) * 512]
    w_in_p = np.ascontiguousarray(np.concatenate([blk(4), blk(0), blk(1), blk(5), blk(6), blk(2), blk(3)], axis=1))
    rpb = f('na_rpb')[0]
    tab = np.full((8, 2, 64, 7, 2, 64), NEG, dtype=np.float32)
    w = np.arange(64)
    c0 = np.clip(w - 8, 0, 48)
    for a in range(2):
        for di in range(7):
            for b in range(2):
                rel = 2 * di + b - a + 1
                if rel < 0 or rel > 14:
                    continue
                for wi in range(64):
                    ccs = np.arange(c0[wi], c0[wi] + 16)
                    tab[:, a, wi, di, b, ccs] = rpb[:, rel, ccs - wi + 15]
    tab = tab.reshape(8, 128, 896)
    shared = {
        'ada_w': f('ada_w'), 'ada_b': f('ada_b'), 'norm_mix_g': f('norm_mix_g'), 'norm_ffn_g': f('norm_ffn_g'),
        'router_w': f('router_w'), 'router_bias': f('router_bias').reshape(1, 16),
        'moe_w_gate': f('moe_w_gate'), 'moe_w_up': f('moe_w_up'), 'moe_w_down': f('moe_w_down'),
        'ab_w_in': w_in_p, 'ab_w_out': f('ab_w_out')[0], 'ret_decay': f('ret_decay')[0].reshape(1, 16),
        'head_g': np.concatenate([f('ret_norm_g')[0], f('na_q_g')[0], f('na_k_g')[0]]).reshape(1, 192),
        'bias_tab': tab,
        'sgu_w_in': f('sgu_w_in')[0], 'sgu_b_in': f('sgu_b_in')[0].reshape(1, 6144),
        'sgu_norm_g': f('sgu_norm_g')[0].reshape(1, 3072), 'sgu_w_s': f('sgu_w_s')[0],
        'sgu_bsT': np.ascontiguousarray(f('sgu_b_s')[0].T), 'sgu_w_out': f('sgu_w_out')[0],
        'cc_row': c_ctx.reshape(1, D),
        'ident': np.eye(128, dtype=np.float32),
        'a2': np.repeat(np.eye(2, dtype=np.float32), 64, axis=1),
    }
    jj = np.arange(128, dtype=np.float32)[:, None]
    ii = np.arange(128, dtype=np.float32)[None, :]
    DF = np.where(ii >= jj, ii - jj, BIG).astype(np.float32)
    DB = np.where(jj >= ii, jj - ii, BIG).astype(np.float32)
    p = np.arange(128, dtype=np.float32)
    pos = np.stack([p + 1, 128 - p, 127 - p, p], axis=1)
    shared['cst'] = np.concatenate([DF, DB, pos], axis=1).astype(np.float32)
    maps = []
    for core in range(8):
        b, s = core // 2, core % 2
        lo = s * TOK
        m = dict(shared)
        m['x_loc'] = np.ascontiguousarray(x[b, lo:lo + TOK])
        m['x_oth'] = np.ascontiguousarray(x[b, (1 - s) * TOK:(2 - s) * TOK])
        halo = np.zeros((512, D), np.float32)
        if s == 1:
            halo[0:256] = x[b, lo - 256:lo]
        else:
            halo[256:512] = x[b, lo + TOK:lo + TOK + 256]
        m['x_halo'] = halo
        m['x_ctx'] = np.ascontiguousarray(ctx[b])
        m['c_row'] = c[b].reshape(1, D)
        C, S = _rope_tables(np.arange(lo, lo + TOK), 0.125)
        m['rope_loc'] = np.concatenate([C, C * 0.125, S, S * 0.125], axis=1).astype(np.float32)
        Co, So = _rope_tables(np.arange((1 - s) * TOK, (2 - s) * TOK), 0.125)
        ro = np.concatenate([Co * 0.125, So * 0.125], axis=1)
        rc = np.concatenate([np.full((256, 64), 0.125, np.float32), np.zeros((256, 64), np.float32)], axis=1)
        m['rope_oth'] = np.concatenate([ro, rc], axis=0).astype(np.float32)
        mo = np.arange(TOK, dtype=np.float32)
        n = np.arange(256, dtype=np.float32)
        if s == 1:
            EFo = 2047.0 - mo
            EBo = np.full(TOK, BIG, np.float32)
        else:
            EFo = np.full(TOK, BIG, np.float32)
            EBo = mo
        EFc = 255.0 - n + 2048.0 * s
        EBc = n + 2048.0 * (1 - s)
        EF = np.concatenate([EFo, EFc]).reshape(18, 128).T
        EB = np.concatenate([EBo, EBc]).reshape(18, 128).T
        m['exp_init'] = np.ascontiguousarray(np.concatenate([EF, EB], axis=1)).astype(np.float32)
        rm = np.full((2, 16, 6, 2), NEG, np.float32)
        for t in range(16):
            lo_t, hi_t = t - 2, t + 2
            if t < 2:
                hi_t = 3
            if t > 13:
                lo_t = 12
            for a in range(2):
                r = 32 * s + 2 * t + a
                r0 = min(max(r - 4, 0), 56)
                for sl, j in enumerate(range(lo_t, hi_t + 1)):
                    for bb in range(2):
                        kr = 32 * s + 2 * j + bb
                        if r0 <= kr <= r0 + 7:
                            rm[a, t, sl, bb] = 0.0
        m['rowmask'] = rm.reshape(2, 192)
        m['dbg_x'] = np.zeros((TOK, D), np.float32)
        maps.append(m)
    return maps


_CACHE = {}


def kernel(**inputs):
    maps = prep_inputs(inputs)
    if 'full' not in _CACHE:
        _CACHE['full'] = build('full')
    kb = _CACHE['full']
    maps = [{k: v for k, v in m.items() if k in kb.declared} for m in maps]
    res = run_bass_kernel_spmd(kb.nc, maps, core_ids=list(range(8)))
    out = np.zeros((4, 4096, D), np.float32)
    for core in range(8):
        b, s = core // 2, core % 2
        out[b, s * TOK:(s + 1) * TOK] = res.results[core]['y_out']
    return out
```

```python
import numpy as np
import ml_dtypes
from contextlib import ExitStack
import concourse.bass as bass
import concourse.mybir as mybir
from concourse.bass_utils import run_bass_kernel_spmd

F32 = mybir.dt.float32
BF16 = mybir.dt.bfloat16
AF = mybir.ActivationFunctionType
ALU = mybir.AluOpType
AX = mybir.AxisListType

D = 1024
NT = 16
TOK = 2048
NEG = -30000.0
EPS = 1e-6
BIG = 1.0e9


class Buf:
    __slots__ = ("name", "w", "r", "excl")

    def __init__(self, name="", excl=False):
        self.name = name
        self.w = None
        self.r = []
        self.excl = excl


class Eng:
    def __init__(self, name, h, sem):
        self.name = name
        self.h = h
        self.sem = sem
        self.count = 0
        self.waited = {}
        self.dsems = []
        self.dcnt = []
        self.di = 0


class KB:
    def __init__(self):
        self.nc = bass.Bass("TRN2", target_bir_lowering=False)
        self.es = ExitStack()
        nc = self.nc
        self.E = {}
        for name, h in (("pe", nc.tensor), ("act", nc.scalar), ("dve", nc.vector),
                        ("pool", nc.gpsimd), ("sp", nc.sync)):
            sem = self.es.enter_context(nc.semaphore("s_" + name))
            self.E[name] = Eng(name, h, sem)
        for qn in ("sp", "pool", "act"):
            q = self.E[qn]
            for i in range(8):
                q.dsems.append(self.es.enter_context(nc.semaphore("d_%s%d" % (qn, i))))
                q.dcnt.append(0)
        self.dma_events = []

    def _wait(self, e, ev):
        key, sem, val = ev
        if e.waited.get(key, 0) >= val:
            return
        e.h.wait_ge(sem, val)
        e.waited[key] = val

    def _deps(self, e, R, W):
        for b in R:
            if b.w is not None:
                if b.w[0] == e.name and e.name == "pe":
                    continue
                self._wait(e, b.w)
        for b in W:
            if b.w is not None and b.w[0] != e.name:
                self._wait(e, b.w)
            for ev in b.r:
                if ev[0] != e.name:
                    self._wait(e, ev)

    def op(self, en, fn, R=(), W=(), inc=True):
        e = self.E[en]
        if any(b.excl for b in R):
            W = list(W) + [b for b in R if b.excl and b not in W]
            R = [b for b in R if not b.excl]
        self._deps(e, R, W)
        ins = fn()
        val = e.count + 1
        if inc:
            ins.then_inc(e.sem, 1)
            e.count = val
        ev = (en, e.sem, val)
        for b in W:
            b.w = ev
            b.r = []
        for b in R:
            b.r.append(ev)
        return ins

    def dma(self, qn, out, in_, R=(), W=()):
        q = self.E[qn]
        self._deps(q, R, W)
        slot = q.di % len(q.dsems)
        q.di += 1
        sem = q.dsems[slot]
        key = "d_%s%d" % (qn, slot)
        if q.dcnt[slot] > 0:
            self._wait(q, (key, sem, 16 * q.dcnt[slot]))
        q.dcnt[slot] += 1
        q.h.dma_start(out=out, in_=in_).then_inc(sem, 16)
        ev = (key, sem, 16 * q.dcnt[slot])
        for b in W:
            b.w = ev
            b.r = []
        for b in R:
            b.r.append(ev)
        self.dma_events.append(ev)

    def barrier(self):
        evs = [(n, e.sem, e.count) for n, e in self.E.items() if e.count > 0]
        last = {}
        for ev in self.dma_events:
            last[ev[0]] = ev
        evs += list(last.values())
        self.dma_events = list(last.values())
        for n, e in self.E.items():
            for ev in evs:
                if ev[0] != n:
                    self._wait(e, ev)

    def sb(self, es, name, shape, dt):
        self.uid = getattr(self, "uid", 0) + 1
        return es.enter_context(self.nc.sbuf_tensor("sb%d_%s" % (self.uid, name), shape, dt))


class Ring:
    def __init__(self, kb, es, name, n, shape, dt):
        self.t = [kb.sb(es, "%s%d" % (name, i), shape, dt) for i in range(n)]
        self.b = [Buf("%s%d" % (name, i)) for i in range(n)]
        self.i = 0

    def next(self):
        k = self.i % len(self.t)
        self.i += 1
        return self.t[k], self.b[k]


def build(mode='full'):
    kb = KB()
    nc = kb.nc
    es = kb.es
    op = kb.op
    dma = kb.dma
    V = nc.vector
    A = nc.scalar
    P = nc.tensor
    G = nc.gpsimd

    def din(name, shape, dt=F32):
        return nc.dram_tensor(name, list(shape), dt, kind="ExternalInput").ap()

    SHAPES = {
        "x_loc": [TOK, D], "x_oth": [TOK, D], "x_halo": [512, D], "x_ctx": [256, D], "c_row": [1, D], "cc_row": [1, D],
        "ada_w": [2, D, 6 * D], "ada_b": [2, 6 * D], "norm_mix_g": [2, D], "norm_ffn_g": [2, D],
        "router_w": [D, 16], "router_bias": [1, 16],
        "moe_w_gate": [2, 16, D, 512], "moe_w_up": [2, 16, D, 512], "moe_w_down": [2, 16, 512, D],
        "ab_w_in": [D, 3584], "ab_w_out": [D, D], "ret_decay": [1, 16], "head_g": [1, 192],
        "bias_tab": [8, 128, 896], "rowmask": [2, 192], "rope_loc": [TOK, 256], "rope_oth": [TOK + 256, 128],
        "exp_init": [128, 36], "cst": [128, 260], "ident": [128, 128], "a2": [2, 128],
        "sgu_w_in": [D, 6144], "sgu_b_in": [1, 6144], "sgu_norm_g": [1, 3072], "sgu_w_s": [8, 128, 128],
        "sgu_bsT": [128, 8], "sgu_w_out": [3072, D], "dbg_x": [TOK, D],
    }
    declared = {}

    class Lazy:
        def __init__(self, name):
            self.name = name

        def ap(self):
            if self.name not in declared:
                declared[self.name] = nc.dram_tensor(self.name, list(SHAPES[self.name]), F32, kind="ExternalInput").ap()
            return declared[self.name]

        def __getitem__(self, k):
            return self.ap()[k]

        def rearrange(self, *a, **kw):
            return self.ap().rearrange(*a, **kw)

    kb.declared = declared
    x_loc, x_oth, x_halo, x_ctx, c_row, cc_row = (Lazy(n) for n in ("x_loc", "x_oth", "x_halo", "x_ctx", "c_row", "cc_row"))
    ada_w, ada_b, nmix_g, nffn_g = (Lazy(n) for n in ("ada_w", "ada_b", "norm_mix_g", "norm_ffn_g"))
    router_w, router_b = Lazy("router_w"), Lazy("router_bias")
    w_gate, w_up, w_down = Lazy("moe_w_gate"), Lazy("moe_w_up"), Lazy("moe_w_down")
    ab_w_in, ab_w_out, ret_decay, hg = Lazy("ab_w_in"), Lazy("ab_w_out"), Lazy("ret_decay"), Lazy("head_g")
    bias_tab, rowmask, rope_loc, rope_oth = Lazy("bias_tab"), Lazy("rowmask"), Lazy("rope_loc"), Lazy("rope_oth")
    exp_init, cst, ident_d, a2_d = Lazy("exp_init"), Lazy("cst"), Lazy("ident"), Lazy("a2")
    sgu_w_in, sgu_b_in, sgu_ng, sgu_ws = Lazy("sgu_w_in"), Lazy("sgu_b_in"), Lazy("sgu_norm_g"), Lazy("sgu_w_s")
    sgu_bsT, sgu_w_out, dbg_x = Lazy("sgu_bsT"), Lazy("sgu_w_out"), Lazy("dbg_x")
    y_out = nc.dram_tensor("y_out", [TOK, D], F32, kind="ExternalOutput").ap()
    xa = nc.dram_tensor("xa_scr", [TOK, D], F32, kind="Internal").ap()
    xb = nc.dram_tensor("xb_scr", [TOK, D], F32, kind="Internal").ap()
    xc = nc.dram_tensor("xc_scr", [TOK, D], F32, kind="Internal").ap()

    ps = es.enter_context(nc.psum_tensor("ps", [128, 4096], F32))
    ps_bf = ps.bitcast(BF16)
    psb = [Buf("ps%d" % i, excl=True) for i in range(8)]

    class PsRing:
        def __init__(self, banks):
            self.banks = banks
            self.i = 0

        def next(self):
            k = self.banks[self.i % len(self.banks)]
            self.i += 1
            return k

    def bank(k, n=512, off=0):
        return ps[:, k * 512 + off:k * 512 + off + n]

    def bank_bf(k, n=1024, off=0):
        return ps_bf[:, k * 1024 + off:k * 1024 + off + n]

    g = ExitStack()
    es.enter_context(g)
    ident = kb.sb(g, "ident", [128, 128], BF16)
    identf = kb.sb(g, "identf", [128, 128], F32)
    ones1 = kb.sb(g, "ones1", [1, 128], BF16)
    silc = kb.sb(g, "silc", [128, 8, 128], BF16)
    silcc = kb.sb(g, "silcc", [128, 8, 128], BF16)
    b_const = Buf("const")
    dma("pool", ident[:], ident_d[:, :], W=[b_const])
    dma("sp", identf[:], ident_d[:, :], W=[b_const])
    op("dve", lambda: V.memset(ones1[:], 1.0), W=[b_const])

    with ExitStack() as t:
        crow = kb.sb(t, "crow", [128, 2, D], F32)
        csil = kb.sb(t, "csil", [128, 2, D], BF16)
        bt = Buf("crow")
        dma("sp", crow[:, 0, :], c_row[0:1, :].partition_broadcast(128), W=[bt])
        dma("sp", crow[:, 1, :], cc_row[0:1, :].partition_broadcast(128), W=[bt])
        op("act", lambda: A.activation(out=csil[:], in_=crow[:], func=AF.Silu), R=[bt], W=[bt])
        for j, dst in enumerate((silc, silcc)):
            for dk in range(8):
                op("pe", lambda: P.transpose(bank_bf(0, 128, dk * 128), csil[:, j, dk * 128:(dk + 1) * 128], ident[:]),
                   R=[bt, b_const], W=[psb[0]], inc=(dk == 7))
            op("dve", lambda: V.tensor_copy(dst[:].rearrange("p a b -> p (a b)"), bank_bf(0)), R=[psb[0]], W=[b_const])
        kb.barrier()

    def ada_chunk(l, j, lhs, out_ap, out_buf, wring, bring, post=None):
        wt, wb = wring.next()
        dma("pool", wt[:], ada_w[l, :, j * 1024:(j + 1) * 1024].rearrange("(k p) n -> p k n", p=128), W=[wb])
        bt_, bb = bring.next()
        dma("pool", bt_[:], ada_b[l:l + 1, j * 1024:(j + 1) * 1024], W=[bb])
        for hf in range(2):
            k = 6 + hf
            for dk in range(8):
                op("pe", lambda: P.matmul(bank(k), lhs[:, dk, :], wt[:, dk, hf * 512:(hf + 1) * 512],
                                          start=(dk == 0), stop=False), R=[wb, b_const], W=[psb[k]], inc=False)
            op("pe", lambda: P.matmul(bank(k), ones1[:], bt_[:, hf * 512:(hf + 1) * 512], start=False, stop=True),
               R=[bb, b_const], W=[psb[k]])
        if post is None:
            op("act", lambda: A.copy(out=out_ap, in_=ps[:, 6 * 512:8 * 512]), R=[psb[6], psb[7]], W=[out_buf])
        else:
            post(ps[:, 6 * 512:8 * 512], [psb[6], psb[7]])

    def front_a(xt, xbuf, G1, SH, gbuf, fr, want_f32T=False):
        junk, jb = fr["junk"].next()
        st, sbf = fr["st"].next()
        op("act", lambda: A.activation(out=junk[:], in_=xt, func=AF.Square, accum_out=st[:, 0:1]),
           R=[xbuf], W=[jb, sbf])
        op("act", lambda: A.activation(out=st[:, 1:2], in_=st[:, 0:1], func=AF.Sqrt, scale=1.0 / D, bias=fr["eps"][:, 0:1]),
           R=[sbf, b_const], W=[sbf])
        op("dve", lambda: V.reciprocal(st[:, 2:3], st[:, 1:2]), R=[sbf], W=[sbf])
        tmp, tb = fr["tmp"].next()
        op("dve", lambda: V.scalar_tensor_tensor(out=tmp[:], in0=xt, scalar=st[:, 2:3], in1=G1, op0=ALU.mult, op1=ALU.mult),
           R=[xbuf, sbf, gbuf], W=[tb])
        if want_f32T:
            h, hb = fr["h32"].next()
        else:
            h, hb = fr["h"].next()
        op("pool", lambda: G.tensor_tensor(out=h[:], in0=tmp[:], in1=SH, op=ALU.add), R=[tb, gbuf], W=[hb])
        return h, hb, want_f32T

    def front_b(fa, fr, bf_dst=None):
        h, hb, want_f32T = fa
        if not want_f32T:
            hT, hTb = fr["hT"].next()
            k = fr["ps"].next()
            for dk in range(8):
                op("pe", lambda: P.transpose(bank_bf(k, 128, dk * 128), h[:, dk * 128:(dk + 1) * 128], ident[:]),
                   R=[hb, b_const], W=[psb[k]], inc=(dk == 7))
            op("act", lambda: A.copy(out=hT[:].rearrange("p a b -> p (a b)"), in_=bank_bf(k)), R=[psb[k]], W=[hTb])
            return hT, hTb, None, None
        else:
            hT32, hT32b = fr["hT32"].next()
            dst, dstb = bf_dst
            for half in range(2):
                k = fr["ps"].next()
                for q in range(4):
                    dk = half * 4 + q
                    op("pe", lambda: P.transpose(bank(k, 128, q * 128), h[:, dk * 128:(dk + 1) * 128], identf[:]),
                       R=[hb, b_const], W=[psb[k]], inc=(q == 3))
                op("act", lambda: A.copy(out=dst[:, half * 4:(half + 1) * 4, :], in_=bank(k).rearrange("p (a b) -> p a b", b=128)),
                   R=[psb[k]], W=[dstb])
                op("dve", lambda: V.tensor_copy(hT32[:, half * 4:(half + 1) * 4, :].rearrange("p a b -> p (a b)"), bank(k)),
                   R=[psb[k]], W=[hT32b])
            return None, None, hT32, hT32b

    def front(xt, xbuf, G1, SH, gbuf, fr, want_f32T=False):
        return front_b(front_a(xt, xbuf, G1, SH, gbuf, fr, want_f32T), fr)

    def pipelined(n, head, body):
        nxt = head(0)
        for i in range(n):
            cur = nxt
            nxt = head(i + 1) if i + 1 < n else None
            body(i, cur)

    def make_front(t, ps_banks, f32T=False):
        fr = {}
        fr["junk"] = Ring(kb, t, "fjunk", 1, [128, D], BF16)
        fr["st"] = Ring(kb, t, "fst", 3, [128, 4], F32)
        fr["tmp"] = Ring(kb, t, "ftmp", 1, [128, D], F32)
        if f32T:
            fr["h32"] = Ring(kb, t, "fh32", 2, [128, D], F32)
            fr["hT32"] = Ring(kb, t, "fhT32", 2, [128, 8, 128], F32)
        else:
            fr["h"] = Ring(kb, t, "fh", 2, [128, D], BF16)
        if not f32T:
            fr["hT"] = Ring(kb, t, "fhT", 2, [128, 8, 128], BF16)
        fr["ps"] = PsRing(ps_banks)
        eps = kb.sb(t, "feps", [128, 1], F32)
        op("dve", lambda: V.memset(eps[:], EPS), W=[b_const])
        fr["eps"] = eps
        return fr

    def make_mod(t, l, jshift, jscale, gsrc, lhs, name, wring, bring):
        G1 = kb.sb(t, name + "G1", [128, D], F32)
        SH = kb.sb(t, name + "SH", [128, D], F32)
        gb = kb.sb(t, name + "gb", [128, D], F32)
        mb = Buf(name)
        dma("sp", gb[:], gsrc.partition_broadcast(128), W=[mb])
        ada_chunk(l, jshift, lhs, SH[:], mb, wring, bring)

        def post(psap, pbufs):
            op("dve", lambda: V.scalar_tensor_tensor(out=G1[:], in0=psap, scalar=1.0, in1=gb[:], op0=ALU.add, op1=ALU.mult),
               R=pbufs + [mb], W=[mb])
        ada_chunk(l, jscale, lhs, None, mb, wring, bring, post=post)
        return G1, SH, mb

    def moe_phase(l, x_src, x_dst):
        with ExitStack() as t:
            xres = kb.sb(t, "xres", [128, NT, D], F32)
            xrb = [Buf("xres%d" % i) for i in range(NT)]
            hfT = kb.sb(t, "hfT", [128, 8, TOK], BF16)
            hfTb = [Buf("hfT%d" % i) for i in range(NT)]
            comb = kb.sb(t, "comb", [128, NT, 16], F32)
            combb = [Buf("comb%d" % i) for i in range(NT)]
            rw = kb.sb(t, "rw", [128, 8, 16], F32)
            rbias = kb.sb(t, "rbias", [128, 16], F32)
            gfb = kb.sb(t, "gfb", [128, D], F32)
            gfbuf = Buf("gfb")
            cb = Buf("rconst")
            dma("sp", rw[:], router_w.rearrange("(k p) e -> p k e", p=128), W=[cb])
            dma("sp", rbias[:], router_b[0:1, :].partition_broadcast(128), W=[cb])
            for i in range(NT):
                dma("sp", xres[:, i, :], x_src[i * 128:(i + 1) * 128, :], W=[xrb[i]])
            with ExitStack() as t2:
                wring = Ring(kb, t2, "adaw", 2, [128, 8, 1024], BF16)
                bring = Ring(kb, t2, "adab", 2, [1, 1024], BF16)
                G1, SH, mb = make_mod(t2, l, 3, 4, nffn_g[l:l + 1, :], silc, "mf", wring, bring)
                ada_chunk(l, 5, silc, gfb[:], gfbuf, wring, bring)
                fr = make_front(t2, [0, 1], f32T=True)
                RB = kb.sb(t2, "RB", [128, 4, NT, 16], F32)
                RS = kb.sb(t2, "RS", [128, 6, NT, 4], F32)
                rb = Buf("RB")
                fas = {}
                t32 = {}
                for j in range(NT + 2):
                    if j < NT:
                        fas[j] = front_a(xres[:, j, :], xrb[j], G1[:], SH[:], mb, fr, want_f32T=True)
                    i = j - 1
                    if 0 <= i < NT:
                        _, _, hT32, hT32b = front_b(fas.pop(i), fr, bf_dst=(hfT[:, :, i * 128:(i + 1) * 128], hfTb[i]))
                        t32[i] = (hT32, hT32b)
                    i = j - 2
                    if 0 <= i < NT:
                        hT32, hT32b = t32.pop(i)
                        k = 2 + (i % 2)
                        for dk in range(8):
                            op("pe", lambda: P.matmul(bank(k, 16), hT32[:, dk, :], rw[:, dk, :], start=(dk == 0), stop=(dk == 7)),
                               R=[hT32b, cb], W=[psb[k]], inc=(dk == 7))
                        op("act", lambda: A.activation(out=RB[:, 0, i, :], in_=bank(k, 16), func=AF.Sigmoid), R=[psb[k]], W=[rb])
                aff = RB[:, 0]
                sel = RB[:, 1]
                tmp = RB[:, 2]
                msk = RB[:, 3]
                m1 = RS[:, 0]
                m2 = RS[:, 1]
                gs = RS[:, 2]
                goh = RS[:, 3]
                gm = RS[:, 4, :, 0]
                den = RS[:, 5, :, 0]
                rden = RS[:, 5, :, 1]
                g4 = lambda ap: ap.rearrange("p t (g e) -> p (t g) e", e=4)
                f4 = lambda ap: ap.rearrange("p t g -> p (t g)")
                b4 = lambda ap: f4(ap).unsqueeze(2).to_broadcast([128, NT * 4, 4])
                op("dve", lambda: V.tensor_tensor(out=sel, in0=aff, in1=rbias[:].unsqueeze(1).to_broadcast([128, NT, 16]), op=ALU.add),
                   R=[rb, cb], W=[rb])
                op("dve", lambda: V.tensor_reduce(out=f4(m1), in_=g4(sel), op=ALU.max, axis=AX.X), R=[rb], W=[rb])
                op("dve", lambda: V.tensor_tensor(out=g4(tmp), in0=g4(sel), in1=b4(m1), op=ALU.is_ge), R=[rb], W=[rb])
                op("dve", lambda: V.scalar_tensor_tensor(out=tmp, in0=tmp, scalar=-1.0e4, in1=sel, op0=ALU.mult, op1=ALU.add),
                   R=[rb], W=[rb])
                op("dve", lambda: V.tensor_reduce(out=f4(m2), in_=g4(tmp), op=ALU.max, axis=AX.X), R=[rb], W=[rb])
                op("dve", lambda: V.tensor_tensor(out=gs, in0=m1, in1=m2, op=ALU.add), R=[rb], W=[rb])
                op("dve", lambda: V.tensor_reduce(out=gm, in_=gs, op=ALU.max, axis=AX.X), R=[rb], W=[rb])
                op("dve", lambda: V.tensor_tensor(out=goh, in0=gs, in1=gm.unsqueeze(2).to_broadcast([128, NT, 4]), op=ALU.is_ge),
                   R=[rb], W=[rb])
                op("dve", lambda: V.tensor_tensor(out=g4(msk), in0=g4(sel), in1=b4(m2), op=ALU.is_ge), R=[rb], W=[rb])
                op("dve", lambda: V.tensor_tensor(out=g4(msk), in0=g4(msk), in1=b4(goh), op=ALU.mult), R=[rb], W=[rb])
                op("dve", lambda: V.tensor_tensor(out=msk, in0=msk, in1=aff, op=ALU.mult), R=[rb], W=[rb])
                op("dve", lambda: V.tensor_reduce(out=den, in_=msk, op=ALU.add, axis=AX.X), R=[rb], W=[rb])
                op("dve", lambda: V.reciprocal(rden, den), R=[rb], W=[rb])
                op("dve", lambda: V.tensor_tensor(out=comb[:], in0=msk, in1=rden.unsqueeze(2).to_broadcast([128, NT, 16]), op=ALU.mult),
                   R=[rb], W=combb)
                kb.barrier()
            with ExitStack() as t3:
                wg = Ring(kb, t3, "wg", 2, [128, 8, 512], BF16)
                wu = Ring(kb, t3, "wu", 2, [128, 8, 512], BF16)
                wd = Ring(kb, t3, "wd", 2, [128, 4, D], BF16)
                wd2 = Ring(kb, t3, "wd2", 2, [128, 4, D], BF16)
                sg = Ring(kb, t3, "sg", 2, [128, 512], BF16)
                at = Ring(kb, t3, "at", 2, [128, 4, 512], BF16)
                gu = PsRing([0, 1, 2, 3])
                yr = PsRing([4, 5, 6, 7])
                for e in range(16):
                    wgt, wgb = wg.next()
                    wut, wub = wu.next()
                    wdt, wdb = wd.next()
                    wd2t, wd2b = wd2.next()
                    dma("pool", wgt[:], w_gate[l, e].rearrange("(k p) n -> p k n", p=128), W=[wgb])
                    dma("pool", wut[:], w_up[l, e].rearrange("(k p) n -> p k n", p=128), W=[wub])
                    dma("pool", wdt[:], w_down[l, e].rearrange("(k p) n -> p k n", p=128), W=[wdb])
                    for fc in range(4):
                        op("pool", lambda: G.tensor_tensor(out=wd2t[:, fc, :], in0=wdt[:, fc, :], in1=gfb[:], op=ALU.mult),
                           R=[wdb, gfbuf], W=[wd2b])
                    for tg in range(4):
                        att, atb = at.next()
                        toks = slice(tg * 512, (tg + 1) * 512)
                        hb4 = hfTb[tg * 4:(tg + 1) * 4]
                        for fc in range(4):
                            kg = gu.next()
                            ku = gu.next()
                            for dk in range(8):
                                op("pe", lambda: P.matmul(bank(kg), wgt[:, dk, fc * 128:(fc + 1) * 128], hfT[:, dk, toks],
                                                          start=(dk == 0), stop=(dk == 7)), R=[wgb] + hb4, W=[psb[kg]], inc=(dk == 7))
                            for dk in range(8):
                                op("pe", lambda: P.matmul(bank(ku), wut[:, dk, fc * 128:(fc + 1) * 128], hfT[:, dk, toks],
                                                          start=(dk == 0), stop=(dk == 7)), R=[wub] + hb4, W=[psb[ku]], inc=(dk == 7))
                            sgt, sgb = sg.next()
                            op("act", lambda: A.activation(out=sgt[:], in_=bank(kg), func=AF.Silu), R=[psb[kg]], W=[sgb])
                            op("dve", lambda: V.tensor_tensor(out=att[:, fc, :], in0=bank(ku), in1=sgt[:], op=ALU.mult),
                               R=[psb[ku], sgb], W=[atb])
                        for ti in range(4):
                            i = tg * 4 + ti
                            for hf in range(2):
                                ky = yr.next()
                                for fc in range(4):
                                    op("pe", lambda: P.matmul(bank(ky), att[:, fc, ti * 128:(ti + 1) * 128],
                                                              wd2t[:, fc, hf * 512:(hf + 1) * 512], start=(fc == 0), stop=(fc == 3)),
                                       R=[atb, wd2b], W=[psb[ky]], inc=(fc == 3))
                                xs = xres[:, i, hf * 512:(hf + 1) * 512]
                                op("dve", lambda: V.scalar_tensor_tensor(out=xs, in0=bank(ky), scalar=comb[:, i, e:e + 1], in1=xs,
                                                                         op0=ALU.mult, op1=ALU.add),
                                   R=[psb[ky], combb[i], xrb[i]], W=[xrb[i]])
                for i in range(NT):
                    dma("sp", x_dst[i * 128:(i + 1) * 128, :], xres[:, i, :], R=[xrb[i]])
                kb.barrier()

    def sgu_phase(x_src, x_dst):
        l = 1
        with ExitStack() as t:
            wout = kb.sb(t, "swout", [128, 24, D], BF16)
            wsT = kb.sb(t, "swsT", [128, 8, 128], BF16)
            bsT = kb.sb(t, "sbsT", [128, 8], F32)
            ngb = kb.sb(t, "sngb", [128, 3072], BF16)
            gmb = kb.sb(t, "sgmb", [128, D], F32)
            gmbuf = Buf("gmb")
            cb = Buf("sconst")
            for q in range(4):
                dma("pool", wout[:, q * 6:(q + 1) * 6, :],
                    sgu_w_out[q * 768:(q + 1) * 768, :].rearrange("(k p) n -> p k n", p=128), W=[cb])
            dma("sp", bsT[:], sgu_bsT[:, :], W=[cb])
            dma("pool", ngb[:], sgu_ng[0:1, :].partition_broadcast(128), W=[cb])
            G1 = kb.sb(t, "smG1", [128, D], F32)
            SH = kb.sb(t, "smSH", [128, D], F32)
            with ExitStack() as t0:
                wring = Ring(kb, t0, "adaw", 2, [128, 8, 1024], BF16)
                bring = Ring(kb, t0, "adab", 2, [1, 1024], BF16)
                G1_, SH_, mb = make_mod(t0, l, 0, 1, nmix_g[l:l + 1, :], silc, "sm", wring, bring)
                op("pool", lambda: G.tensor_copy(G1[:], G1_[:]), R=[mb], W=[cb])
                op("pool", lambda: G.tensor_copy(SH[:], SH_[:]), R=[mb], W=[cb])
                ada_chunk(l, 2, silc, gmb[:], gmbuf, wring, bring)
                wsf = kb.sb(t0, "wsf", [128, 8, 128], F32)
                wsb = Buf("wsf")
                dma("sp", wsf[:], sgu_ws.rearrange("g i j -> i g j"), W=[wsb])
                for gi in range(8):
                    op("pe", lambda: P.transpose(bank(gi // 4, 128, (gi % 4) * 128), wsf[:, gi, :], identf[:]),
                       R=[wsb, b_const], W=[psb[gi // 4]])
                op("dve", lambda: V.tensor_copy(wsT[:].rearrange("p a b -> p (a b)"), ps[:, 0:1024]), R=[psb[0], psb[1]], W=[cb])
                kb.barrier()
            fr = make_front(t, [0])
            hTblk = kb.sb(t, "shTblk", [128, 8, 512], BF16)
            hTblkb = Buf("hTblk")
            uu = kb.sb(t, "suu", [128, 4, 3072], BF16)
            vv = kb.sb(t, "svv", [128, 4, 3072], BF16)
            uvb = [Buf("uv%d" % i) for i in range(4)]
            wblk = Ring(kb, t, "swblk", 2, [128, 8, 512], BF16)
            bblk = Ring(kb, t, "sbblk", 2, [1, 512], BF16)
            vn = Ring(kb, t, "svn", 2, [128, 3072], BF16)
            gt = Ring(kb, t, "sgt", 1, [128, 3072], BF16)
            gT = Ring(kb, t, "sgT", 1, [128, 24, 128], BF16)
            st = Ring(kb, t, "sst", 2, [128, 4], F32)
            yt = Ring(kb, t, "syt", 1, [128, 512], F32)
            xo = Ring(kb, t, "sxo", 1, [128, D], F32)
            eps = fr["eps"]
            zr = PsRing([1, 2, 3, 4])
            xin = Ring(kb, t, "sxin", 2, [128, D], F32)
            xres_r = Ring(kb, t, "sxres", 2, [128, D], F32)

            def ldA(tb, ti):
                i = tb * 4 + ti
                xt, xbuf = xin.next()
                dma("sp", xt[:], x_src[i * 128:(i + 1) * 128, :], W=[xbuf])
                return xt, xbuf

            def stAa(xa_):
                xt, xbuf = xa_
                return front_a(xt[:], xbuf, G1[:], SH[:], cb, fr)

            def stAb(ti, fa):
                hT, hTb, _, _ = front_b(fa, fr)
                op("pool", lambda: G.tensor_copy(hTblk[:, :, ti * 128:(ti + 1) * 128], hT[:]), R=[hTb], W=[hTblkb])

            def stB(tb):
                for cbk in range(12):
                    wt, wb = wblk.next()
                    bt_, bb = bblk.next()
                    dma("pool", wt[:], sgu_w_in[:, cbk * 512:(cbk + 1) * 512].rearrange("(k p) n -> p k n", p=128), W=[wb])
                    dma("pool", bt_[:], sgu_b_in[0:1, cbk * 512:(cbk + 1) * 512], W=[bb])
                    for ti in range(4):
                        k = zr.next()
                        for dk in range(8):
                            op("pe", lambda: P.matmul(bank(k), hTblk[:, dk, ti * 128:(ti + 1) * 128], wt[:, dk, :],
                                                      start=(dk == 0), stop=False), R=[hTblkb, wb], W=[psb[k]], inc=False)
                        op("pe", lambda: P.matmul(bank(k), ones1[:], bt_[:], start=False, stop=True), R=[bb, b_const], W=[psb[k]])
                        dst = uu[:, ti, cbk * 512:(cbk + 1) * 512] if cbk < 6 else vv[:, ti, (cbk - 6) * 512:(cbk - 5) * 512]
                        op("act", lambda: A.activation(out=dst, in_=bank(k), func=AF.Gelu), R=[psb[k]], W=[uvb[ti]])

            def ldR(tb, ti):
                i = tb * 4 + ti
                xr, xrb_ = xres_r.next()
                dma("sp", xr[:], x_src[i * 128:(i + 1) * 128, :], W=[xrb_])
                return xr, xrb_

            def stN(ti):
                s_, sbf = st.next()
                vnt, vnb = vn.next()
                op("act", lambda: A.activation(out=vnt[:], in_=vv[:, ti, :], func=AF.Square, accum_out=s_[:, 0:1]),
                   R=[uvb[ti]], W=[vnb, sbf])
                op("act", lambda: A.activation(out=s_[:, 1:2], in_=s_[:, 0:1], func=AF.Sqrt, scale=1.0 / 3072, bias=eps[:, 0:1]),
                   R=[sbf, b_const], W=[sbf])
                op("dve", lambda: V.reciprocal(s_[:, 2:3], s_[:, 1:2]), R=[sbf], W=[sbf])
                op("dve", lambda: V.scalar_tensor_tensor(out=vnt[:], in0=vv[:, ti, :], scalar=s_[:, 2:3], in1=ngb[:],
                                                         op0=ALU.mult, op1=ALU.mult), R=[uvb[ti], sbf, cb], W=[vnb])
                return vnt, vnb

            def stC12(ti, vn_):
                vnt, vnb = vn_
                gtt, gtb = gt.next()
                for gi in range(8):
                    k = zr.next()
                    op("pe", lambda: P.matmul(bank(k, 384), wsT[:, gi, :], vnt[:, gi * 384:(gi + 1) * 384], start=True, stop=True),
                       R=[vnb, cb], W=[psb[k]])
                    op("dve", lambda: V.scalar_tensor_tensor(out=gtt[:, gi * 384:(gi + 1) * 384], in0=bank(k, 384),
                                                             scalar=bsT[:, gi:gi + 1], in1=uu[:, ti, gi * 384:(gi + 1) * 384],
                                                             op0=ALU.add, op1=ALU.mult), R=[psb[k], cb, uvb[ti]], W=[gtb])
                gTt, gTb = gT.next()
                for q in range(3):
                    k = zr.next()
                    for c8 in range(8):
                        kc = q * 8 + c8
                        op("pe", lambda: P.transpose(bank_bf(k, 128, c8 * 128), gtt[:, kc * 128:(kc + 1) * 128], ident[:]),
                           R=[gtb, b_const], W=[psb[k]], inc=(c8 == 7))
                    op("act", lambda: A.copy(out=gTt[:, q * 8:(q + 1) * 8, :].rearrange("p a b -> p (a b)"), in_=bank_bf(k)),
                       R=[psb[k]], W=[gTb])
                return gTt, gTb

            def stC3(tb, ti, g_, xr_):
                i = tb * 4 + ti
                gTt, gTb = g_
                xr, xrb_ = xr_
                xot, xob = xo.next()
                for hf in range(2):
                    k = 5 + hf
                    for kc in range(24):
                        op("pe", lambda: P.matmul(bank(k), gTt[:, kc, :], wout[:, kc, hf * 512:(hf + 1) * 512],
                                                  start=(kc == 0), stop=(kc == 23)), R=[gTb, cb], W=[psb[k]], inc=(kc == 23))
                    ytt, ytb = yt.next()
                    op("dve", lambda: V.tensor_tensor(out=ytt[:], in0=bank(k), in1=gmb[:, hf * 512:(hf + 1) * 512], op=ALU.mult),
                       R=[psb[k], gmbuf], W=[ytb])
                    op("pool", lambda: G.tensor_tensor(out=xot[:, hf * 512:(hf + 1) * 512], in0=ytt[:],
                                                       in1=xr[:, hf * 512:(hf + 1) * 512], op=ALU.add),
                       R=[ytb, xrb_], W=[xob])
                dma("sp", x_dst[i * 128:(i + 1) * 128, :], xot[:], R=[xob])

            for ti in range(4):
                stAb(ti, stAa(ldA(0, ti)))
            for tb in range(4):
                stB(tb)
                more = tb + 1 < 4
                xr_ = ldR(tb, 0)
                vn_ = stN(0)
                if more:
                    stAb(0, stAa(ldA(tb + 1, 0)))
                for ti in range(4):
                    nxt = ti + 1 < 4
                    if nxt:
                        xr_n = ldR(tb, ti + 1)
                        if more:
                            xa_n = ldA(tb + 1, ti + 1)
                    g_ = stC12(ti, vn_)
                    if nxt:
                        vn_ = stN(ti + 1)
                        if more:
                            fa_n = stAa(xa_n)
                    stC3(tb, ti, g_, xr_)
                    if nxt:
                        if more:
                            stAb(ti + 1, fa_n)
                        xr_ = xr_n
            kb.barrier()


    def mixer0_phase(x_dst):
        l = 0
        import os
        MS = os.environ.get("MIX_STOP", "")
        b3 = lambda ap, n: ap.unsqueeze(2).to_broadcast([128, n, 64])
        h3 = lambda ap: ap.rearrange("p (h e) -> p h e", e=64)
        with ExitStack() as t:
            G1m = kb.sb(t, "G1m", [128, D], F32)
            SHm = kb.sb(t, "SHm", [128, D], F32)
            gmb = kb.sb(t, "gmb", [128, D], F32)
            mbuf = Buf("modm")
            lgb = kb.sb(t, "lgb", [128, 16], F32)
            hgb = kb.sb(t, "hgb", [128, 192], F32)
            eps = kb.sb(t, "eps0", [128, 1], F32)
            one = kb.sb(t, "one0", [128, 1], F32)
            ret_out = kb.sb(t, "ret_out", [128, NT, 512], BF16)
            retb = [Buf("ret%d" % i) for i in range(NT)]
            KcT = kb.sb(t, "KcT", [128, 4, 256], BF16)
            Vc = kb.sb(t, "Vc", [128, 2, 512], BF16)
            ctxb = Buf("ctxkv")
            tb_ = Buf("tables")
            retg = hgb[:, 0:64]
            qg = hgb[:, 64:128]
            kg = hgb[:, 128:192]
            op("dve", lambda: V.memset(eps[:], EPS), W=[tb_])
            op("dve", lambda: V.memset(one[:], 1.0), W=[tb_])
            dma("sp", hgb[:], hg[0:1, :].partition_broadcast(128), W=[tb_])
            dma("sp", lgb[:], ret_decay[0:1, :].partition_broadcast(128), W=[tb_])
            op("dve", lambda: V.tensor_scalar(out=qg, in0=qg, scalar1=0.125, scalar2=None, op0=ALU.mult), R=[tb_], W=[tb_])
            op("act", lambda: A.activation(out=lgb[:], in_=lgb[:], func=AF.Exp), R=[tb_], W=[tb_])
            op("act", lambda: A.activation(out=lgb[:], in_=lgb[:], func=AF.Ln, bias=one[:, 0:1]), R=[tb_], W=[tb_])
            op("dve", lambda: V.tensor_scalar(out=lgb[:], in0=lgb[:], scalar1=-1.0, scalar2=None, op0=ALU.mult), R=[tb_], W=[tb_])
            with ExitStack() as t0:
                wring = Ring(kb, t0, "adaw", 2, [128, 8, 1024], BF16)
                bring = Ring(kb, t0, "adab", 2, [1, 1024], BF16)
                G1_, SH_, mb_ = make_mod(t0, l, 0, 1, nmix_g[l:l + 1, :], silc, "mm", wring, bring)
                op("pool", lambda: G.tensor_copy(G1m[:], G1_[:]), R=[mb_], W=[mbuf])
                op("pool", lambda: G.tensor_copy(SHm[:], SH_[:]), R=[mb_], W=[mbuf])
                ada_chunk(l, 2, silc, gmb[:], mbuf, wring, bring)
                kb.barrier()

            def norm_heads_T(src_bank, gain, tmps, dstK, dstKb):
                sqt, sqb = tmps["sq"].next()
                s8, s8b = tmps["s8"].next()
                op("act", lambda: A.activation(out=sqt[:], in_=bank(src_bank), func=AF.Square), R=[psb[src_bank]], W=[sqb])
                op("dve", lambda: V.tensor_reduce(out=s8[:, 0:8], in_=h3(sqt[:]), op=ALU.add, axis=AX.X), R=[sqb], W=[s8b])
                op("act", lambda: A.activation(out=s8[:, 8:16], in_=s8[:, 0:8], func=AF.Sqrt, scale=1.0 / 64, bias=eps[:, 0:1]),
                   R=[s8b, tb_], W=[s8b])
                op("dve", lambda: V.reciprocal(s8[:, 16:24], s8[:, 8:16]), R=[s8b], W=[s8b])
                op("dve", lambda: V.tensor_tensor(out=h3(sqt[:]), in0=h3(bank(src_bank)), in1=b3(s8[:, 16:24], 8), op=ALU.mult),
                   R=[psb[src_bank], s8b], W=[sqb])
                knb, knbb = tmps["knb"].next()
                op("pool", lambda: G.tensor_tensor(out=h3(knb[:]), in0=h3(sqt[:]), in1=gain.unsqueeze(1).to_broadcast([128, 8, 64]),
                                                   op=ALU.mult), R=[sqb, tb_], W=[knbb])
                k = tmps["ps"].next()
                for hp in range(4):
                    op("pe", lambda: P.transpose(bank_bf(k, 128, hp * 128), knb[:, hp * 128:(hp + 1) * 128], ident[:]),
                       R=[knbb, b_const], W=[psb[k]], inc=(hp == 3))
                op("dve", lambda: V.tensor_copy(dstK, bank_bf(k, 512).rearrange("p (a b) -> p a b", b=128)), R=[psb[k]], W=[dstKb])

            def make_tmps(tt, ps_banks):
                return {"sq": Ring(kb, tt, "nsq", 2, [128, 512], F32), "s8": Ring(kb, tt, "ns8", 2, [128, 24], F32),
                        "knb": Ring(kb, tt, "nknb", 2, [128, 512], BF16), "ps": PsRing(ps_banks)}

            def proj(hT, hTb, w, wb, c0, k):
                for dk in range(8):
                    op("pe", lambda: P.matmul(bank(k), hT[:, dk, :], w[:, dk, c0:c0 + 512], start=(dk == 0), stop=(dk == 7)),
                       R=[hTb, wb], W=[psb[k]], inc=(dk == 7))

            def rope(src, srcbufs, Ct, St, tabb, t1, t1b, t2, t2b):
                s5 = lambda ap: ap.rearrange("p (h b f q) -> p h b f q", h=8, b=2, f=2, q=16)
                S4 = St.rearrange("p (b f q) -> p b f q", b=2, f=2, q=16)
                op("dve", lambda: V.tensor_tensor(out=h3(t1), in0=h3(src), in1=Ct.unsqueeze(1).to_broadcast([128, 8, 64]), op=ALU.mult),
                   R=srcbufs + [tabb], W=[t1b])
                for f in range(2):
                    op("dve", lambda: V.tensor_tensor(out=s5(t2)[:, :, :, f, :], in0=s5(src)[:, :, :, 1 - f, :],
                                                      in1=S4[:, :, f, :].unsqueeze(1).to_broadcast([128, 8, 2, 16]), op=ALU.mult),
                       R=srcbufs + [tabb], W=[t2b])

            if MS == "tables":
                return
            with ExitStack() as tR:
                cstt = kb.sb(tR, "cstt", [128, 260], F32)
                MT = kb.sb(tR, "MT", [128, 8, 128], F32)
                dec = kb.sb(tR, "dec", [128, 4, 8], F32)
                CD = kb.sb(tR, "CD", [128, 2, 4], F32)
                ei = kb.sb(tR, "ei", [128, 36], F32)
                Sinit = kb.sb(tR, "Sinit", [128, 2, 256], F32)
                sib = Buf("Sinit")
                G1c = kb.sb(tR, "G1c", [128, D], F32)
                SHc = kb.sb(tR, "SHc", [128, D], F32)
                cbuf = Buf("modc")
                wq = kb.sb(tR, "w_qkvg", [128, 8, 2048], BF16)
                wqb = Buf("wq")
                wn = kb.sb(tR, "w_nkv", [128, 8, 1024], BF16)
                wnb = Buf("wn")
                DBt = kb.sb(tR, "DBt", [128, NT, 256], BF16)
                SBst = kb.sb(tR, "SBst", [128, NT, 256], BF16)
                dbb = [Buf("db%d" % i) for i in range(NT)]
                sbb = [Buf("sb%d" % i) for i in range(NT)]
                for q in range(4):
                    dma("pool", wq[:, :, q * 512:(q + 1) * 512], ab_w_in[:, q * 512:(q + 1) * 512].rearrange("(k p) n -> p k n", p=128), W=[wqb])
                for q in range(2):
                    dma("pool", wn[:, :, q * 512:(q + 1) * 512],
                        ab_w_in[:, 2560 + q * 512:2560 + (q + 1) * 512].rearrange("(k p) n -> p k n", p=128), W=[wnb])
                dma("sp", cstt[:], cst[:, :], W=[tb_])
                dma("sp", ei[:], exp_init[:, :], W=[tb_])
                DFt = cstt[:, 0:128]
                DBe = cstt[:, 128:256]
                with ExitStack() as t0:
                    wring = Ring(kb, t0, "adaw", 2, [128, 8, 1024], BF16)
                    bring = Ring(kb, t0, "adab", 2, [1, 1024], BF16)
                    G1_, SH_, mb_ = make_mod(t0, l, 0, 1, nmix_g[l:l + 1, :], silcc, "mc", wring, bring)
                    op("pool", lambda: G.tensor_copy(G1c[:], G1_[:]), R=[mb_], W=[cbuf])
                    op("pool", lambda: G.tensor_copy(SHc[:], SH_[:]), R=[mb_], W=[cbuf])
                    tmpM = kb.sb(t0, "tmpM", [128, 128], F32)
                    tmb = Buf("tmpM")
                    for h in range(8):
                        op("act", lambda: A.activation(out=MT[:, h, :], in_=DFt, func=AF.Exp, scale=lgb[:, h:h + 1]), R=[tb_], W=[tb_])
                        op("act", lambda: A.activation(out=tmpM[:], in_=DBe, func=AF.Exp, scale=lgb[:, 8 + h:9 + h]), R=[tb_], W=[tmb])
                        op("dve", lambda: V.tensor_tensor(out=MT[:, h, :], in0=MT[:, h, :], in1=tmpM[:], op=ALU.add), R=[tb_, tmb], W=[tb_])
                    for j, (c0, pc) in enumerate(((0, 256), (8, 257), (0, 258), (8, 259))):
                        op("act", lambda: A.activation(out=dec[:, j, :], in_=lgb[:, c0:c0 + 8], func=AF.Exp, scale=cstt[:, pc:pc + 1]),
                           R=[tb_], W=[tb_])
                    lg4 = lgb[:].rearrange("p (d q h) -> p d q h", d=2, q=4, h=2)
                    for hh in range(2):
                        ps_ = slice(hh * 64, (hh + 1) * 64)
                        op("act", lambda: A.activation(out=CD[ps_, :, :], in_=lg4[ps_, :, :, hh], func=AF.Exp, scale=128.0), R=[tb_], W=[tb_])
                    kb.barrier()
                if MS == "tables2":
                    dma("sp", x_dst[0:128, 0:1024], MT[:].rearrange("p a b -> p (a b)"), R=[tb_])
                    dma("sp", x_dst[128:256, 0:32], dec[:].rearrange("p a b -> p (a b)"), R=[tb_])
                    dma("sp", x_dst[128:256, 32:40], CD[:].rearrange("p a b -> p (a b)"), R=[tb_])
                    dma("sp", x_dst[128:256, 64:80], lgb[:], R=[tb_])
                    kb.barrier()
                    return
                fr = make_front(tR, [0])
                xin = Ring(kb, tR, "rxin", 2, [128, D], F32)
                ropt = Ring(kb, tR, "ropt", 2, [128, 256], F32)
                t1r = Ring(kb, tR, "t1r", 1, [128, 1024], F32)
                t2r = Ring(kb, tR, "t2r", 1, [128, 1024], F32)
                krr = Ring(kb, tR, "krr", 1, [128, 512], F32)
                wtr = Ring(kb, tR, "wtr", 2, [128, 2, 8], F32)
                kfr = Ring(kb, tR, "kfr", 2, [128, 2, 512], BF16)
                vbr = Ring(kb, tR, "vbr", 2, [128, 512], BF16)
                tmps = make_tmps(tR, [5])

                NO = 18
                def headO(i):
                    isctx = i >= 16
                    xt, xbuf = xin.next()
                    src = x_ctx[(i - 16) * 128:(i - 15) * 128, :] if isctx else x_oth[i * 128:(i + 1) * 128, :]
                    dma("sp", xt[:], src, W=[xbuf])
                    rt, rtb = ropt.next()
                    dma("sp", rt[:, 0:128], rope_oth[i * 128:(i + 1) * 128, :], W=[rtb])
                    fa = front_a(xt[:], xbuf, (G1c if isctx else G1m)[:], (SHc if isctx else SHm)[:], cbuf if isctx else mbuf, fr)
                    return xt, xbuf, rt, rtb, fa

                def bodyO(i, hd):
                    isctx = i >= 16
                    xt, xbuf, rt, rtb, fa = hd
                    hT, hTb, _, _ = front_b(fa, fr)
                    proj(hT, hTb, wq, wqb, 512, 1)
                    proj(hT, hTb, wq, wqb, 1024, 2)
                    t1, t1b = t1r.next()
                    t2, t2b = t2r.next()
                    rope(bank(1), [psb[1]], rt[:, 0:64], rt[:, 64:128], rtb, t1[:, 0:512], t1b, t2[:, 0:512], t2b)
                    kr, krb = krr.next()
                    op("pool", lambda: G.tensor_tensor(out=kr[:], in0=t1[:, 0:512], in1=t2[:, 0:512], op=ALU.add), R=[t1b, t2b], W=[krb])
                    wt, wtb = wtr.next()
                    for d_ in range(2):
                        op("act", lambda: A.activation(out=wt[:, d_, :], in_=lgb[:, d_ * 8:(d_ + 1) * 8], func=AF.Exp,
                                                       scale=ei[:, d_ * 18 + i:d_ * 18 + i + 1]), R=[tb_], W=[wtb])
                    kf, kfb = kfr.next()
                    for d_ in range(2):
                        op("pool", lambda: G.tensor_tensor(out=h3(kf[:, d_, :]), in0=h3(kr[:]), in1=b3(wt[:, d_, :], 8), op=ALU.mult),
                           R=[krb, wtb], W=[kfb])
                    vb, vbb = vbr.next()
                    op("act", lambda: A.copy(out=vb[:], in_=bank(2)), R=[psb[2]], W=[vbb])
                    for d_ in range(2):
                        kk = 6 + d_
                        for h in range(8):
                            hp, hh = h // 2, h % 2
                            op("pe", lambda: P.matmul(ps[hh * 64:(hh + 1) * 64, kk * 512 + hp * 64:kk * 512 + (hp + 1) * 64],
                                                      kf[:, d_, h * 64:(h + 1) * 64], vb[:, h * 64:(h + 1) * 64],
                                                      start=(i == 0 and h < 2), stop=(i == NO - 1), skip_group_check=True),
                               R=[kfb, vbb], W=[psb[kk]], inc=(h == 7))
                    if isctx:
                        proj(hT, hTb, wn, wnb, 0, 3)
                        proj(hT, hTb, wn, wnb, 512, 4)
                        norm_heads_T(3, kg, tmps, KcT[:, :, (i - 16) * 128:(i - 15) * 128], ctxb)
                        op("act", lambda: A.copy(out=Vc[:, i - 16, :], in_=bank(4)), R=[psb[4]], W=[ctxb])
                pipelined(NO, headO, bodyO)
                for d_ in range(2):
                    op("act", lambda: A.copy(out=Sinit[:, d_, :], in_=bank(6 + d_, 256)), R=[psb[6 + d_]], W=[sib])
                if MS == "sweepO":
                    dma("sp", x_dst[0:128, 0:512], Sinit[:].rearrange("p a b -> p (a b)"), R=[sib])
                    dma("sp", x_dst[128:256, 0:512].bitcast(BF16), KcT[:].rearrange("p a b -> p (a b)"), R=[ctxb])
                    dma("sp", x_dst[256:384, 0:512].bitcast(BF16), Vc[:].rearrange("p a b -> p (a b)"), R=[ctxb])
                    kb.barrier()
                    return

                def headL(c):
                    xt, xbuf = xin.next()
                    dma("sp", xt[:], x_loc[c * 128:(c + 1) * 128, :], W=[xbuf])
                    rt, rtb = ropt.next()
                    dma("sp", rt[:], rope_loc[c * 128:(c + 1) * 128, :], W=[rtb])
                    fa = front_a(xt[:], xbuf, G1m[:], SHm[:], mbuf, fr)
                    return xt, xbuf, rt, rtb, fa

                def bodyE1(c, hd):
                    xt, xbuf, rt, rtb, fa = hd
                    hT, hTb, _, _ = front_b(fa, fr)
                    proj(hT, hTb, wq, wqb, 512, 1)
                    proj(hT, hTb, wq, wqb, 1024, 2)
                    t1, t1b = t1r.next()
                    t2, t2b = t2r.next()
                    rope(bank(1), [psb[1]], rt[:, 64:128], rt[:, 192:256], rtb, t1[:, 0:512], t1b, t2[:, 0:512], t2b)
                    kr, krb = krr.next()
                    op("pool", lambda: G.tensor_tensor(out=kr[:], in0=t1[:, 0:512], in1=t2[:, 0:512], op=ALU.add), R=[t1b, t2b], W=[krb])
                    kf, kfb = kfr.next()
                    op("pool", lambda: G.tensor_tensor(out=h3(kf[:, 0, :]), in0=h3(kr[:]), in1=b3(dec[:, 3, :], 8), op=ALU.mult),
                       R=[krb, tb_], W=[kfb])
                    vb, vbb = vbr.next()
                    op("act", lambda: A.copy(out=vb[:], in_=bank(2)), R=[psb[2]], W=[vbb])
                    kk = 3 + (c % 2)
                    for h in range(8):
                        hp, hh = h // 2, h % 2
                        op("pe", lambda: P.matmul(ps[hh * 64:(hh + 1) * 64, kk * 512 + hp * 64:kk * 512 + (hp + 1) * 64],
                                                  kf[:, 0, h * 64:(h + 1) * 64], vb[:, h * 64:(h + 1) * 64], start=True, stop=True),
                           R=[kfb, vbb], W=[psb[kk]], inc=(h == 7))
                    op("act", lambda: A.copy(out=DBt[:, c, :], in_=bank(kk, 256)), R=[psb[kk]], W=[dbb[c]])
                pipelined(NT, headL, bodyE1)
                srun = Ring(kb, tR, "srun", 2, [128, 256], F32)
                stmp = Ring(kb, tR, "stmp", 2, [128, 256], F32)
                q4 = lambda ap: ap.rearrange("p (q e) -> p q e", e=64)
                cdb = lambda d_: CD[:, d_, :].unsqueeze(2).to_broadcast([128, 4, 64])
                cur, curb = srun.next()
                op("dve", lambda: V.tensor_copy(cur[:], Sinit[:, 1, :]), R=[sib], W=[curb])
                for c in range(NT - 1, -1, -1):
                    op("act", lambda: A.copy(out=SBst[:, c, :], in_=cur[:]), R=[curb], W=[sbb[c]])
                    if c == 0:
                        break
                    tm, tmb_ = stmp.next()
                    op("pool", lambda: G.tensor_tensor(out=q4(tm[:]), in0=q4(cur[:]), in1=cdb(1), op=ALU.mult), R=[curb, tb_], W=[tmb_])
                    nxt, nxtb = srun.next()
                    op("dve", lambda: V.tensor_tensor(out=nxt[:], in0=tm[:], in1=DBt[:, c, :], op=ALU.add), R=[tmb_, dbb[c]], W=[nxtb])
                    cur, curb = nxt, nxtb
                if MS == "sweepE1":
                    for c in range(NT):
                        dma("sp", x_dst[c * 128:(c + 1) * 128, 0:128].bitcast(BF16), SBst[:, c, :], R=[sbb[c]])
                    kb.barrier()
                    return

                qkr_r = Ring(kb, tR, "qkr", 2, [128, 1024], BF16)
                qkT_r = Ring(kb, tR, "qkT", 2, [128, 8, 128], BF16)
                qz_r = Ring(kb, tR, "qz", 2, [128, 2, 4, 128], BF16)
                for qi_ in range(2):
                    op("pool", lambda: G.memset(qz_r.t[qi_][:], 0.0), W=[qz_r.b[qi_]])
                gs_r = Ring(kb, tR, "gsr", 2, [128, 512], BF16)
                gs2_r = Ring(kb, tR, "gs2r", 2, [128, 512], BF16)
                Pm_r = Ring(kb, tR, "Pmr", 1, [128, 8, 128], BF16)
                o_r = Ring(kb, tR, "or", 2, [128, 512], F32)
                s8r = Ring(kb, tR, "rs8", 2, [128, 24], F32)
                SFr = Ring(kb, tR, "SFr", 2, [128, 256], F32)
                SFbr = Ring(kb, tR, "SFbr", 2, [128, 256], BF16)
                SF, SFb_ = SFr.next()
                op("dve", lambda: V.tensor_copy(SF[:], Sinit[:, 0, :]), R=[sib], W=[SFb_])
                SFh, SFhb = SFbr.next()
                op("act", lambda: A.copy(out=SFh[:], in_=SF[:]), R=[SFb_], W=[SFhb])
                RS_ = {"SF": SF, "SFb": SFb_, "SFh": SFh, "SFhb": SFhb}

                def bodyR(c, hd):
                    xt, xbuf, rt, rtb, fa = hd
                    SF, SFb_, SFh, SFhb = RS_["SF"], RS_["SFb"], RS_["SFh"], RS_["SFhb"]
                    hT, hTb, _, _ = front_b(fa, fr)
                    for j in range(4):
                        proj(hT, hTb, wq, wqb, j * 512, 1 + j)
                    t1, t1b = t1r.next()
                    t2, t2b = t2r.next()
                    for j in range(2):
                        rope(bank(1 + j), [psb[1 + j]], rt[:, j * 64:(j + 1) * 64], rt[:, 128 + j * 64:128 + (j + 1) * 64], rtb,
                             t1[:, j * 512:(j + 1) * 512], t1b, t2[:, j * 512:(j + 1) * 512], t2b)
                    qkr, qkrb = qkr_r.next()
                    op("pool", lambda: G.tensor_tensor(out=qkr[:], in0=t1[:], in1=t2[:], op=ALU.add), R=[t1b, t2b], W=[qkrb])
                    vb, vbb = vbr.next()
                    op("act", lambda: A.copy(out=vb[:], in_=bank(3)), R=[psb[3]], W=[vbb])
                    gs, gsb = gs_r.next()
                    op("act", lambda: A.activation(out=gs[:], in_=bank(4), func=AF.Silu), R=[psb[4]], W=[gsb])
                    gs2, gs2b = gs2_r.next()
                    op("pool", lambda: G.tensor_tensor(out=h3(gs2[:]), in0=h3(gs[:]), in1=retg.unsqueeze(1).to_broadcast([128, 8, 64]),
                                                       op=ALU.mult), R=[gsb, tb_], W=[gs2b])
                    kf, kfb = kfr.next()
                    op("pool", lambda: G.tensor_tensor(out=h3(kf[:, 0, :]), in0=h3(qkr[:, 512:1024]), in1=b3(dec[:, 2, :], 8), op=ALU.mult),
                       R=[qkrb, tb_], W=[kfb])
                    qkT, qkTb = qkT_r.next()
                    for j in range(8):
                        op("pe", lambda: P.transpose(bank_bf(0, 128, j * 128), qkr[:, j * 128:(j + 1) * 128], ident[:]),
                           R=[qkrb, b_const], W=[psb[0]], inc=(j == 7))
                    op("dve", lambda: V.tensor_copy(qkT[:].rearrange("p a b -> p (a b)"), bank_bf(0)), R=[psb[0]], W=[qkTb])
                    qz, qzb = qz_r.next()
                    for hh in range(2):
                        pr = slice(hh * 64, (hh + 1) * 64)
                        op("act", lambda: A.copy(out=qz[pr, hh, :, :], in_=qkT[pr, 0:4, :]), R=[qkTb], W=[qzb])
                    for h in range(8):
                        hp, hh = h // 2, h % 2
                        op("pe", lambda: P.matmul(bank(5 + h // 4, 128, (h % 4) * 128), qkT[:, 4 + hp, :], qz[:, hh, hp, :], start=True, stop=True),
                           R=[qkTb, qzb], W=[psb[5 + h // 4]], inc=(h % 4 == 3))
                    Pm, Pmb = Pm_r.next()
                    for b_ in range(2):
                        op("dve", lambda: V.tensor_tensor(out=Pm[:, 4 * b_:4 * b_ + 4, :], in0=bank(5 + b_).rearrange("p (a b) -> p a b", b=128),
                                                          in1=MT[:, 4 * b_:4 * b_ + 4, :], op=ALU.mult), R=[psb[5 + b_], tb_], W=[Pmb])
                    for h in range(8):
                        op("pe", lambda: P.matmul(bank(7, 64, h * 64), Pm[:, h, :], vb[:, h * 64:(h + 1) * 64], start=True, stop=True),
                           R=[Pmb, vbb], W=[psb[7]], inc=(h == 7))
                    for h in range(8):
                        hp, hh = h // 2, h % 2
                        op("pe", lambda: P.matmul(bank(3, 64, h * 64), qz[:, hh, hp, :], SFh[:, hp * 64:(hp + 1) * 64], start=True, stop=True),
                           R=[qzb, SFhb], W=[psb[3]], inc=(h == 7))
                    for h in range(8):
                        hp, hh = h // 2, h % 2
                        op("pe", lambda: P.matmul(bank(4, 64, h * 64), qz[:, hh, hp, :], SBst[:, c, hp * 64:(hp + 1) * 64], start=True, stop=True),
                           R=[qzb, sbb[c]], W=[psb[4]], inc=(h == 7))
                    o1, o1b = o_r.next()
                    o2, o2b = o_r.next()
                    op("dve", lambda: V.tensor_tensor(out=h3(o1[:]), in0=h3(bank(3)), in1=b3(dec[:, 0, :], 8), op=ALU.mult), R=[psb[3], tb_], W=[o1b])
                    op("dve", lambda: V.tensor_tensor(out=h3(o2[:]), in0=h3(bank(4)), in1=b3(dec[:, 1, :], 8), op=ALU.mult), R=[psb[4], tb_], W=[o2b])
                    op("dve", lambda: V.tensor_tensor(out=o1[:], in0=bank(7), in1=o1[:], op=ALU.add), R=[psb[7], o1b], W=[o1b])
                    op("pool", lambda: G.tensor_tensor(out=o1[:], in0=o1[:], in1=o2[:], op=ALU.add), R=[o1b, o2b], W=[o1b])
                    op("pool", lambda: G.tensor_tensor(out=o2[:], in0=o1[:], in1=o1[:], op=ALU.mult), R=[o1b], W=[o2b])
                    s8, s8b = s8r.next()
                    op("dve", lambda: V.tensor_reduce(out=s8[:, 0:8], in_=h3(o2[:]), op=ALU.add, axis=AX.X), R=[o2b], W=[s8b])
                    op("act", lambda: A.activation(out=s8[:, 8:16], in_=s8[:, 0:8], func=AF.Sqrt, scale=1.0 / 64, bias=eps[:, 0:1]),
                       R=[s8b, tb_], W=[s8b])
                    op("dve", lambda: V.reciprocal(s8[:, 16:24], s8[:, 8:16]), R=[s8b], W=[s8b])
                    op("pool", lambda: G.tensor_tensor(out=h3(o2[:]), in0=h3(o1[:]), in1=b3(s8[:, 16:24], 8), op=ALU.mult), R=[o1b, s8b], W=[o2b])
                    op("pool", lambda: G.tensor_tensor(out=ret_out[:, c, :], in0=o2[:], in1=gs2[:], op=ALU.mult), R=[o2b, gs2b], W=[retb[c]])
                    if c < NT - 1:
                        for h in range(8):
                            hp, hh = h // 2, h % 2
                            op("pe", lambda: P.matmul(ps[hh * 64:(hh + 1) * 64, 1 * 512 + hp * 64:1 * 512 + (hp + 1) * 64],
                                                      kf[:, 0, h * 64:(h + 1) * 64], vb[:, h * 64:(h + 1) * 64], start=True, stop=True),
                               R=[kfb, vbb], W=[psb[1]], inc=(h == 7))
                        tm, tmb_ = stmp.next()
                        op("pool", lambda: G.tensor_tensor(out=q4(tm[:]), in0=q4(SF[:]), in1=cdb(0), op=ALU.mult), R=[SFb_, tb_], W=[tmb_])
                        SF, SFb_ = SFr.next()
                        op("dve", lambda: V.tensor_tensor(out=SF[:], in0=bank(1, 256), in1=tm[:], op=ALU.add), R=[psb[1], tmb_], W=[SFb_])
                        SFh, SFhb = SFbr.next()
                        op("act", lambda: A.copy(out=SFh[:], in_=SF[:]), R=[SFb_], W=[SFhb])
                        RS_.update({"SF": SF, "SFb": SFb_, "SFh": SFh, "SFhb": SFhb})
                pipelined(NT, headL, bodyR)
                kb.barrier()
            if MS == "sweepR":
                for c in range(NT):
                    dma("sp", x_dst[c * 128:(c + 1) * 128, 0:256].bitcast(BF16), ret_out[:, c, :], R=[retb[c]])
                kb.barrier()
                return

            with ExitStack() as tN:
                wn3 = kb.sb(tN, "w_n3", [128, 8, 1536], BF16)
                wn3b = Buf("wn3")
                wo = kb.sb(tN, "w_o", [128, 8, D], BF16)
                wob = Buf("wo")
                KT = kb.sb(tN, "KT", [128, 4, 20 * 128], BF16)
                VE = kb.sb(tN, "VE", [128, 20, 512], BF16)
                kvb = [Buf("kv%d" % i) for i in range(20)]
                biasT = kb.sb(tN, "biasT", [128, 8, 896], BF16)
                rowm = kb.sb(tN, "rowm", [2, 192], BF16)
                A2 = kb.sb(tN, "A2", [2, 128], BF16)
                nb_ = Buf("naconst")
                for q in range(3):
                    dma("pool", wn3[:, :, q * 512:(q + 1) * 512],
                        ab_w_in[:, 2048 + q * 512:2048 + (q + 1) * 512].rearrange("(k p) n -> p k n", p=128), W=[wn3b])
                for q in range(2):
                    dma("pool", wo[:, :, q * 512:(q + 1) * 512], ab_w_out[:, q * 512:(q + 1) * 512].rearrange("(k p) n -> p k n", p=128), W=[wob])
                for h in range(8):
                    dma("pool", biasT[:, h, :], bias_tab[h, :, :], W=[nb_])
                dma("pool", rowm[:], rowmask[:, :], W=[nb_])
                dma("pool", A2[:], a2_d[:, :], W=[nb_])
                fr = make_front(tN, [0])
                xin = Ring(kb, tN, "nxin", 2, [128, D], F32)
                tmps = make_tmps(tN, [3])
                def headE2(e_):
                    xt, xbuf = xin.next()
                    if e_ < 2:
                        src = x_halo[e_ * 128:(e_ + 1) * 128, :]
                    elif e_ < 18:
                        src = x_loc[(e_ - 2) * 128:(e_ - 1) * 128, :]
                    else:
                        src = x_halo[256 + (e_ - 18) * 128:256 + (e_ - 17) * 128, :]
                    dma("sp", xt[:], src, W=[xbuf])
                    return xt, xbuf, front_a(xt[:], xbuf, G1m[:], SHm[:], mbuf, fr)

                def bodyE2(e_, hd):
                    xt, xbuf, fa = hd
                    hT, hTb, _, _ = front_b(fa, fr)
                    proj(hT, hTb, wn3, wn3b, 512, 1)
                    proj(hT, hTb, wn3, wn3b, 1024, 2)
                    norm_heads_T(1, kg, tmps, KT[:, :, e_ * 128:(e_ + 1) * 128], kvb[e_])
                    op("act", lambda: A.copy(out=VE[:, e_, :], in_=bank(2)), R=[psb[2]], W=[kvb[e_]])
                pipelined(20, headE2, bodyE2)
                if MS == "sweepE2":
                    for e_ in range(16):
                        dma("sp", x_dst[e_ * 128:(e_ + 1) * 128, 0:256].bitcast(BF16), KT[:, :, e_ * 128:(e_ + 1) * 128], R=[kvb[e_]])
                        dma("sp", x_dst[e_ * 128:(e_ + 1) * 128, 256:512].bitcast(BF16), VE[:, e_, :], R=[kvb[e_]])
                    kb.barrier()
                    return
                qnT_r = Ring(kb, tN, "qnT", 2, [128, 4, 128], BF16)
                qnz_r = Ring(kb, tN, "qnz", 2, [128, 2, 4, 128], BF16)
                for qi_ in range(2):
                    op("pool", lambda: G.memset(qnz_r.t[qi_][:], 0.0), W=[qnz_r.b[qi_]])
                Pe_r = Ring(kb, tN, "Pe", 2, [128, 1024], BF16)
                PT_r = Ring(kb, tN, "PT", 2, [128, 8, 128], BF16)
                sm_r = Ring(kb, tN, "sm", 2, [128, 24], F32)
                mix_r = Ring(kb, tN, "mix", 2, [128, D], BF16)
                mixT_r = Ring(kb, tN, "mixT", 2, [128, 8, 128], BF16)
                yt_r = Ring(kb, tN, "nyt", 2, [128, 512], F32)
                xo_r = Ring(kb, tN, "nxo", 2, [128, D], F32)
                ab_ring = PsRing([2, 4])
                def headN(t_):
                    xt, xbuf = xin.next()
                    dma("sp", xt[:], x_loc[t_ * 128:(t_ + 1) * 128, :], W=[xbuf])
                    return xt, xbuf, front_a(xt[:], xbuf, G1m[:], SHm[:], mbuf, fr)

                def geomN(t_):
                    lo, hi = t_ - 2, t_ + 2
                    if t_ < 2:
                        hi = 3
                    if t_ > 13:
                        lo = 12
                    n = hi - lo + 1
                    return lo, hi, n, (n - 4) * 128

                def stA(t_, hd):
                    xt, xbuf, fa = hd
                    hT, hTb, _, _ = front_b(fa, fr)
                    proj(hT, hTb, wn3, wn3b, 0, 1)
                    qnT, qnTb = qnT_r.next()
                    tmps["ps"] = PsRing([6])
                    norm_heads_T(1, qg, tmps, qnT[:], qnTb)
                    qnz, qnzb = qnz_r.next()
                    for hh in range(2):
                        pr = slice(hh * 64, (hh + 1) * 64)
                        op("pool", lambda: G.tensor_copy(qnz[pr, hh, :, :], qnT[pr, :, :]), R=[qnTb], W=[qnzb])
                    sm, smb = sm_r.next()
                    return xt, xbuf, qnz, qnzb, sm, smb

                def stS(t_, cx, h):
                    xt, xbuf, qnz, qnzb, sm, smb = cx
                    lo, hi, n, nbk = geomN(t_)
                    kv_need = [kvb[j + 2] for j in range(lo, hi + 1)]
                    hp, hh = h // 2, h % 2
                    ka = ab_ring.next()
                    kbk = ka + 1
                    e0 = (lo + 2) * 128
                    d0 = (lo - t_ + 3) * 128
                    op("pe", lambda: P.matmul(bank(ka), qnz[:, hh, hp, :], KT[:, hp, e0:e0 + 512], start=True, stop=False),
                       R=[qnzb] + kv_need, W=[psb[ka]], inc=False)
                    op("pe", lambda: P.matmul(bank(ka), ident[:], biasT[:, h, d0:d0 + 512], start=False, stop=False),
                       R=[nb_, b_const], W=[psb[ka]], inc=False)
                    op("pe", lambda: P.matmul(bank(ka), A2[:], rowm[:, t_ * 12:t_ * 12 + 8].unsqueeze(2).to_broadcast([2, 8, 64]),
                                              start=False, stop=True), R=[nb_], W=[psb[ka]])
                    op("pe", lambda: P.matmul(bank(kbk, nbk), qnz[:, hh, hp, :], KT[:, hp, e0 + 512:e0 + 512 + nbk], start=True, stop=False),
                       R=[qnzb] + kv_need, W=[psb[kbk]], inc=False)
                    op("pe", lambda: P.matmul(bank(kbk, nbk), ident[:], biasT[:, h, d0 + 512:d0 + 512 + nbk], start=False, stop=False),
                       R=[nb_, b_const], W=[psb[kbk]], inc=False)
                    op("pe", lambda: P.matmul(bank(kbk, nbk), A2[:],
                                              rowm[:, t_ * 12 + 8:t_ * 12 + 8 + (n - 4) * 2].unsqueeze(2).to_broadcast([2, (n - 4) * 2, 64]),
                                              start=False, stop=True), R=[nb_], W=[psb[kbk]], inc=False)
                    op("pe", lambda: P.matmul(bank(kbk, 256, nbk), qnz[:, hh, hp, :], KcT[:, hp, :], start=True, stop=True),
                       R=[qnzb, ctxb], W=[psb[kbk]])
                    return ka, kbk

                def stETV(t_, cx, h, ka, kbk):
                    xt, xbuf, qnz, qnzb, sm, smb = cx
                    lo, hi, n, nbk = geomN(t_)
                    kv_need = [kvb[j + 2] for j in range(lo, hi + 1)]
                    Pe, Peb = Pe_r.next()
                    op("act", lambda: A.activation(out=Pe[:, 0:512], in_=bank(ka), func=AF.Exp, accum_out=sm[:, 2 * h:2 * h + 1]),
                       R=[psb[ka]], W=[Peb, smb])
                    op("act", lambda: A.activation(out=Pe[:, 512:768 + nbk], in_=bank(kbk, nbk + 256), func=AF.Exp,
                                                   accum_out=sm[:, 2 * h + 1:2 * h + 2]), R=[psb[kbk]], W=[Peb, smb])
                    nblk = n + 2
                    for bk in range(nblk):
                        op("pe", lambda: P.transpose(bank_bf(6, 128, bk * 128), Pe[:, bk * 128:(bk + 1) * 128], ident[:]),
                           R=[Peb, b_const], W=[psb[6]], inc=(bk == nblk - 1))
                    PT, PTb = PT_r.next()
                    op("dve", lambda: V.tensor_copy(PT[:, 0:nblk, :].rearrange("p a b -> p (a b)"), bank_bf(6, nblk * 128)), R=[psb[6]], W=[PTb])
                    for bk in range(nblk):
                        if bk < n:
                            rhs = VE[:, lo + 2 + bk, h * 64:(h + 1) * 64]
                        else:
                            rhs = Vc[:, bk - n, h * 64:(h + 1) * 64]
                        op("pe", lambda: P.matmul(bank(7, 64, h * 64), PT[:, bk, :], rhs, start=(bk == 0), stop=(bk == nblk - 1)),
                           R=[PTb, ctxb] + kv_need, W=[psb[7]], inc=(bk == nblk - 1))

                def stF(t_, cx):
                    xt, xbuf, qnz, qnzb, sm, smb = cx
                    op("dve", lambda: V.tensor_reduce(out=sm[:, 16:24], in_=sm[:, 0:16].rearrange("p (h two) -> p h two", two=2),
                                                      op=ALU.add, axis=AX.X), R=[smb], W=[smb])
                    op("dve", lambda: V.reciprocal(sm[:, 16:24], sm[:, 16:24]), R=[smb], W=[smb])
                    mix, mixb = mix_r.next()
                    op("dve", lambda: V.tensor_tensor(out=h3(mix[:, 512:1024]), in0=h3(bank(7)), in1=b3(sm[:, 16:24], 8), op=ALU.mult),
                       R=[psb[7], smb], W=[mixb])
                    op("pool", lambda: G.tensor_copy(mix[:, 0:512], ret_out[:, t_, :]), R=[retb[t_]], W=[mixb])
                    mixT, mixTb = mixT_r.next()
                    for dk in range(8):
                        op("pe", lambda: P.transpose(bank_bf(0, 128, dk * 128), mix[:, dk * 128:(dk + 1) * 128], ident[:]),
                           R=[mixb, b_const], W=[psb[0]], inc=(dk == 7))
                    op("act", lambda: A.copy(out=mixT[:].rearrange("p a b -> p (a b)"), in_=bank_bf(0)), R=[psb[0]], W=[mixTb])
                    xo, xob = xo_r.next()
                    for hf in range(2):
                        k = 1 if hf == 0 else 6
                        for dk in range(8):
                            op("pe", lambda: P.matmul(bank(k), mixT[:, dk, :], wo[:, dk, hf * 512:(hf + 1) * 512], start=(dk == 0), stop=(dk == 7)),
                               R=[mixTb, wob], W=[psb[k]], inc=(dk == 7))
                        yt, ytb = yt_r.next()
                        op("dve", lambda: V.tensor_tensor(out=yt[:], in0=bank(k), in1=gmb[:, hf * 512:(hf + 1) * 512], op=ALU.mult),
                           R=[psb[k], mbuf], W=[ytb])
                        op("pool", lambda: G.tensor_tensor(out=xo[:, hf * 512:(hf + 1) * 512], in0=yt[:], in1=xt[:, hf * 512:(hf + 1) * 512],
                                                           op=ALU.add), R=[ytb, xbuf], W=[xob])
                    dma("sp", x_dst[t_ * 128:(t_ + 1) * 128, :], xo[:], R=[xob])
                hdsN = {0: headN(0)}
                cxN = {0: stA(0, hdsN[0])}
                pendN = stS(0, cxN[0], 0)
                for t_ in range(NT):
                    if t_ + 1 < NT:
                        hdsN[t_ + 1] = headN(t_ + 1)
                    cx = cxN.pop(t_)
                    pend = pendN
                    for h in range(8):
                        cur = pend
                        if h < 7:
                            pend = stS(t_, cx, h + 1)
                        if h == 3 and t_ + 1 < NT:
                            cxN[t_ + 1] = stA(t_ + 1, hdsN.pop(t_ + 1))
                        stETV(t_, cx, h, *cur)
                    if t_ + 1 < NT:
                        pendN = stS(t_ + 1, cxN[t_ + 1], 0)
                    stF(t_, cx)
                kb.barrier()

    if mode == 'full':
        mixer0_phase(xa)
        moe_phase(0, xa, xb)
        sgu_phase(xb, xc)
        moe_phase(1, xc, y_out)
    elif mode == 'mix0':
        mixer0_phase(y_out)
    elif mode == 'pro':
        with ExitStack() as t:
            wring = Ring(kb, t, "adaw", 2, [128, 8, 1024], BF16)
            bring = Ring(kb, t, "adab", 2, [1, 1024], BF16)
            o1 = kb.sb(t, "o1", [128, D], F32)
            ob = Buf("o1")
            ada_chunk(0, 2, silc, o1[:], ob, wring, bring)
            dma("sp", y_out[0:128, :], o1[:], R=[ob])
            G1, SH, mb = make_mod(t, 1, 3, 4, nffn_g[1:2, :], silcc, "mf", wring, bring)
            dma("sp", y_out[128:256, :], G1[:], R=[mb])
            dma("sp", y_out[256:384, :], SH[:], R=[mb])
            kb.barrier()
    elif mode == 'moe0':
        moe_phase(0, dbg_x, y_out)
    elif mode == 'sgu':
        sgu_phase(dbg_x, y_out)
    elif mode == 'moe1':
        moe_phase(1, dbg_x, y_out)
    kb.barrier()
    return kb


def _bf(a):
    return np.ascontiguousarray(a).astype(np.float32)


def _rope_tables(pos, scale_k):
    inv = (10000.0 ** (-np.arange(16, dtype=np.float32) / 16.0)).astype(np.float32)
    rows = (pos // 64).astype(np.float32)
    cols = (pos % 64).astype(np.float32)
    ar = rows[:, None] * inv[None, :]
    ac = cols[:, None] * inv[None, :]
    cr, sr, cc_, sc = np.cos(ar), np.sin(ar), np.cos(ac), np.sin(ac)
    C = np.concatenate([cr, cr, cc_, cc_], axis=1).astype(np.float32)
    S = np.concatenate([-sr, sr, -sc, sc], axis=1).astype(np.float32)
    return C, S


def prep_inputs(inputs):
    f = lambda k: np.asarray(inputs[k], dtype=np.float32)
    x, c, ctx, c_ctx = f('x'), f('c'), f('ctx'), f('c_ctx')
    w_in = f('ab_w_in')[0]
    blk = lambda i: w_in[:, i * 512:(i + 1) * 512]
    w_in_p = np.ascontiguousarray(np.concatenate([blk(4), blk(0), blk(1), blk(5), blk(6), blk(2), blk(3)], axis=1))
    rpb = f('na_rpb')[0]
    tab = np.full((8, 2, 64, 7, 2, 64), NEG, dtype=np.float32)
    w = np.arange(64)
    c0 = np.clip(w - 8, 0, 48)
    for a in range(2):
        for di in range(7):
            for b in range(2):
                rel = 2 * di + b - a + 1
                if rel < 0 or rel > 14:
                    continue
                for wi in range(64):
                    ccs = np.arange(c0[wi], c0[wi] + 16)
                    tab[:, a, wi, di, b, ccs] = rpb[:, rel, ccs - wi + 15]
    tab = tab.reshape(8, 128, 896)
    shared = {
        'ada_w': f('ada_w'), 'ada_b': f('ada_b'), 'norm_mix_g': f('norm_mix_g'), 'norm_ffn_g': f('norm_ffn_g'),
        'router_w': f('router_w'), 'router_bias': f('router_bias').reshape(1, 16),
        'moe_w_gate': f('moe_w_gate'), 'moe_w_up': f('moe_w_up'), 'moe_w_down': f('moe_w_down'),
        'ab_w_in': w_in_p, 'ab_w_out': f('ab_w_out')[0], 'ret_decay': f('ret_decay')[0].reshape(1, 16),
        'head_g': np.concatenate([f('ret_norm_g')[0], f('na_q_g')[0], f('na_k_g')[0]]).reshape(1, 192),
        'bias_tab': tab,
        'sgu_w_in': f('sgu_w_in')[0], 'sgu_b_in': f('sgu_b_in')[0].reshape(1, 6144),
        'sgu_norm_g': f('sgu_norm_g')[0].reshape(1, 3072), 'sgu_w_s': f('sgu_w_s')[0],
        'sgu_bsT': np.ascontiguousarray(f('sgu_b_s')[0].T), 'sgu_w_out': f('sgu_w_out')[0],
        'cc_row': c_ctx.reshape(1, D),
        'ident': np.eye(128, dtype=np.float32),
        'a2': np.repeat(np.eye(2, dtype=np.float32), 64, axis=1),
    }
    jj = np.arange(128, dtype=np.float32)[:, None]
    ii = np.arange(128, dtype=np.float32)[None, :]
    DF = np.where(ii >= jj, ii - jj, BIG).astype(np.float32)
    DB = np.where(jj >= ii, jj - ii, BIG).astype(np.float32)
    p = np.arange(128, dtype=np.float32)
    pos = np.stack([p + 1, 128 - p, 127 - p, p], axis=1)
    shared['cst'] = np.concatenate([DF, DB, pos], axis=1).astype(np.float32)
    maps = []
    for core in range(8):
        b, s = core // 2, core % 2
        lo = s * TOK
        m = dict(shared)
        m['x_loc'] = np.ascontiguousarray(x[b, lo:lo + TOK])
        m['x_oth'] = np.ascontiguousarray(x[b, (1 - s) * TOK:(2 - s) * TOK])
        halo = np.zeros((512, D), np.float32)
        if s == 1:
            halo[0:256] = x[b, lo - 256:lo]
        else:
            halo[256:512] = x[b, lo + TOK:lo + TOK + 256]
        m['x_halo'] = halo
        m['x_ctx'] = np.ascontiguousarray(ctx[b])
        m['c_row'] = c[b].reshape(1, D)
        C, S = _rope_tables(np.arange(lo, lo + TOK), 0.125)
        m['rope_loc'] = np.concatenate([C, C * 0.125, S, S * 0.125], axis=1).astype(np.float32)
        Co, So = _rope_tables(np.arange((1 - s) * TOK, (2 - s) * TOK), 0.125)
        ro = np.concatenate([Co * 0.125, So * 0.125], axis=1)
        rc = np.concatenate([np.full((256, 64), 0.125, np.float32), np.zeros((256, 64), np.float32)], axis=1)
        m['rope_oth'] = np.concatenate([ro, rc], axis=0).astype(np.float32)
        mo = np.arange(TOK, dtype=np.float32)
        n = np.arange(256, dtype=np.float32)
        if s == 1:
            EFo = 2047.0 - mo
            EBo = np.full(TOK, BIG, np.float32)
        else:
            EFo = np.full(TOK, BIG, np.float32)
            EBo = mo
        EFc = 255.0 - n + 2048.0 * s
        EBc = n + 2048.0 * (1 - s)
        EF = np.concatenate([EFo, EFc]).reshape(18, 128).T
        EB = np.concatenate([EBo, EBc]).reshape(18, 128).T
        m['exp_init'] = np.ascontiguousarray(np.concatenate([EF, EB], axis=1)).astype(np.float32)
        rm = np.full((2, 16, 6, 2), NEG, np.float32)
        for t in range(16):
            lo_t, hi_t = t - 2, t + 2
            if t < 2:
                hi_t = 3
            if t > 13:
                lo_t = 12
            for a in range(2):
                r = 32 * s + 2 * t + a
                r0 = min(max(r - 4, 0), 56)
                for sl, j in enumerate(range(lo_t, hi_t + 1)):
                    for bb in range(2):
                        kr = 32 * s + 2 * j + bb
                        if r0 <= kr <= r0 + 7:
                            rm[a, t, sl, bb] = 0.0
        m['rowmask'] = rm.reshape(2, 192)
        m['dbg_x'] = np.zeros((TOK, D), np.float32)
        maps.append(m)
    return maps


_CACHE = {}


def kernel(**inputs):
    maps = prep_inputs(inputs)
    if 'full' not in _CACHE:
        _CACHE['full'] = build('full')
    kb = _CACHE['full']
    maps = [{k: v for k, v in m.items() if k in kb.declared} for m in maps]
    res = run_bass_kernel_spmd(kb.nc, maps, core_ids=list(range(8)))
    out = np.zeros((4, 4096, D), np.float32)
    for core in range(8):
        b, s = core // 2, core % 2
        out[b, s * TOK:(s + 1) * TOK] = res.results[core]['y_out']
    return out
```
